# Optimizing a Trainium2 kernel written in Bass

```python
import jax, jax.numpy as jnp
from jax import lax
import numpy as np

D_MODEL = 1024
BATCH = 8
SEQ = 8192
DEPTH = 1

NSA_HEADS = 8
NSA_KV_GROUPS = 2
NSA_REP = NSA_HEADS // NSA_KV_GROUPS
HEAD_DIM = 64
NSA_WIDTH = NSA_HEADS * HEAD_DIM
KV_WIDTH = NSA_KV_GROUPS * HEAD_DIM
CMP_BLOCK = 32
CMP_STRIDE = 16
CMP_HIDDEN = 2 * HEAD_DIM
SEL_BLOCK = 64
SEL_TOPK = 16
WINDOW = 512
Q_BLOCK = 64
ROPE_THETA = 10000.0
SGU_GROUPS = 8
SGU_WIDTH = D_MODEL // 2
SGU_GROUP_DIM = SGU_WIDTH // SGU_GROUPS
SGU_CHUNK = 128
IN_COLS = NSA_WIDTH + 6 * KV_WIDTH + 3 * NSA_HEADS + 2 * SGU_WIDTH
PEER_HEADS = 8
PEER_NKEYS = 128
PEER_EXPERTS = PEER_NKEYS * PEER_NKEYS
PEER_QDIM = 256
PEER_HALF = PEER_QDIM // 2
PEER_TOPK = 16
PEER_TOK_BLOCK = 128
DN_ALPHA = (2.0 * DEPTH) ** 0.25
DN_BETA = (8.0 * DEPTH) ** -0.25
LN_EPS = 1e-5
NEG_INF = -1e30
FORCE_SCORE = 1e9

kernel_name = 'hybrid_nsa_sgu_peer_block'


def _layernorm(x, g=None, b=None):
    xf = x.astype(jnp.float32)
    mu = jnp.mean(xf, axis=-1, keepdims=True)
    var = jnp.mean(jnp.square(xf - mu), axis=-1, keepdims=True)
    y = (xf - mu) * lax.rsqrt(var + LN_EPS)
    if g is not None:
        y = y * g.astype(jnp.float32) + b.astype(jnp.float32)
    return y.astype(x.dtype)


def _rope(t):
    half = HEAD_DIM // 2
    pos = jnp.arange(t.shape[1], dtype=jnp.float32)
    inv_freq = ROPE_THETA ** (-jnp.arange(half, dtype=jnp.float32) / half)
    ang = pos[:, None] * inv_freq[None, :]
    cos = jnp.cos(ang)[:, None, :]
    sin = jnp.sin(ang)[:, None, :]
    tf = t.astype(jnp.float32)
    t1, t2 = tf[..., :half], tf[..., half:]
    return jnp.concatenate([t1 * cos - t2 * sin, t1 * sin + t2 * cos], axis=-1).astype(t.dtype)


def _masked_softmax(s, mask):
    p = jax.nn.softmax(jnp.where(mask, s, NEG_INF), axis=-1)
    return p * mask


def _compress(t, pos_emb, w1, b1, w2, b2):
    B, S, G, _ = t.shape
    n_cmp = (S - CMP_BLOCK) // CMP_STRIDE + 1
    idx = np.arange(n_cmp)[:, None] * CMP_STRIDE + np.arange(CMP_BLOCK)[None, :]
    blk = t[:, idx] + pos_emb[:, None, :]
    blk = jnp.moveaxis(blk, 3, 2).reshape(B, n_cmp, G, CMP_BLOCK * HEAD_DIM)
    hid = jax.nn.gelu(blk @ w1 + b1, approximate=False)
    return hid @ w2 + b2


def _sel_aggregation(n_cmp, n_sel):
    c0 = np.arange(n_cmp)[:, None] * CMP_STRIDE
    s0 = np.arange(n_sel)[None, :] * SEL_BLOCK
    ov = np.clip(np.minimum(c0 + CMP_BLOCK, s0 + SEL_BLOCK) - np.maximum(c0, s0), 0, None)
    return (ov / CMP_BLOCK).astype(np.float32)


def _nsa(q, kc, vc, ks, vs, kw, vw, gate_logits):
    B, S = q.shape[0], q.shape[1]
    G, R, HD = NSA_KV_GROUPS, NSA_REP, HEAD_DIM
    n_cmp = kc.shape[1]
    n_sel = S // SEL_BLOCK
    k_top = min(SEL_TOPK, n_sel)
    n_qb = S // Q_BLOCK
    scale = HD ** -0.5
    cmp_end = jnp.arange(n_cmp) * CMP_STRIDE + CMP_BLOCK - 1
    agg = jnp.asarray(_sel_aggregation(n_cmp, n_sel))
    blk_id = jnp.arange(n_sel)
    ks_b = ks.reshape(B, n_sel, SEL_BLOCK, G, HD).transpose(0, 3, 1, 2, 4)
    vs_b = vs.reshape(B, n_sel, SEL_BLOCK, G, HD).transpose(0, 3, 1, 2, 4)
    pad = ((0, 0), (WINDOW, 0), (0, 0), (0, 0))
    kw_p = jnp.pad(kw, pad)
    vw_p = jnp.pad(vw, pad)
    span = Q_BLOCK + WINDOW
    gather = jax.vmap(jax.vmap(lambda tab, idx: tab[idx]))

    def block(args):
        qb, q_blk, g_blk = args
        t = qb * Q_BLOCK + jnp.arange(Q_BLOCK)
        qg = q_blk.reshape(B, Q_BLOCK, G, R, HD)
        s_c = jnp.einsum('bqgrd,bngd->bgrqn', qg, kc).astype(jnp.float32) * scale
        p_c = _masked_softmax(s_c, cmp_end[None, :] <= t[:, None])
        o_c = jnp.einsum('bgrqn,bngd->bqgrd', p_c.astype(vc.dtype), vc)
        imp = jnp.einsum('bgrqn,nj->bgqj', p_c, agg)
        cur = (t // SEL_BLOCK)[:, None]
        forced = (blk_id == 0) | (blk_id == cur) | (blk_id == cur - 1)
        imp = jnp.where(forced, FORCE_SCORE, jnp.where(blk_id <= cur, imp, NEG_INF))
        _, sel = lax.top_k(imp, k_top)
        k_g = gather(ks_b, sel)
        v_g = gather(vs_b, sel)
        kpos = sel[..., None] * SEL_BLOCK + jnp.arange(SEL_BLOCK)
        valid_s = (kpos <= t[:, None, None]).reshape(B, G, 1, Q_BLOCK, k_top * SEL_BLOCK)
        s_s = jnp.einsum('bqgrd,bgqnkd->bgrqnk', qg, k_g).astype(jnp.float32) * scale
        p_s = _masked_softmax(s_s.reshape(B, G, R, Q_BLOCK, k_top * SEL_BLOCK), valid_s)
        o_s = jnp.einsum('bgrqm,bgqmd->bqgrd', p_s.astype(vs.dtype),
                         v_g.reshape(B, G, Q_BLOCK, k_top * SEL_BLOCK, HD))
        start = qb * Q_BLOCK
        k_w = lax.dynamic_slice_in_dim(kw_p, start, span, axis=1)
        v_w = lax.dynamic_slice_in_dim(vw_p, start, span, axis=1)
        kpos_w = start - WINDOW + jnp.arange(span)
        dlt = t[:, None] - kpos_w[None, :]
        valid_w = (dlt >= 0) & (dlt < WINDOW) & (kpos_w[None, :] >= 0)
        s_w = jnp.einsum('bqgrd,bkgd->bgrqk', qg, k_w).astype(jnp.float32) * scale
        p_w = _masked_softmax(s_w, valid_w)
        o_w = jnp.einsum('bgrqk,bkgd->bqgrd', p_w.astype(vw.dtype), v_w)
        g = jax.nn.sigmoid(g_blk.astype(jnp.float32)).reshape(B, Q_BLOCK, G, R, 3).astype(q.dtype)
        o = g[..., 0:1] * o_c + g[..., 1:2] * o_s + g[..., 2:3] * o_w
        return o.reshape(B, Q_BLOCK, NSA_WIDTH)

    qs = q.reshape(B, n_qb, Q_BLOCK, NSA_HEADS, HD).swapaxes(0, 1)
    gs = gate_logits.reshape(B, n_qb, Q_BLOCK, NSA_HEADS, 3).swapaxes(0, 1)
    out = lax.map(block, (jnp.arange(n_qb), qs, gs))
    return out.swapaxes(0, 1).reshape(B, S, NSA_WIDTH)


def _sgu(z, ln_g, ln_b, w_s, b_s):
    B, S, _ = z.shape
    z = jax.nn.gelu(z, approximate=False)
    u, v = z[..., :SGU_WIDTH], z[..., SGU_WIDTH:]
    v = _layernorm(v, ln_g, ln_b)
    v = v.reshape(B, S // SGU_CHUNK, SGU_CHUNK, SGU_GROUPS, SGU_GROUP_DIM)
    mixed = jnp.einsum('gij,bcjgd->bcigd', jnp.tril(w_s), v) + b_s.T[None, None, :, :, None]
    return u * mixed.reshape(B, S, SGU_WIDTH)


def _peer(h, w_q, sub_keys, expert_u, expert_v):
    B, S, D = h.shape
    tokens = h.reshape((B * S) // PEER_TOK_BLOCK, PEER_TOK_BLOCK, D)

    def step(xt):
        qh = (xt @ w_q).reshape(PEER_TOK_BLOCK, PEER_HEADS, 2, PEER_HALF)
        s = jnp.einsum('thcd,hcnd->thcn', qh, sub_keys).astype(jnp.float32)
        s1, i1 = lax.top_k(s[:, :, 0], PEER_TOPK)
        s2, i2 = lax.top_k(s[:, :, 1], PEER_TOPK)
        cand = (s1[..., :, None] + s2[..., None, :]).reshape(PEER_TOK_BLOCK, PEER_HEADS, PEER_TOPK * PEER_TOPK)
        cidx = (i1[..., :, None] * PEER_NKEYS + i2[..., None, :]).reshape(PEER_TOK_BLOCK, PEER_HEADS, PEER_TOPK * PEER_TOPK)
        top_s, pos = lax.top_k(cand, PEER_TOPK)
        eidx = jnp.take_along_axis(cidx, pos, axis=-1)
        gate = jax.nn.softmax(top_s, axis=-1)
        u = expert_u[eidx]
        act = jax.nn.gelu(jnp.einsum('thkd,td->thk', u, xt).astype(jnp.float32), approximate=False)
        w = (gate * act).astype(xt.dtype)
        return jnp.einsum('thk,thkd->td', w, expert_v[eidx])

    return lax.map(step, tokens).reshape(B, S, D)


def _token_mixers(h, w_in, cmp_pos, cmp_w1, cmp_b1, cmp_w2, cmp_b2, sgu_ln_g, sgu_ln_b,
                  sgu_w, sgu_b, w_branch, w_merge, b_merge, w_out):
    B, S, _ = h.shape
    G = NSA_KV_GROUPS
    proj = h @ w_in
    splits = np.cumsum([NSA_WIDTH] + [KV_WIDTH] * 6 + [3 * NSA_HEADS]).tolist()
    q, kc, vc, ks, vs, kw, vw, g_nsa, z = jnp.split(proj, splits, axis=-1)
    q = _rope(q.reshape(B, S, NSA_HEADS, HEAD_DIM))
    kc = _compress(_rope(kc.reshape(B, S, G, HEAD_DIM)), cmp_pos[0], cmp_w1[0], cmp_b1[0], cmp_w2[0], cmp_b2[0])
    vc = _compress(vc.reshape(B, S, G, HEAD_DIM), cmp_pos[1], cmp_w1[1], cmp_b1[1], cmp_w2[1], cmp_b2[1])
    ks = _rope(ks.reshape(B, S, G, HEAD_DIM))
    vs = vs.reshape(B, S, G, HEAD_DIM)
    kw = _rope(kw.reshape(B, S, G, HEAD_DIM))
    vw = vw.reshape(B, S, G, HEAD_DIM)
    o_nsa = _nsa(q, kc, vc, ks, vs, kw, vw, g_nsa.reshape(B, S, NSA_HEADS, 3))
    o_sgu = _sgu(z, sgu_ln_g, sgu_ln_b, sgu_w, sgu_b)
    gate_a, gate_b = jnp.split(jax.nn.sigmoid(h @ w_merge + b_merge), 2, axis=-1)
    merged = gate_a * (o_nsa @ w_branch[0]) + gate_b * (o_sgu @ w_branch[1])
    return merged @ w_out


def setup_inputs(seed: int = 0) -> dict:
    key = jax.random.key(seed)
    ks = jax.random.split(key, 26)
    L, D = DEPTH, D_MODEL
    f32 = jnp.float32

    def nrm(k, shape, scale):
        return scale * jax.random.normal(k, shape, f32)

    return {
        'x': nrm(ks[0], (BATCH, SEQ, D), 1.0),
        'c': nrm(ks[1], (BATCH, D), 1.0),
        'w_ada': nrm(ks[2], (L, D, 6 * D), 0.5 * D ** -0.5),
        'b_ada': nrm(ks[3], (L, 6 * D), 0.02),
        'w_in': nrm(ks[4], (L, D, IN_COLS), D ** -0.5),
        'cmp_pos': nrm(ks[5], (L, 2, CMP_BLOCK, HEAD_DIM), 0.1),
        'cmp_w1': nrm(ks[6], (L, 2, CMP_BLOCK * HEAD_DIM, CMP_HIDDEN), (CMP_BLOCK * HEAD_DIM) ** -0.5),
        'cmp_b1': nrm(ks[7], (L, 2, CMP_HIDDEN), 0.02),
        'cmp_w2': nrm(ks[8], (L, 2, CMP_HIDDEN, HEAD_DIM), CMP_HIDDEN ** -0.5),
        'cmp_b2': nrm(ks[9], (L, 2, HEAD_DIM), 0.02),
        'sgu_ln_g': 1.0 + nrm(ks[10], (L, SGU_WIDTH), 0.02),
        'sgu_ln_b': nrm(ks[11], (L, SGU_WIDTH), 0.02),
        'sgu_w': nrm(ks[12], (L, SGU_GROUPS, SGU_CHUNK, SGU_CHUNK), SGU_CHUNK ** -0.5),
        'sgu_b': 1.0 + nrm(ks[13], (L, SGU_GROUPS, SGU_CHUNK), 0.1),
        'w_branch': nrm(ks[14], (L, 2, NSA_WIDTH, D), NSA_WIDTH ** -0.5),
        'w_merge': nrm(ks[15], (L, D, 2 * D), D ** -0.5),
        'b_merge': nrm(ks[16], (L, 2 * D), 0.02),
        'w_out': nrm(ks[17], (L, D, D), DN_BETA * D ** -0.5),
        'ln1_g': 1.0 + nrm(ks[18], (L, D), 0.02),
        'ln1_b': nrm(ks[19], (L, D), 0.02),
        'peer_wq': nrm(ks[20], (L, D, PEER_HEADS * PEER_QDIM), D ** -0.5),
        'peer_keys': nrm(ks[21], (L, PEER_HEADS, 2, PEER_NKEYS, PEER_HALF), PEER_HALF ** -0.5),
        'peer_u': nrm(ks[22], (L, PEER_EXPERTS, D), D ** -0.5),
        'peer_v': nrm(ks[23], (L, PEER_EXPERTS, D), DN_BETA),
        'ln2_g': 1.0 + nrm(ks[24], (L, D), 0.02),
        'ln2_b': nrm(ks[25], (L, D), 0.02),
    }


def reference(x, c, w_ada, b_ada, w_in, cmp_pos, cmp_w1, cmp_b1, cmp_w2, cmp_b2,
              sgu_ln_g, sgu_ln_b, sgu_w, sgu_b, w_branch, w_merge, b_merge, w_out,
              ln1_g, ln1_b, peer_wq, peer_keys, peer_u, peer_v, ln2_g, ln2_b):
    for l in range(DEPTH):
        mod = jax.nn.silu(c) @ w_ada[l] + b_ada[l]
        sh1, sc1, gt1, sh2, sc2, gt2 = [m[:, None, :] for m in jnp.split(mod, 6, axis=-1)]
        h = _layernorm(x) * (1.0 + sc1) + sh1
        mix = _token_mixers(h, w_in[l], cmp_pos[l], cmp_w1[l], cmp_b1[l], cmp_w2[l], cmp_b2[l],
                            sgu_ln_g[l], sgu_ln_b[l], sgu_w[l], sgu_b[l], w_branch[l],
                            w_merge[l], b_merge[l], w_out[l])
        x = _layernorm(DN_ALPHA * x + gt1 * mix, ln1_g[l], ln1_b[l])
        h = _layernorm(x) * (1.0 + sc2) + sh2
        ffn = _peer(h, peer_wq[l], peer_keys[l], peer_u[l], peer_v[l])
        x = _layernorm(DN_ALPHA * x + gt2 * ffn, ln2_g[l], ln2_b[l])
    return x
```

```python
import numpy as np
import concourse.bass as bass
import concourse.mybir as mybir
from concourse.bass_utils import run_bass_kernel_spmd
from contextlib import ExitStack

F32 = mybir.dt.float32
BF16 = mybir.dt.bfloat16
U32 = mybir.dt.uint32
AF = mybir.ActivationFunctionType
ALU = mybir.AluOpType
AX = mybir.AxisListType

D = 1024
ALPHA = 2.0 ** 0.25
EPS = 1e-5


class Sem:
    def __init__(self, h, name):
        self.h = h
        self.name = name
        self.count = 0


class T:
    def __init__(self, t, name, shape, dt, space):
        self.t = t
        self.name = name
        self.shape = list(shape)
        self.dt = dt
        self.space = space
        self.w = {}
        self.r = {}
        self.dsem = None
        self.fsize = int(np.prod(shape[1:]))

    def __getitem__(self, idx):
        return self.t[idx]

    def ap(self, off, dims, parts=None, pstart=0):
        if self.space == "dram":
            return bass.AP(self.t, off, [list(d) for d in dims])
        if parts is None:
            parts = self.shape[0]
        return bass.AP(self.t, pstart * self.fsize + off, [[self.fsize, parts]] + [list(d) for d in dims])


class KB:
    def __init__(self, nc):
        self.nc = nc
        self.es = ExitStack()
        self.engs = {"pe": nc.tensor, "act": nc.scalar, "dve": nc.vector, "pool": nc.gpsimd, "sp": nc.sync}
        self.esem = {}
        self.waited = {k: {} for k in self.engs}
        self.all_sems = []
        for k in self.engs:
            self.esem[k] = self.new_sem("e_" + k)
        self.n_instr = 0
        self.n_wait = 0
        self.marks = []

    def new_sem(self, name):
        h = self.es.enter_context(self.nc.semaphore(name))
        s = Sem(h, name)
        self.all_sems.append(s)
        return s

    def sb(self, name, shape, dt, es=None):
        t = (es or self.es).enter_context(self.nc.sbuf_tensor(name, list(shape), dt))
        return T(t, name, shape, dt, "sb")

    def ps(self, name, shape, dt, es=None):
        t = (es or self.es).enter_context(self.nc.psum_tensor(name, list(shape), dt))
        return T(t, name, shape, dt, "ps")

    def dram(self, name, shape, dt, kind=None):
        if kind is None:
            t = self.nc.dram_tensor(name, list(shape), dt)
        else:
            t = self.nc.dram_tensor(name, list(shape), dt, kind=kind)
        return T(t, name, shape, dt, "dram")

    def _wait(self, e, sem, val):
        if val <= 0:
            return
        w = self.waited[e]
        if w.get(sem, 0) >= val:
            return
        self.engs[e].wait_ge(sem.h, val)
        w[sem] = val
        self.n_wait += 1

    def _deps(self, e, reads, writes):
        mysem = self.esem[e]
        for b in reads:
            for s, v in b.w.items():
                if s is mysem and e == "pe":
                    continue
                self._wait(e, s, v)
        for b in writes:
            for s, v in b.w.items():
                if s is mysem and e == "pe":
                    continue
                self._wait(e, s, v)
            for s, v in b.r.items():
                if s is mysem and e == "pe":
                    continue
                self._wait(e, s, v)

    def op(self, e, fn, reads, writes, *a, **kw):
        self._deps(e, reads, writes)
        ins = getattr(self.engs[e], fn)(*a, **kw)
        s = self.esem[e]
        s.count += 1
        ins.then_inc(s.h, 1)
        for b in reads:
            b.r[s] = s.count
        for b in writes:
            b.w[s] = s.count
        self.n_instr += 1
        return ins

    def dma(self, q, out_ap, in_ap, reads, writes, sem=None, **kw):
        if sem is None:
            tgt = writes[0]
            if tgt.dsem is None:
                tgt.dsem = self.new_sem("d_" + tgt.name)
            sem = tgt.dsem
        self._deps(q, reads, writes)
        ins = self.engs[q].dma_start(out=out_ap, in_=in_ap, **kw)
        sem.count += 16
        ins.then_inc(sem.h, 16)
        for b in reads:
            b.r[sem] = sem.count
        for b in writes:
            b.w[sem] = sem.count
        self.n_instr += 1
        return ins

    def mark(self, name):
        self.marks.append((name, self.n_instr, self.n_wait))

    def barrier(self):
        for e in self.engs:
            for s in self.all_sems:
                self._wait(e, s, s.count)

    def wait_all(self, e):
        for s in self.all_sems:
            self._wait(e, s, s.count)


def build(S, with_peer=True, with_nsa=True, stop_after=None):
    nc = bass.Bass("TRN2", target_bir_lowering=False)
    kb = KB(nc)
    NS = S // 128

    def din(name, shape, dt=F32):
        return kb.dram(name, shape, dt, kind="ExternalInput")

    x = din("x", [S, D])
    c_col = din("c_col", [128, 8])
    w_ada = din("w_ada", [1024, 6144])
    b_ada = din("b_ada", [1, 6144])
    rows = din("rows", [1, 5120])
    wz = din("wz", [1024, 1024])
    w_merge = din("w_merge", [1024, 2048])
    b_merge_col = din("b_merge_col", [128, 16])
    w_b1 = din("w_b1", [512, 1024])
    w_out = din("w_out", [1024, 1024])
    wsT = din("wsT", [128, 1024])
    sgub_col = din("sgub_col", [128, 8])
    trilm = din("trilm", [128, 128])
    peer_wq = din("peer_wq", [1024, 2048])
    keysT = din("keysT", [128, 2048])
    UT = din("UT", [128, 128, 1024])
    VJ = din("VJ", [128, 128, 1024])
    y = kb.dram("y", [S, D], F32, kind="ExternalOutput")
    x1s = kb.dram("x1s", [S, D], F32)
    UTb = kb.dram("UTb", [128, 128, 1024], BF16)
    VJb = kb.dram("VJb", [128, 128, 1024], BF16)
    NEG = -30000.0
    if with_nsa:
        WAf = din("WAf", [1024, 1920])
        WAt = din("WAt", [1024, 280])
        cosT = din("cosT", [128, S])
        sinS = din("sinS", [128, S])
        w1d = din("w1d", [2, 128, 4096])
        posTd = din("posTd", [128, 64])
        b1col = din("b1col", [128, 2])
        w2kd = din("w2kd", [128, 128])
        w2v = din("w2v", [128, 64])
        b2kcol = din("b2kcol", [128, 1])
        b2vrow = din("b2vrow", [1, 64])
        aggd = din("aggd", [128, 512])
        cmpb = din("cmpb", [128, 2048])
        caus = din("caus", [128, 256])
        based = din("based", [128, 382])
        ebig = din("ebig", [128, S])
        w_b0 = din("w_b0", [512, 1024])
        QTd = kb.dram("QTd", [128, 4 * S], BF16)
        ONd = kb.dram("ONd", [128, 4 * S], BF16)

    ident_f = kb.sb("ident_f", [128, 128], F32)
    ident_b = kb.sb("ident_b", [128, 128], BF16)
    ones_row = kb.sb("ones_row", [1, 128], F32)
    eps_col = kb.sb("eps_col", [128, 1], F32)
    modcol = kb.sb("modcol", [128, 32], F32)
    gt_bc = kb.sb("gt_bc", [128, 2048], F32)
    ln_bc = kb.sb("ln_bc", [128, 5120], F32)
    iota128 = kb.sb("iota128", [128, 128], F32)
    thr15 = kb.sb("thr15", [128, 15], F32)

    kb.op("pool", "memset", [], [ident_f], ident_f[:], 0.0)
    kb.op("pool", "affine_select", [ident_f], [ident_f], out=ident_f[:], in_=ident_f[:], pattern=[[-1, 128]],
          compare_op=ALU.not_equal, fill=1.0, base=0, channel_multiplier=1)
    kb.op("dve", "tensor_copy", [ident_f], [ident_b], out=ident_b[:], in_=ident_f[:])
    kb.op("pool", "memset", [], [ones_row], ones_row[:], 1.0)
    kb.op("pool", "memset", [], [eps_col], eps_col[:], EPS)
    kb.op("pool", "iota", [], [iota128], iota128[:], [[1, 128]], base=0, channel_multiplier=0,
          allow_small_or_imprecise_dtypes=True)
    kb.op("pool", "iota", [], [thr15], thr15[:], [[16, 15]], base=16, channel_multiplier=0,
          allow_small_or_imprecise_dtypes=True)

    def wslab(src, k, n, c0, w):
        return src.ap(c0, [[n, 128], [128 * n, k], [1, w]])

    kb.mark("P")
    with ExitStack() as es:
        wa = [kb.sb("wa%d" % i, [128, 8, 512], F32, es) for i in range(2)]
        mod_row = kb.sb("mod_row", [1, 6144], F32, es)
        b_row = kb.sb("b_row", [1, 6144], F32, es)
        rows_sb = kb.sb("rows_sb", [1, 5120], F32, es)
        ccol = kb.sb("ccol", [128, 8], F32, es)
        scol = kb.sb("scol", [128, 8], F32, es)
        P0 = kb.ps("pP0", [128, 512], F32, es)
        P1 = kb.ps("pP1", [128, 512], F32, es)
        kb.dma("sp", ccol[:], c_col[:], [c_col], [ccol])
        kb.dma("sp", b_row[:], b_ada[:], [b_ada], [b_row])
        kb.dma("sp", rows_sb[:], rows[:], [rows], [rows_sb])
        kb.op("act", "activation", [ccol], [scol], out=scol[:], in_=ccol[:], func=AF.Silu)
        for blk in range(12):
            wt = wa[blk % 2]
            kb.dma("sp", wt[:], wslab(w_ada, 8, 6144, blk * 512, 512), [w_ada], [wt])
            Pb = P0 if blk % 2 == 0 else P1
            for k in range(8):
                kb.op("pe", "matmul", [scol, wt], [Pb], Pb[0:1, 0:512], lhsT=scol[:, k:k + 1], rhs=wt[:, k, :],
                      start=(k == 0), stop=(k == 7))
            kb.op("dve", "tensor_tensor", [b_row], [mod_row, Pb], out=mod_row[0:1, blk * 512:(blk + 1) * 512],
                  in0=Pb[0:1, 0:512], in1=b_row[0:1, blk * 512:(blk + 1) * 512], op=ALU.add)
        for i, c0 in enumerate([2048, 2560, 5120, 5632]):
            Pb = P0 if i % 2 == 0 else P1
            kb.op("pe", "matmul", [ones_row, mod_row], [Pb], Pb[:, 0:512], lhsT=ones_row[0:1, :],
                  rhs=mod_row[0:1, c0:c0 + 512], start=True, stop=True)
            kb.op("act", "copy", [], [gt_bc, Pb], out=gt_bc[:, i * 512:(i + 1) * 512], in_=Pb[:, 0:512])
        for i in range(10):
            Pb = P0 if i % 2 == 0 else P1
            kb.op("pe", "matmul", [ones_row, rows_sb], [Pb], Pb[:, 0:512], lhsT=ones_row[0:1, :],
                  rhs=rows_sb[0:1, i * 512:(i + 1) * 512], start=True, stop=True)
            kb.op("act", "copy", [], [ln_bc, Pb], out=ln_bc[:, i * 512:(i + 1) * 512], in_=Pb[:, 0:512])
        chunks = list(range(0, 8)) + list(range(8, 16)) + list(range(24, 32)) + list(range(32, 40))
        for j, cch in enumerate(chunks):
            kb.op("pe", "matmul", [ones_row, mod_row], [P0], P0[:, j:j + 1], lhsT=mod_row[0:1, cch * 128:(cch + 1) * 128],
                  rhs=ones_row[0:1, 0:1], start=True, stop=True)
        kb.op("dve", "tensor_copy", [], [modcol, P0], out=modcol[:], in_=P0[:, 0:32])
        kb.op("dve", "tensor_scalar", [modcol], [modcol], out=modcol[:, 8:16], in0=modcol[:, 8:16], scalar1=1.0,
              scalar2=None, op0=ALU.add)
        kb.op("dve", "tensor_scalar", [modcol], [modcol], out=modcol[:, 24:32], in0=modcol[:, 24:32], scalar1=1.0,
              scalar2=None, op0=ALU.add)
        kb.barrier()

    def layernorm_stats(src, st, mv, sd, rstd):
        kb.op("dve", "bn_stats", [src], [st], out=st[:, 0:6], in_=src[:, 0:512])
        kb.op("dve", "bn_stats", [src], [st], out=st[:, 6:12], in_=src[:, 512:1024])
        kb.op("dve", "bn_aggr", [st], [mv], out=mv[:, 0:2], in_=st[:, 0:12])
        kb.op("act", "activation", [mv, eps_col], [sd], out=sd[:, 0:1], in_=mv[:, 1:2], func=AF.Sqrt,
              bias=eps_col[:, 0:1], scale=1.0)
        kb.op("dve", "reciprocal", [sd], [rstd], out=rstd[:, 0:1], in_=sd[:, 0:1])

    def ln_transpose(src, xn_b, PT, dstT, col0, ncols, sc_off, sh_off, st, mv, sd, rstd):
        layernorm_stats(src, st, mv, sd, rstd)
        kb.op("dve", "tensor_scalar", [src, mv, rstd], [xn_b], out=xn_b[:], in0=src[:], scalar1=mv[:, 0:1],
              scalar2=rstd[:, 0:1], op0=ALU.subtract, op1=ALU.mult)
        for k in range(8):
            kb.op("pe", "transpose", [xn_b, ident_b], [PT], PT[:, k * 128:(k + 1) * 128], xn_b[:, k * 128:(k + 1) * 128],
                  ident_b[:])
        for k in range(8):
            kb.op("act", "activation", [modcol], [dstT, PT], out=dstT[:, k, col0:col0 + 128],
                  in_=PT[:, k * 128:(k + 1) * 128], func=AF.Identity, bias=modcol[:, sh_off + k:sh_off + k + 1],
                  scale=modcol[:, sc_off + k:sc_off + k + 1])

    def resid_ln(xsrc, Pa, Pb, gt_off, g_off, b_off, ybuf, obuf, st, mv, sd, rstd):
        for half, Pp in enumerate([Pa, Pb]):
            kb.op("dve", "tensor_tensor", [gt_bc], [ybuf, Pp], out=ybuf[:, half * 512:(half + 1) * 512], in0=Pp[:, 0:512],
                  in1=gt_bc[:, gt_off + half * 512:gt_off + (half + 1) * 512], op=ALU.mult)
        kb.op("dve", "scalar_tensor_tensor", [xsrc, ybuf], [ybuf], out=ybuf[:], in0=xsrc[:], scalar=ALPHA, in1=ybuf[:],
              op0=ALU.mult, op1=ALU.add)
        layernorm_stats(ybuf, st, mv, sd, rstd)
        kb.op("dve", "scalar_tensor_tensor", [ybuf, mv, ln_bc], [obuf], out=obuf[:], in0=ybuf[:], scalar=mv[:, 0:1],
              in1=ln_bc[:, g_off:g_off + 1024], op0=ALU.subtract, op1=ALU.mult)
        kb.op("dve", "scalar_tensor_tensor", [obuf, rstd, ln_bc], [obuf], out=obuf[:], in0=obuf[:], scalar=rstd[:, 0:1],
              in1=ln_bc[:, b_off:b_off + 1024], op0=ALU.mult, op1=ALU.add)


    kb.mark("A")
    if with_nsa:
        NQ = S // 128
        es1 = ExitStack()
        KsT = kb.sb("KsT", [128, S], BF16, es1)
        KwT = kb.sb("KwT", [128, S], BF16, es1)
        VS = kb.sb("VS", [128, NQ * 130], BF16, es1)
        VW = kb.sb("VW", [128, NQ * 130], BF16, es1)
        Gt = kb.sb("Gt", [128, NQ * 24], F32, es1)
        KCT = kb.sb("KCT", [128, 512], BF16, es1)
        VC = kb.sb("VC", [128, 4 * 130], BF16, es1)
        kb.op("pool", "memset", [], [VS], VS[:], 1.0)
        kb.op("pool", "memset", [], [VW], VW[:], 1.0)
        kb.op("pool", "memset", [], [VC], VC[:], 1.0)
        kb.op("pool", "memset", [], [KCT], KCT[:], 0.0)
        with ExitStack() as es:
            KcR = kb.sb("KcR", [128, S], BF16, es)
            VcR = kb.sb("VcR", [128, S], BF16, es)
            xs1 = kb.sb("xsA", [128, 1024], F32, es)
            xn_b = kb.sb("xn_bA", [128, 1024], BF16, es)
            hT = kb.sb("hTA", [128, 8, 512], BF16, es)
            WAt_b = kb.sb("WAt_b", [128, 8, 280], BF16, es)
            wch = [kb.sb("wch%d" % i, [128, 8, 128], BF16, es) for i in range(4)]
            cs = kb.sb("cs", [128, 512], F32, es)
            sn = kb.sb("sn", [128, 512], F32, es)
            t1 = kb.sb("t1", [128, 512], F32, es)
            t2 = kb.sb("t2", [128, 512], F32, es)
            qtmp = [kb.sb("qtmp%d" % i, [128, 512], BF16, es) for i in range(2)]
            st = kb.sb("stA", [128, 12], F32, es)
            mv = kb.sb("mvA", [128, 2], F32, es)
            sd = kb.sb("sdA", [128, 1], F32, es)
            rstd = kb.sb("rstdA", [128, 1], F32, es)
            w1_b = [kb.sb("w1_b%d" % i, [128, 4096], BF16, es) for i in range(2)]
            posT_b = kb.sb("posT_b", [128, 64], BF16, es)
            b1c = kb.sb("b1c", [128, 2], F32, es)
            w2k_b = kb.sb("w2k_b", [128, 128], BF16, es)
            w2v_b = kb.sb("w2v_b", [128, 64], BF16, es)
            b2kc = kb.sb("b2kc", [128, 1], F32, es)
            b2v_b = kb.sb("b2v_b", [1, 64], BF16, es)
            ones_b = kb.sb("ones_b", [1, 128], BF16, es)
            hidT = kb.sb("hidT", [128, 512], BF16, es)
            biasv = kb.sb("biasv", [128, 1], F32, es)
            PT = kb.ps("PTA", [128, 1024], BF16, es)
            Pq = kb.ps("Pq", [128, 512], F32, es)
            Pqs = kb.ps("Pqs", [128, 512], F32, es)
            PV = kb.ps("PV", [128, 512], F32, es)

            kb.dma("pool", WAt_b[:], wslab(WAt, 8, 280, 0, 280), [WAt], [WAt_b])
            nw = 0
            for tg in range(S // 512):
                t0 = tg * 512
                for s in range(4):
                    kb.dma("sp", xs1[:], x.ap((t0 + s * 128) * D, [[D, 128], [1, D]]), [x], [xs1])
                    ln_transpose(xs1, xn_b, PT, hT, s * 128, 128, 8, 0, st, mv, sd, rstd)
                kb.dma("sp", cs[:], cosT.ap(t0, [[S, 128], [1, 512]]), [cosT], [cs])
                kb.dma("sp", sn[:], sinS.ap(t0, [[S, 128], [1, 512]]), [sinS], [sn])
                for s in range(4):
                    tile = tg * 4 + s
                    for k in range(8):
                        kb.op("pe", "matmul", [hT, WAt_b], [PV], PV[:, 0:280], lhsT=hT[:, k, s * 128:(s + 1) * 128],
                              rhs=WAt_b[:, k, :], start=(k == 0), stop=(k == 7))
                    kb.op("act", "copy", [], [VS, PV], out=VS.ap(tile * 130, [[65, 2], [1, 64]]),
                          in_=PV.ap(0, [[64, 2], [1, 64]]))
                    kb.op("act", "copy", [], [VW, PV], out=VW.ap(tile * 130, [[65, 2], [1, 64]]),
                          in_=PV.ap(128, [[64, 2], [1, 64]]))
                    kb.op("act", "activation", [], [Gt, PV], out=Gt.ap(tile * 24, [[1, 8], [8, 3]]),
                          in_=PV.ap(256, [[3, 8], [1, 3]]), func=AF.Sigmoid)
                jobs = [(c, c + 4, "q", c) for c in range(4)] + [(8, 11, "kc", 0), (9, 12, "ks", 0), (10, 13, "kw", 0)]
                for ji, (ca, cb_, kind, r) in enumerate(jobs):
                    wa_ = wch[nw % 4]
                    wb_ = wch[(nw + 1) % 4]
                    nw += 2
                    kb.dma("pool", wa_[:], wslab(WAf, 8, 1920, ca * 128, 128), [WAf], [wa_])
                    kb.dma("pool", wb_[:], wslab(WAf, 8, 1920, cb_ * 128, 128), [WAf], [wb_])
                    for (wt_, Pp) in ((wa_, Pq), (wb_, Pqs)):
                        for k in range(8):
                            kb.op("pe", "matmul", [hT, wt_], [Pp], Pp[:, 0:512], lhsT=wt_[:, k, :], rhs=hT[:, k, :],
                                  start=(k == 0), stop=(k == 7))
                    kb.op("dve", "tensor_tensor", [cs], [t1, Pq], out=t1[:], in0=Pq[:, 0:512], in1=cs[:], op=ALU.mult)
                    kb.op("dve", "tensor_tensor", [sn], [t2, Pqs], out=t2[:], in0=Pqs[:, 0:512], in1=sn[:], op=ALU.mult)
                    if kind == "q":
                        qb = qtmp[ji % 2]
                        kb.op("dve", "tensor_tensor", [t1, t2], [qb], out=qb[:], in0=t1[:], in1=t2[:], op=ALU.add)
                        kb.dma("sp", QTd.ap(r * S + t0, [[4 * S, 128], [1, 512]]), qb[:], [qb], [QTd])
                    else:
                        dstT = {"kc": KcR, "ks": KsT, "kw": KwT}[kind]
                        kb.op("dve", "tensor_tensor", [t1, t2], [dstT], out=dstT[:, t0:t0 + 512], in0=t1[:], in1=t2[:],
                              op=ALU.add)
                wv_ = wch[nw % 4]
                nw += 1
                kb.dma("pool", wv_[:], wslab(WAf, 8, 1920, 14 * 128, 128), [WAf], [wv_])
                for k in range(8):
                    kb.op("pe", "matmul", [hT, wv_], [Pq], Pq[:, 0:512], lhsT=wv_[:, k, :], rhs=hT[:, k, :],
                          start=(k == 0), stop=(k == 7))
                kb.op("act", "copy", [], [VcR, Pq], out=VcR[:, t0:t0 + 512], in_=Pq[:, 0:512])

            kb.mark("B")
            if S >= 512:
                ncmp = (S - 32) // 16 + 1
                for kv in range(2):
                    kb.dma("pool", w1_b[kv][:], w1d.ap(kv * 128 * 4096, [[4096, 128], [1, 4096]]), [w1d], [w1_b[kv]])
                kb.dma("pool", posT_b[:], posTd[:], [posTd], [posT_b])
                kb.dma("sp", b1c[:], b1col[:], [b1col], [b1c])
                kb.dma("pool", w2k_b[:], w2kd[:], [w2kd], [w2k_b])
                kb.dma("pool", w2v_b[:], w2v[:], [w2v], [w2v_b])
                kb.dma("sp", b2kc[:], b2kcol[:], [b2kcol], [b2kc])
                kb.dma("pool", b2v_b[:], b2vrow[:], [b2vrow], [b2v_b])
                kb.op("pool", "memset", [], [ones_b], ones_b[:], 1.0)
                kb.op("pool", "memset", [], [hidT], hidT[:], 0.0)
                nc_pad = min(ncmp, 511)
                for kv in range(2):
                    raw = KcR if kv == 0 else VcR
                    for p in range(32):
                        kb.op("pe", "matmul", [w1_b[kv], posT_b], [PV], PV[:, 0:1],
                              lhsT=w1_b[kv].ap(p * 128, [[1, 128]], parts=64), rhs=posT_b.ap(kv * 32 + p, [[1, 1]], parts=64),
                              start=(p == 0), stop=(p == 31))
                    kb.op("dve", "tensor_tensor", [b1c], [biasv, PV], out=biasv[:], in0=PV[:, 0:1], in1=b1c[:, kv:kv + 1],
                          op=ALU.add)
                    for g in range(2):
                        for p in range(32):
                            kb.op("pe", "matmul", [w1_b[kv], raw], [Pq], Pq[:, 0:nc_pad],
                                  lhsT=w1_b[kv].ap(p * 128, [[1, 128]], parts=64, pstart=g * 64),
                                  rhs=raw.ap(p, [[16, nc_pad]], parts=64, pstart=g * 64), start=(p == 0), stop=(p == 31))
                        kb.op("act", "activation", [biasv], [hidT, Pq], out=hidT[:, 0:nc_pad], in_=Pq[:, 0:nc_pad], func=AF.Gelu,
                              bias=biasv[:, 0:1], scale=1.0)
                        if kv == 0:
                            kb.op("pe", "matmul", [w2k_b, hidT], [Pqs], Pqs[:, 0:nc_pad], lhsT=w2k_b[:], rhs=hidT[:, 0:nc_pad],
                                  start=True, stop=True)
                            kb.op("act", "activation", [b2kc], [KCT, Pqs], out=KCT.ap(0, [[1, nc_pad]], parts=64, pstart=g * 64),
                                  in_=Pqs.ap(0, [[1, nc_pad]], parts=64, pstart=g * 64), func=AF.Identity,
                                  bias=b2kc.ap(0, [[1, 1]], parts=64, pstart=g * 64), scale=1.0)
                        else:
                            for nt in range(4):
                                kb.op("pe", "matmul", [hidT, w2v_b], [PV], PV[:, 0:64], lhsT=hidT[:, nt * 128:(nt + 1) * 128],
                                      rhs=w2v_b[:], start=True, stop=False)
                                kb.op("pe", "matmul", [ones_b, b2v_b], [PV], PV[:, 0:64], lhsT=ones_b[0:1, :], rhs=b2v_b[0:1, :],
                                      start=False, stop=True)
                                kb.op("act", "copy", [], [VC, PV], out=VC.ap((nt * 2 + g) * 65, [[1, 64]]), in_=PV[:, 0:64])
            kb.barrier()

        kb.mark("C1")
        with ExitStack() as es:
            E_b = kb.sb("E_b", [128, S], BF16, es)
            CMPB = kb.sb("CMPB", [128, 2048], BF16, es)
            CAUS = kb.sb("CAUS", [128, 256], BF16, es)
            AGG = kb.sb("AGG", [128, 512], BF16, es)
            BASE = kb.sb("BASE", [128, 382], F32, es)
            Qt = kb.sb("Qt", [128, 512], BF16, es)
            Pt = [kb.sb("Pt%d" % i, [128, 512], BF16, es) for i in range(2)]
            Osb = kb.sb("Osb", [128, 3 * 512], F32, es)
            onsa = kb.sb("onsa", [128, 512], F32, es)
            onT = kb.sb("onT", [128, 512], BF16, es)
            imp = kb.sb("imp", [128, 128], F32, es)
            val = kb.sb("val", [128, 128], F32, es)
            val2 = kb.sb("val2", [128, 128], F32, es)
            negs = kb.sb("negs", [128, 128], F32, es)
            negT = kb.sb("negT", [128, 128], BF16, es)
            m8 = kb.sb("m8", [128, 16], F32, es)
            zc = kb.sb("zc", [128, 4], F32, es)
            rzc = kb.sb("rzc", [128, 4], F32, es)
            zt = kb.sb("zt", [128, 4], F32, es)
            coef = kb.sb("coef", [128, 4], F32, es)
            PS = [kb.ps("PS%d" % i, [128, 512], F32, es) for i in range(2)]
            PO3 = [kb.ps("PO3_%d" % i, [128, 512], F32, es) for i in range(3)]
            PU = kb.ps("PU", [128, 512], F32, es)
            PTr = kb.ps("PTr", [128, 512], F32, es)

            kb.dma("pool", E_b[:], ebig[:], [ebig], [E_b])
            kb.dma("pool", CMPB[:], cmpb[:], [cmpb], [CMPB])
            kb.dma("pool", CAUS[:], caus[:], [caus], [CAUS])
            kb.dma("pool", AGG[:], aggd[:], [aggd], [AGG])
            kb.dma("sp", BASE[:], based[:], [based], [BASE])
            npt = [0]

            def attn_tile(Pout, first, last, kT, kcol, Vt, voff, g, biases):
                Ps = PS[npt[0] % 2]
                Pb = Pt[npt[0] % 2]
                npt[0] += 1
                nb = len(biases)
                for bi, (lt, lap, rt, rap) in enumerate(biases):
                    kb.op("pe", "matmul", [lt, rt], [Ps], Ps[:, 0:512], lhsT=lap, rhs=rap, start=(bi == 0), stop=False)
                kb.op("pe", "matmul", [kT, Qt], [Ps], Ps[:, 0:512], lhsT=kT.ap(kcol, [[1, 128]], parts=64, pstart=g * 64),
                      rhs=Qt.ap(0, [[128, 4], [1, 128]], parts=64, pstart=g * 64), start=(nb == 0), stop=True)
                kb.op("act", "activation", [], [Pb, Ps], out=Pb[:], in_=Ps[:, 0:512], func=AF.Exp, scale=0.125)
                kb.op("pe", "matmul", [Vt, Pb], [Pout], Pout.ap(0, [[1, 512]], parts=65), lhsT=Vt.ap(voff, [[1, 65]]), rhs=Pb[:],
                      start=first, stop=last)
                return Pb

            for qt in range(NQ):
                kb.dma("sp", Qt.ap(0, [[128, 4], [1, 128]]), QTd.ap(qt * 128, [[4 * S, 128], [S, 4], [1, 128]]), [QTd], [Qt])
                for g in range(2):
                    ntmax = min(3, (8 * qt + 6) // 128)
                    for nt in range(ntmax + 1):
                        m = qt - 16 * nt
                        biases = []
                        if m <= 15:
                            biases.append((ident_b, ident_b[:], CMPB, CMPB.ap(m * 128, [[0, 4], [1, 128]])))
                        Pb = attn_tile(PO3[0], nt == 0, nt == ntmax, KCT, nt * 128, VC, (nt * 2 + g) * 65, g, biases)
                        for r in range(4):
                            kb.op("pe", "matmul", [Pb, AGG], [PU], PU[:, r * 128:(r + 1) * 128], lhsT=Pb[:, r * 128:(r + 1) * 128],
                                  rhs=AGG[:, nt * 128:(nt + 1) * 128], start=(nt == 0 and r == 0), stop=(nt == ntmax and r == 3),
                                  skip_group_check=True)
                    kb.op("dve", "tensor_reduce", [], [zc, PU], out=zc[:], in_=PU.ap(0, [[128, 4], [1, 128]]), axis=AX.X, op=ALU.add)
                    kb.op("dve", "tensor_scalar", [zc], [zc], out=zc[:], in0=zc[:], scalar1=1e-30, scalar2=None, op0=ALU.max)
                    kb.op("dve", "reciprocal", [zc], [rzc], out=rzc[:], in_=zc[:])
                    kb.op("dve", "tensor_scalar", [rzc], [imp, PU], out=imp[:], in0=PU[:, 0:128], scalar1=rzc[:, 0:1], scalar2=None,
                          op0=ALU.mult)
                    for r in range(1, 4):
                        kb.op("dve", "scalar_tensor_tensor", [rzc, imp], [imp, PU], out=imp[:], in0=PU[:, r * 128:(r + 1) * 128],
                              scalar=rzc[:, r:r + 1], in1=imp[:], op0=ALU.mult, op1=ALU.add)
                    kb.op("dve", "tensor_tensor", [imp, BASE], [val], out=val[:], in0=imp[:], in1=BASE[:, 126 - 2 * qt:254 - 2 * qt],
                          op=ALU.add)
                    kb.op("dve", "tensor_tensor", [val, BASE], [val], out=val[:], in0=val[:], in1=BASE[:, 254:382], op=ALU.add)
                    kb.op("dve", "max", [val], [m8], out=m8[:, 0:8], in_=val[:])
                    kb.op("dve", "match_replace", [m8, val], [val2], out=val2[:], in_to_replace=m8[:, 0:8], in_values=val[:],
                          imm_value=-3e38)
                    kb.op("dve", "max", [val2], [m8], out=m8[:, 8:16], in_=val2[:])
                    kb.op("dve", "tensor_scalar", [val, m8], [negs], out=negs[:], in0=val[:], scalar1=m8[:, 15:16], scalar2=NEG,
                          op0=ALU.is_lt, op1=ALU.mult)
                    kb.op("pe", "transpose", [negs, ident_f], [PTr], PTr[:, 0:128], negs[:], ident_f[:])
                    kb.op("act", "copy", [], [negT, PTr], out=negT[:], in_=PTr[:, 0:128])
                    for kt in range(qt + 1):
                        biases = [(E_b, E_b[:, kt * 128:(kt + 1) * 128], negT, negT.ap(0, [[0, 4], [1, 128]]))]
                        if kt == qt:
                            biases.append((ident_b, ident_b[:], CAUS, CAUS.ap(0, [[0, 4], [1, 128]])))
                        attn_tile(PO3[1], kt == 0, kt == qt, KsT, kt * 128, VS, (kt * 2 + g) * 65, g, biases)
                    k0 = max(0, qt - 4)
                    for kt in range(k0, qt + 1):
                        biases = []
                        if kt == qt:
                            biases.append((ident_b, ident_b[:], CAUS, CAUS.ap(0, [[0, 4], [1, 128]])))
                        elif kt == qt - 4:
                            biases.append((ident_b, ident_b[:], CAUS, CAUS.ap(128, [[0, 4], [1, 128]])))
                        attn_tile(PO3[2], kt == k0, kt == qt, KwT, kt * 128, VW, (kt * 2 + g) * 65, g, biases)
                    for br in range(3):
                        kb.op("act", "copy", [], [Osb, PO3[br]], out=Osb.ap(br * 512, [[1, 512]], parts=65),
                              in_=PO3[br].ap(0, [[1, 512]], parts=65))
                    for br in range(3):
                        for r in range(4):
                            kb.op("pe", "transpose", [Osb, ident_f], [PTr], PTr[:, r * 65:(r + 1) * 65],
                                  Osb.ap(br * 512 + r * 128, [[1, 128]], parts=65), ident_f.ap(0, [[1, 65]], parts=65))
                        kb.op("dve", "tensor_scalar", [], [zt, PTr], out=zt[:], in0=PTr.ap(64, [[65, 4]]), scalar1=1e-30, scalar2=None,
                              op0=ALU.max)
                        kb.op("dve", "reciprocal", [zt], [zt], out=zt[:], in_=zt[:])
                        kb.op("dve", "tensor_tensor", [zt, Gt], [coef], out=coef[:], in0=zt[:],
                              in1=Gt[:, qt * 24 + br * 8 + g * 4:qt * 24 + br * 8 + g * 4 + 4], op=ALU.mult)
                        for r in range(4):
                            h = g * 4 + r
                            if br == 0:
                                kb.op("dve", "tensor_scalar", [coef], [onsa, PTr], out=onsa[:, h * 64:(h + 1) * 64],
                                      in0=PTr[:, r * 65:r * 65 + 64], scalar1=coef[:, r:r + 1], scalar2=None, op0=ALU.mult)
                            else:
                                kb.op("dve", "scalar_tensor_tensor", [coef, onsa], [onsa, PTr], out=onsa[:, h * 64:(h + 1) * 64],
                                      in0=PTr[:, r * 65:r * 65 + 64], scalar=coef[:, r:r + 1], in1=onsa[:, h * 64:(h + 1) * 64],
                                      op0=ALU.mult, op1=ALU.add)
                for kc in range(4):
                    kb.op("pe", "transpose", [onsa, ident_f], [PTr], PTr[:, kc * 128:(kc + 1) * 128], onsa[:, kc * 128:(kc + 1) * 128],
                          ident_f[:])
                kb.op("act", "copy", [], [onT, PTr], out=onT[:], in_=PTr[:, 0:512])
                kb.dma("sp", ONd.ap(qt * 128, [[4 * S, 128], [S, 4], [1, 128]]), onT.ap(0, [[128, 4], [1, 128]]), [onT], [ONd])
            kb.barrier()
        es1.close()

    kb.mark("W")
    if with_peer:
        with ExitStack() as es:
            cb = [kb.sb("cb%d" % i, [128, 2048], BF16, es) for i in range(4)]
            n = 0
            for src, dst in ((UT, UTb), (VJ, VJb)):
                for j2 in range(64):
                    b = cb[n % 4]
                    n += 1
                    kb.dma("pool", b.ap(0, [[1024, 2], [1, 1024]]), src.ap(j2 * 2 * 131072, [[1024, 128], [131072, 2], [1, 1024]]),
                           [src], [b])
                    kb.dma("sp", dst.ap(j2 * 2 * 131072, [[1024, 128], [131072, 2], [1, 1024]]), b.ap(0, [[1024, 2], [1, 1024]]),
                           [b], [dst])
            kb.barrier()

    kb.mark("C2")
    with ExitStack() as es:
        Wz_b = kb.sb("Wz_b", [128, 8, 1024], BF16, es)
        Wm_b = kb.sb("Wm_b", [128, 8, 2048], BF16, es)
        Wb1_b = kb.sb("Wb1_b", [128, 4, 1024], BF16, es)
        Wo_b = kb.sb("Wo_b", [128, 8, 1024], BF16, es)
        WsT_f = kb.sb("WsT_f", [128, 1024], F32, es)
        WsT_b = kb.sb("WsT_b", [128, 8, 128], BF16, es)
        tril_sb = kb.sb("tril_sb", [128, 128], F32, es)
        sgub = kb.sb("sgub", [128, 8], F32, es)
        bmcol = kb.sb("bmcol", [128, 16], F32, es)
        xs = [kb.sb("xs%d" % i, [128, 1024], F32, es) for i in range(4)]
        xn_b = kb.sb("xn_b", [128, 1024], BF16, es)
        hT = kb.sb("hT", [128, 8, 512], BF16, es)
        gab = kb.sb("gab", [128, 2, 512], BF16, es)
        u_b = kb.sb("u_b", [128, 512], BF16, es)
        v_f = kb.sb("v_f", [128, 512], F32, es)
        vb = kb.sb("vb", [128, 512], BF16, es)
        tmp = kb.sb("tmp", [128, 512], F32, es)
        osgu = kb.sb("osgu", [128, 512], BF16, es)
        osT = kb.sb("osT", [128, 4, 512], BF16, es)
        mT = kb.sb("mT", [128, 8, 512], BF16, es)
        ybuf = kb.sb("ybuf", [128, 1024], F32, es)
        x1 = kb.sb("x1", [128, 1024], F32, es)
        st = kb.sb("st", [128, 12], F32, es)
        mv = kb.sb("mv", [128, 2], F32, es)
        sd = kb.sb("sd", [128, 1], F32, es)
        rstd = kb.sb("rstd", [128, 1], F32, es)
        PT = kb.ps("PT", [128, 1024], BF16, es)
        PGA = kb.ps("PGA", [128, 512], F32, es)
        PGB = kb.ps("PGB", [128, 512], F32, es)
        PB_ = kb.ps("PB_", [128, 512], F32, es)
        if with_nsa:
            PA_ = kb.ps("PA_", [128, 512], F32, es)
            Wb0_b = kb.sb("Wb0_b", [128, 4, 1024], BF16, es)
            onsaT = kb.sb("onsaT", [128, 4, 512], BF16, es)
            tmpa = kb.sb("tmpa", [128, 512], F32, es)
            kb.dma("pool", Wb0_b[:], wslab(w_b0, 4, 1024, 0, 1024), [w_b0], [Wb0_b])
        PZ0 = kb.ps("PZ0", [128, 512], F32, es)
        PZ1 = kb.ps("PZ1", [128, 512], F32, es)
        PM = kb.ps("PM", [128, 512], F32, es)

        kb.dma("pool", Wz_b[:], wslab(wz, 8, 1024, 0, 1024), [wz], [Wz_b])
        kb.dma("pool", Wm_b[:], wslab(w_merge, 8, 2048, 0, 2048), [w_merge], [Wm_b])
        kb.dma("pool", Wb1_b[:], wslab(w_b1, 4, 1024, 0, 1024), [w_b1], [Wb1_b])
        kb.dma("pool", Wo_b[:], wslab(w_out, 8, 1024, 0, 1024), [w_out], [Wo_b])
        kb.dma("sp", WsT_f[:], wsT[:], [wsT], [WsT_f])
        kb.dma("sp", tril_sb[:], trilm[:], [trilm], [tril_sb])
        kb.dma("sp", sgub[:], sgub_col[:], [sgub_col], [sgub])
        kb.dma("sp", bmcol[:], b_merge_col[:], [b_merge_col], [bmcol])
        kb.op("dve", "tensor_tensor", [WsT_f, tril_sb], [WsT_b], out=WsT_b.ap(0, [[128, 8], [1, 128]]),
              in0=WsT_f.ap(0, [[128, 8], [1, 128]]), in1=tril_sb.ap(0, [[0, 8], [1, 128]]), op=ALU.mult)

        for tg in range(S // 512):
            t0 = tg * 512
            for s in range(4):
                kb.dma("sp", xs[s][:], x.ap((t0 + s * 128) * D, [[D, 128], [1, D]]), [x], [xs[s]])
            for s in range(4):
                ln_transpose(xs[s], xn_b, PT, hT, s * 128, 128, 8, 0, st, mv, sd, rstd)
            if with_nsa:
                kb.dma("sp", onsaT.ap(0, [[512, 4], [1, 512]]), ONd.ap(t0, [[4 * S, 128], [S, 4], [1, 512]]), [ONd], [onsaT])
            for s in range(4):
                for half, Pz in enumerate([PZ0, PZ1]):
                    for k in range(8):
                        kb.op("pe", "matmul", [hT, Wz_b], [Pz], Pz[:, 0:512], lhsT=hT[:, k, s * 128:(s + 1) * 128],
                              rhs=Wz_b[:, k, half * 512:(half + 1) * 512], start=(k == 0), stop=(k == 7))
                kb.op("act", "activation", [], [u_b, PZ0], out=u_b[:], in_=PZ0[:, 0:512], func=AF.Gelu)
                kb.op("act", "activation", [], [v_f, PZ1], out=v_f[:], in_=PZ1[:, 0:512], func=AF.Gelu)
                kb.op("dve", "bn_stats", [v_f], [st], out=st[:, 0:6], in_=v_f[:])
                kb.op("dve", "bn_aggr", [st], [mv], out=mv[:, 0:2], in_=st[:, 0:6])
                kb.op("act", "activation", [mv, eps_col], [sd], out=sd[:, 0:1], in_=mv[:, 1:2], func=AF.Sqrt,
                      bias=eps_col[:, 0:1], scale=1.0)
                kb.op("dve", "reciprocal", [sd], [rstd], out=rstd[:, 0:1], in_=sd[:, 0:1])
                kb.op("dve", "scalar_tensor_tensor", [v_f, mv, ln_bc], [tmp], out=tmp[:], in0=v_f[:], scalar=mv[:, 0:1],
                      in1=ln_bc[:, 4096:4608], op0=ALU.subtract, op1=ALU.mult)
                kb.op("dve", "scalar_tensor_tensor", [tmp, rstd, ln_bc], [vb], out=vb[:], in0=tmp[:], scalar=rstd[:, 0:1],
                      in1=ln_bc[:, 4608:5120], op0=ALU.mult, op1=ALU.add)
                for g in range(8):
                    kb.op("pe", "matmul", [WsT_b, vb], [PM], PM[:, g * 64:(g + 1) * 64], lhsT=WsT_b[:, g, :],
                          rhs=vb[:, g * 64:(g + 1) * 64], start=True, stop=True)
                kb.op("dve", "tensor_tensor", [sgub], [tmp, PM], out=tmp.ap(0, [[64, 8], [1, 64]]),
                      in0=PM.ap(0, [[64, 8], [1, 64]]), in1=sgub.ap(0, [[1, 8], [0, 64]]), op=ALU.add)
                kb.op("dve", "tensor_tensor", [tmp, u_b], [osgu], out=osgu[:], in0=tmp[:], in1=u_b[:], op=ALU.mult)
                for kc in range(4):
                    kb.op("pe", "transpose", [osgu, ident_b], [PT], PT[:, kc * 128:(kc + 1) * 128],
                          osgu[:, kc * 128:(kc + 1) * 128], ident_b[:])
                kb.op("act", "copy", [], [osT, PT], out=osT.ap(s * 128, [[512, 4], [1, 128]]),
                      in_=PT.ap(0, [[128, 4], [1, 128]]))
            for cch in range(8):
                for gi, (Pg, col) in enumerate([(PGA, cch), (PGB, 8 + cch)]):
                    if gi == 0 and not with_nsa:
                        continue
                    for k in range(8):
                        kb.op("pe", "matmul", [hT, Wm_b], [Pg], Pg[:, 0:512], lhsT=Wm_b[:, k, col * 128:(col + 1) * 128],
                              rhs=hT[:, k, :], start=(k == 0), stop=(k == 7))
                for kc in range(4):
                    kb.op("pe", "matmul", [osT, Wb1_b], [PB_], PB_[:, 0:512], lhsT=Wb1_b[:, kc, cch * 128:(cch + 1) * 128],
                          rhs=osT[:, kc, :], start=(kc == 0), stop=(kc == 3))
                kb.op("act", "activation", [bmcol], [gab, PGB], out=gab[:, 1, :], in_=PGB[:, 0:512], func=AF.Sigmoid,
                      bias=bmcol[:, 8 + cch:9 + cch], scale=1.0)
                if with_nsa:
                    for kc in range(4):
                        kb.op("pe", "matmul", [onsaT, Wb0_b], [PA_], PA_[:, 0:512], lhsT=Wb0_b[:, kc, cch * 128:(cch + 1) * 128],
                              rhs=onsaT[:, kc, :], start=(kc == 0), stop=(kc == 3))
                    kb.op("act", "activation", [bmcol], [gab, PGA], out=gab[:, 0, :], in_=PGA[:, 0:512], func=AF.Sigmoid,
                          bias=bmcol[:, cch:cch + 1], scale=1.0)
                    kb.op("dve", "tensor_tensor", [gab], [tmpa, PA_], out=tmpa[:], in0=PA_[:, 0:512], in1=gab[:, 0, :], op=ALU.mult)
                    kb.op("dve", "tensor_tensor", [gab], [tmp, PB_], out=tmp[:], in0=PB_[:, 0:512], in1=gab[:, 1, :], op=ALU.mult)
                    kb.op("dve", "tensor_tensor", [tmp, tmpa], [mT], out=mT[:, cch, :], in0=tmp[:], in1=tmpa[:], op=ALU.add)
                else:
                    kb.op("dve", "tensor_tensor", [gab], [mT, PB_], out=mT[:, cch, :], in0=PB_[:, 0:512], in1=gab[:, 1, :],
                          op=ALU.mult)
            for s in range(4):
                for half, Pz in enumerate([PZ0, PZ1]):
                    for k in range(8):
                        kb.op("pe", "matmul", [mT, Wo_b], [Pz], Pz[:, 0:512], lhsT=mT[:, k, s * 128:(s + 1) * 128],
                              rhs=Wo_b[:, k, half * 512:(half + 1) * 512], start=(k == 0), stop=(k == 7))
                resid_ln(xs[s], PZ0, PZ1, 0, 0, 1024, ybuf, x1, st, mv, sd, rstd)
                dst = x1s if with_peer else y
                kb.dma("pool", dst.ap((t0 + s * 128) * D, [[D, 128], [1, D]]), x1[:], [x1], [dst])
        kb.barrier()

    if not with_peer:
        kb.wait_all("sp")
        return nc, kb

    kb.mark("D")
    NG = S // 256
    GSZ = 128 * 128 * 256
    GTd = kb.dram("GTd", [NG * 128, 128 * 256], BF16)
    with ExitStack() as es:
        Wq_b = kb.sb("Wq_b", [128, 8, 2048], BF16, es)
        keys_b = kb.sb("keys_b", [128, 16, 128], BF16, es)
        GT = kb.sb("GT", [128, 128 * 128], BF16, es)
        NBUF = 3
        Ubuf = [kb.sb("Ubuf%d" % i, [128, 2, 1024], BF16, es) for i in range(NBUF)]
        Vbuf = [kb.sb("Vbuf%d" % i, [128, 2, 1024], BF16, es) for i in range(NBUF)]
        Gs = [kb.sb("Gs%d" % i, [128, 2, 256], BF16, es) for i in range(NBUF)]
        x1t = [kb.sb("x1t%d" % i, [128, 1024], F32, es) for i in range(2)]
        h2T = kb.sb("h2T", [128, 8, 256], BF16, es)
        qT = kb.sb("qT", [128, 16, 256], BF16, es)
        sc = kb.sb("sc", [128, 4, 128], F32, es)
        scrA = kb.sb("scrA", [128, 2048], F32, es)
        scrB = kb.sb("scrB", [128, 2048], F32, es)
        t16 = kb.sb("t16", [128, 256], F32, es)
        i16 = kb.sb("i16", [128, 256], U32, es)
        i16f = kb.sb("i16f", [128, 256], F32, es)
        tv = kb.sb("tv", [128, 128], F32, es)
        pv = kb.sb("pv", [128, 128], U32, es)
        pvf = kb.sb("pvf", [128, 128], F32, es)
        ee = kb.sb("ee", [128, 128], F32, es)
        zz = kb.sb("zz", [128, 8], F32, es)
        rz = kb.sb("rz", [128, 8], F32, es)
        ak = kb.sb("ak", [128, 128], F32, es)
        bk = kb.sb("bk", [128, 128], F32, es)
        III = kb.sb("III", [128, 384], F32, es)
        ITs = kb.sb("ITs", [128, 384], F32, es)
        iota16 = kb.sb("iota16", [128, 16], F32, es)
        CH = 16
        Lb = kb.sb("Lb", [128, CH * 128], BF16, es)
        Rb = kb.sb("Rb", [128, CH * 128], BF16, es)
        actg = [kb.sb("actg%d" % i, [128, 512], BF16, es) for i in range(2)]
        wd = [kb.sb("wd%d" % i, [128, 512], BF16, es) for i in range(2)]
        ybuf = kb.sb("ybuf2", [128, 1024], F32, es)
        st = kb.sb("st2", [128, 12], F32, es)
        mv = kb.sb("mv2", [128, 2], F32, es)
        sd = kb.sb("sd2", [128, 1], F32, es)
        rstd = kb.sb("rstd2", [128, 1], F32, es)
        PO = [kb.ps("PO%d" % i, [128, 512], F32, es) for i in range(4)]
        PA = [kb.ps("PA%d" % i, [128, 512], F32, es) for i in range(2)]
        PG = [kb.ps("PG%d" % i, [128, 512], F32, es) for i in range(2)]

        kb.dma("pool", Wq_b[:], wslab(peer_wq, 8, 2048, 0, 2048), [peer_wq], [Wq_b])
        kb.dma("pool", keys_b.ap(0, [[1, 2048]]), keysT[:], [keysT], [keys_b])
        kb.op("pool", "iota", [], [iota16], iota16[:], [[1, 16]], base=0, channel_multiplier=0,
              allow_small_or_imprecise_dtypes=True)

        def ln_transpose_f(src, dstT, col0, sc_off, sh_off):
            layernorm_stats(src, st, mv, sd, rstd)
            kb.op("dve", "tensor_scalar", [src, mv, rstd], [scrA], out=scrA[:, 0:1024], in0=src[:], scalar1=mv[:, 0:1],
                  scalar2=rstd[:, 0:1], op0=ALU.subtract, op1=ALU.mult)
            for k in range(8):
                Pp = PA[k // 4]
                kb.op("pe", "transpose", [scrA, ident_f], [Pp], Pp[:, (k % 4) * 128:(k % 4 + 1) * 128],
                      scrA[:, k * 128:(k + 1) * 128], ident_f[:])
            for k in range(8):
                Pp = PA[k // 4]
                kb.op("act", "activation", [modcol], [dstT, Pp], out=dstT[:, k, col0:col0 + 128],
                      in_=Pp[:, (k % 4) * 128:(k % 4 + 1) * 128], func=AF.Identity,
                      bias=modcol[:, sh_off + k:sh_off + k + 1], scale=modcol[:, sc_off + k:sc_off + k + 1])

        for tg in range(NG):
            t0 = tg * 256
            for s in range(2):
                kb.dma("sp", x1t[s][:], x1s.ap((t0 + s * 128) * D, [[D, 128], [1, D]]), [x1s], [x1t[s]])
                ln_transpose_f(x1t[s], h2T, s * 128, 24, 16)
            for cch in range(16):
                Pq = PA[cch % 2]
                for k in range(8):
                    kb.op("pe", "matmul", [h2T, Wq_b], [Pq], Pq[:, 0:256], lhsT=Wq_b[:, k, cch * 128:(cch + 1) * 128],
                          rhs=h2T[:, k, :], start=(k == 0), stop=(k == 7))
                kb.op("act", "copy", [], [qT, Pq], out=qT[:, cch, :], in_=Pq[:, 0:256])
            for s in range(2):
                ts = slice(s * 128, (s + 1) * 128)
                for r4 in range(4):
                    Ps = PG[r4 % 2]
                    for rr in range(4):
                        r = r4 * 4 + rr
                        kb.op("pe", "matmul", [qT, keys_b], [Ps], Ps[:, rr * 128:(rr + 1) * 128], lhsT=qT[:, r, ts],
                              rhs=keys_b[:, r, :], start=True, stop=True)
                    kb.op("act", "copy", [], [sc, Ps], out=sc.ap(0, [[1, 512]]), in_=Ps[:, 0:512])
                    for rr in range(4):
                        r = r4 * 4 + rr
                        kb.op("dve", "max", [sc], [t16], out=t16[:, r * 16:r * 16 + 8], in_=sc[:, rr, :])
                        kb.op("dve", "max_index", [t16, sc], [i16], out=i16[:, r * 16:r * 16 + 8],
                              in_max=t16[:, r * 16:r * 16 + 8], in_values=sc[:, rr, :])
                        kb.op("dve", "match_replace", [t16, sc], [scrA], out=scrA[:, rr * 128:(rr + 1) * 128],
                              in_to_replace=t16[:, r * 16:r * 16 + 8], in_values=sc[:, rr, :], imm_value=-1e30)
                        kb.op("dve", "max", [scrA], [t16], out=t16[:, r * 16 + 8:r * 16 + 16],
                              in_=scrA[:, rr * 128:(rr + 1) * 128])
                        kb.op("dve", "max_index", [t16, scrA], [i16], out=i16[:, r * 16 + 8:r * 16 + 16],
                              in_max=t16[:, r * 16 + 8:r * 16 + 16], in_values=scrA[:, rr * 128:(rr + 1) * 128])
                kb.op("dve", "tensor_copy", [i16], [i16f], out=i16f[:], in_=i16[:])
                kb.op("dve", "tensor_tensor", [t16], [scrB], out=scrB.ap(0, [[256, 8], [16, 16], [1, 16]]),
                      in0=t16.ap(0, [[32, 8], [1, 16], [0, 16]]), in1=t16.ap(16, [[32, 8], [0, 16], [1, 16]]), op=ALU.add)
                for h in range(8):
                    cs_ = slice(h * 256, (h + 1) * 256)
                    kb.op("dve", "max", [scrB], [tv], out=tv[:, h * 16:h * 16 + 8], in_=scrB[:, cs_])
                    kb.op("dve", "max_index", [tv, scrB], [pv], out=pv[:, h * 16:h * 16 + 8], in_max=tv[:, h * 16:h * 16 + 8],
                          in_values=scrB[:, cs_])
                    kb.op("dve", "match_replace", [tv, scrB], [scrA], out=scrA[:, cs_], in_to_replace=tv[:, h * 16:h * 16 + 8],
                          in_values=scrB[:, cs_], imm_value=-1e30)
                    kb.op("dve", "max", [scrA], [tv], out=tv[:, h * 16 + 8:h * 16 + 16], in_=scrA[:, cs_])
                    kb.op("dve", "max_index", [tv, scrA], [pv], out=pv[:, h * 16 + 8:h * 16 + 16],
                          in_max=tv[:, h * 16 + 8:h * 16 + 16], in_values=scrA[:, cs_])
                kb.op("dve", "tensor_tensor", [tv], [ee], out=ee.ap(0, [[16, 8], [1, 16]]), in0=tv.ap(0, [[16, 8], [1, 16]]),
                      in1=tv.ap(0, [[16, 8], [0, 16]]), op=ALU.subtract)
                kb.op("act", "activation", [ee], [ee], out=ee[:], in_=ee[:], func=AF.Exp)
                kb.op("dve", "tensor_reduce", [ee], [zz], out=zz[:], in_=ee.ap(0, [[16, 8], [1, 16]]), axis=AX.X, op=ALU.add)
                kb.op("dve", "reciprocal", [zz], [rz], out=rz[:], in_=zz[:])
                kb.op("dve", "tensor_tensor", [ee, rz], [III], out=III.ap(256, [[16, 8], [1, 16]]),
                      in0=ee.ap(0, [[16, 8], [1, 16]]), in1=rz.ap(0, [[1, 8], [0, 16]]), op=ALU.mult)
                kb.op("dve", "tensor_copy", [pv], [pvf], out=pvf[:], in_=pv[:])
                kb.op("dve", "tensor_tensor", [pvf, thr15], [scrA], out=scrA.ap(0, [[15, 128], [1, 15]]),
                      in0=pvf.ap(0, [[1, 128], [0, 15]]), in1=thr15.ap(0, [[0, 128], [1, 15]]), op=ALU.is_ge)
                kb.op("dve", "tensor_reduce", [scrA], [ak], out=ak[:], in_=scrA.ap(0, [[15, 128], [1, 15]]), axis=AX.X, op=ALU.add)
                kb.op("dve", "scalar_tensor_tensor", [ak, pvf], [bk], out=bk[:], in0=ak[:], scalar=-16.0, in1=pvf[:],
                      op0=ALU.mult, op1=ALU.add)
                for which, (sel, off) in enumerate([(ak, 0), (bk, 16)]):
                    kb.op("dve", "tensor_tensor", [iota16, sel], [scrA], out=scrA.ap(0, [[256, 8], [16, 16], [1, 16]]),
                          in0=iota16.ap(0, [[0, 8], [0, 16], [1, 16]]), in1=sel.ap(0, [[16, 8], [1, 16], [0, 16]]),
                          op=ALU.is_equal)
                    kb.op("dve", "tensor_tensor", [scrA, i16f], [scrB], out=scrB.ap(0, [[256, 8], [16, 16], [1, 16]]),
                          in0=scrA.ap(0, [[256, 8], [16, 16], [1, 16]]), in1=i16f.ap(off, [[32, 8], [0, 16], [1, 16]]),
                          op=ALU.mult)
                    kb.op("dve", "tensor_reduce", [scrB], [III], out=III.ap(which * 128, [[16, 8], [1, 16]]),
                          in_=scrB.ap(0, [[256, 8], [16, 16], [1, 16]]), axis=AX.X, op=ALU.add)
                for i3 in range(3):
                    kb.op("pe", "transpose", [III, ident_f], [PG[0]], PG[0][:, i3 * 128:(i3 + 1) * 128],
                          III[:, i3 * 128:(i3 + 1) * 128], ident_f[:])
                kb.op("act", "copy", [], [ITs, PG[0]], out=ITs[:], in_=PG[0][:, 0:384])
                for ch in range(128 // CH):
                    kb.op("dve", "tensor_tensor", [iota128, ITs], [Lb], out=Lb.ap(0, [[128, CH], [1, 128]]),
                          in0=iota128.ap(0, [[0, CH], [1, 128]]), in1=ITs.ap(ch * CH, [[1, CH], [0, 128]]), op=ALU.is_equal)
                    kb.op("dve", "tensor_tensor", [iota128, ITs], [Rb], out=Rb.ap(0, [[128, CH], [1, 128]]),
                          in0=iota128.ap(0, [[0, CH], [1, 128]]), in1=ITs.ap(128 + ch * CH, [[1, CH], [0, 128]]), op=ALU.is_equal)
                    kb.op("dve", "tensor_tensor", [Rb, ITs], [Rb], out=Rb.ap(0, [[128, CH], [1, 128]]),
                          in0=Rb.ap(0, [[128, CH], [1, 128]]), in1=ITs.ap(256 + ch * CH, [[1, CH], [0, 128]]), op=ALU.mult)
                    for t4 in range(CH // 4):
                        Pg = PG[t4 % 2]
                        for tt in range(4):
                            tl = t4 * 4 + tt
                            kb.op("pe", "matmul", [Lb, Rb], [Pg], Pg[:, tt * 128:(tt + 1) * 128], lhsT=Lb[:, tl * 128:(tl + 1) * 128],
                                  rhs=Rb[:, tl * 128:(tl + 1) * 128], start=True, stop=True)
                        tokb = ch * CH + t4 * 4
                        eng = "act" if t4 % 2 == 0 else "dve"
                        fn = "copy" if eng == "act" else "tensor_copy"
                        kb.op(eng, fn, [], [GT, Pg], out=GT.ap(tokb, [[1, 4], [128, 128]]), in_=Pg.ap(0, [[128, 4], [1, 128]]))
                for jb in range(8):
                    kb.dma("pool", GTd.ap(tg * GSZ + jb * 16 * 256 + s * 128, [[128 * 256, 128], [256, 16], [1, 128]]),
                           GT.ap(jb * 16 * 128, [[128, 16], [1, 128]]), [GT], [GTd])
            for jp in range(64):
                bi = jp % NBUF
                j0 = jp * 2
                kb.dma("sp", Ubuf[bi].ap(0, [[1024, 2], [1, 1024]]), UTb.ap(j0 * 131072, [[1024, 128], [131072, 2], [1, 1024]]),
                       [UTb], [Ubuf[bi]])
                kb.dma("act", Vbuf[bi].ap(0, [[1024, 2], [1, 1024]]), VJb.ap(j0 * 131072, [[1024, 128], [131072, 2], [1, 1024]]),
                       [VJb], [Vbuf[bi]])
                kb.dma("sp", Gs[bi].ap(0, [[256, 2], [1, 256]]), GTd.ap(tg * GSZ + j0 * 256, [[128 * 256, 128], [256, 2], [1, 256]]),
                       [GTd], [Gs[bi]])
                Pa = PA[jp % 2]
                for jj in range(2):
                    for k in range(8):
                        kb.op("pe", "matmul", [h2T, Ubuf[bi]], [Pa], Pa[:, jj * 256:(jj + 1) * 256],
                              lhsT=Ubuf[bi][:, jj, k * 128:(k + 1) * 128], rhs=h2T[:, k, :], start=(k == 0), stop=(k == 7))
                ag = actg[jp % 2]
                wdd = wd[jp % 2]
                kb.op("act", "activation", [], [ag, Pa], out=ag[:], in_=Pa[:, 0:512], func=AF.Gelu)
                kb.op("dve", "tensor_tensor", [ag, Gs[bi]], [wdd], out=wdd[:], in0=ag[:], in1=Gs[bi].ap(0, [[1, 512]]), op=ALU.mult)
                for jj in range(2):
                    j = j0 + jj
                    for s in range(2):
                        for half in range(2):
                            Pp = PO[s * 2 + half]
                            kb.op("pe", "matmul", [wdd, Vbuf[bi]], [Pp], Pp[:, 0:512],
                                  lhsT=wdd[:, jj * 256 + s * 128:jj * 256 + (s + 1) * 128],
                                  rhs=Vbuf[bi][:, jj, half * 512:(half + 1) * 512], start=(j == 0), stop=(j == 127))
            for s in range(2):
                resid_ln(x1t[s], PO[s * 2], PO[s * 2 + 1], 1024, 2048, 3072, ybuf, ybuf, st, mv, sd, rstd)
                kb.dma("pool", y.ap((t0 + s * 128) * D, [[D, 128], [1, D]]), ybuf[:], [ybuf], [y])
        kb.barrier()
    kb.mark("end")
    kb.wait_all("sp")
    kb.wait_all("pool")
    return nc, kb


def prep_shared(inp):
    f = lambda a: np.ascontiguousarray(a, dtype=np.float32)
    sh = {}
    sh["w_ada"] = f(inp["w_ada"][0])
    sh["b_ada"] = f(inp["b_ada"][0][None, :])
    sh["rows"] = f(np.concatenate([inp["ln1_g"][0], inp["ln1_b"][0], inp["ln2_g"][0], inp["ln2_b"][0],
                                   inp["sgu_ln_g"][0], inp["sgu_ln_b"][0]])[None, :])
    w_in = inp["w_in"][0]
    sh["wz"] = f(w_in[:, 1304:2328])
    sh["w_merge"] = f(inp["w_merge"][0])
    sh["b_merge_col"] = f(inp["b_merge"][0].reshape(16, 128).T)
    sh["w_b1"] = f(inp["w_branch"][0, 1])
    sh["w_out"] = f(inp["w_out"][0])
    sh["wsT"] = f(inp["sgu_w"][0].transpose(2, 0, 1).reshape(128, 1024))
    sh["sgub_col"] = f(inp["sgu_b"][0].T)
    jj, ii = np.meshgrid(np.arange(128), np.arange(128), indexing="ij")
    sh["trilm"] = f((jj <= ii).astype(np.float32))
    sh["peer_wq"] = f(inp["peer_wq"][0])
    sh["keysT"] = f(inp["peer_keys"][0].reshape(16, 128, 128).transpose(2, 0, 1).reshape(128, 2048))
    pu = inp["peer_u"][0].reshape(128, 128, 8, 128)
    sh["UT"] = f(pu.transpose(1, 3, 2, 0).reshape(128, 128, 1024))
    pv = inp["peer_v"][0].reshape(128, 128, 1024)
    sh["VJ"] = f(pv.transpose(1, 0, 2))
    def swp(c):
        blocks = [np.concatenate([c[:, i * 64 + 32:(i + 1) * 64], c[:, i * 64:i * 64 + 32]], axis=1) for i in range(c.shape[1] // 64)]
        return np.concatenate(blocks, axis=1)
    qch = [np.concatenate([w_in[:, r * 64:(r + 1) * 64], w_in[:, (4 + r) * 64:(5 + r) * 64]], axis=1) for r in range(4)]
    kcs = [w_in[:, 512:640], w_in[:, 768:896], w_in[:, 1024:1152]]
    chunks = qch + [swp(c) for c in qch] + kcs + [swp(c) for c in kcs] + [w_in[:, 640:768]]
    sh["WAf"] = f(np.concatenate(chunks, axis=1))
    sh["WAt"] = f(np.concatenate([w_in[:, 896:1024], w_in[:, 1152:1280], w_in[:, 1280:1304]], axis=1))
    dup = lambda a: np.concatenate([a, a], axis=0)
    w1 = inp["cmp_w1"][0]
    sh["w1d"] = f(np.stack([dup(w1[kv].reshape(32, 64, 128).transpose(1, 0, 2).reshape(64, 4096)) for kv in range(2)]))
    pos = inp["cmp_pos"][0]
    sh["posTd"] = f(dup(np.concatenate([pos[0].T, pos[1].T], axis=1)))
    sh["b1col"] = f(inp["cmp_b1"][0].T)
    w2 = inp["cmp_w2"][0]
    sh["w2kd"] = f(np.concatenate([w2[0], w2[0]], axis=1))
    sh["w2v"] = f(w2[1])
    sh["b2kcol"] = f(dup(inp["cmp_b2"][0][0][:, None]))
    sh["b2vrow"] = f(inp["cmp_b2"][0][1][None, :])
    sh["w_b0"] = f(inp["w_branch"][0, 0])
    return sh


def prep_consts(S):
    f = lambda a: np.ascontiguousarray(a, dtype=np.float32)
    NEG = -30000.0
    cst = {}
    p = np.arange(128)
    d = p % 64
    inv_freq = (np.float32(10000.0) ** (-(np.arange(32, dtype=np.float32)) / np.float32(32))).astype(np.float32)
    ang = (np.arange(S, dtype=np.float32)[None, :] * inv_freq[d % 32][:, None]).astype(np.float32)
    cst["cosT"] = f(np.cos(ang))
    sgn = np.where(d < 32, -1.0, 1.0).astype(np.float32)[:, None]
    cst["sinS"] = f(np.sin(ang) * sgn)
    ncmp = (S - 32) // 16 + 1
    nsel = S // 64
    c0 = np.arange(ncmp)[:, None] * 16
    s0 = np.arange(nsel)[None, :] * 64
    ov = np.clip(np.minimum(c0 + 32, s0 + 64) - np.maximum(c0, s0), 0, None) / 32.0
    agg = np.zeros((512, 128), np.float32)
    agg[:ncmp, :nsel] = ov
    cst["aggd"] = f(agg.reshape(4, 128, 128).transpose(1, 0, 2).reshape(128, 512))
    nl = np.arange(128)[:, None, None]
    m = np.arange(16)[None, :, None]
    ql = np.arange(128)[None, None, :]
    cst["cmpb"] = f(np.where(16 * nl + 31 - ql <= 128 * m, 0.0, NEG).reshape(128, 2048))
    kk = np.arange(128)[:, None]
    qq = np.arange(128)[None, :]
    cst["caus"] = f(np.concatenate([np.where(kk <= qq, 0.0, NEG), np.where(kk > qq, 0.0, NEG)], axis=1))
    q = np.arange(128)[:, None]
    c = np.arange(254)[None, :]
    dd = c - 126 - (q >= 64)
    base = np.where(dd > 0, -1e30, np.where(dd == 0, 2e9, np.where(dd == -1, 1e9, 0.0)))
    j0 = np.zeros((128, 128))
    j0[:, 0] = 3e9
    cst["based"] = f(np.concatenate([base, j0], axis=1))
    key = np.arange(S)[None, :]
    cst["ebig"] = f((key // 64 == np.arange(128)[:, None]).astype(np.float32))
    return cst


def kernel(**inputs):
    B, S = inputs["x"].shape[0], inputs["x"].shape[1]
    nc, kb = build(S)
    sh = prep_shared(inputs)
    sh.update(prep_consts(S))
    in_maps = []
    for b in range(B):
        m = dict(sh)
        m["x"] = np.ascontiguousarray(inputs["x"][b], dtype=np.float32)
        m["c_col"] = np.ascontiguousarray(inputs["c"][b].reshape(8, 128).T, dtype=np.float32)
        in_maps.append(m)
    res = run_bass_kernel_spmd(nc, in_maps, core_ids=list(range(B)))
    return np.stack([np.asarray(r["y"], dtype=np.float32) for r in res.results], axis=0)
```

```python
import numpy as np
import concourse.bass as bass
import concourse.mybir as mybir
from concourse.bass_utils import run_bass_kernel_spmd
from contextlib import ExitStack

F32 = mybir.dt.float32
BF16 = mybir.dt.bfloat16
U32 = mybir.dt.uint32
AF = mybir.ActivationFunctionType
ALU = mybir.AluOpType
AX = mybir.AxisListType

D = 1024
ALPHA = 2.0 ** 0.25
EPS = 1e-5


class Sem:
    def __init__(self, h, name):
        self.h = h
        self.name = name
        self.count = 0


class T:
    def __init__(self, t, name, shape, dt, space):
        self.t = t
        self.name = name
        self.shape = list(shape)
        self.dt = dt
        self.space = space
        self.w = {}
        self.r = {}
        self.dsem = None
        self.fsize = int(np.prod(shape[1:]))

    def __getitem__(self, idx):
        return self.t[idx]

    def ap(self, off, dims, parts=None, pstart=0):
        if self.space == "dram":
            return bass.AP(self.t, off, [list(d) for d in dims])
        if parts is None:
            parts = self.shape[0]
        return bass.AP(self.t, pstart * self.fsize + off, [[self.fsize, parts]] + [list(d) for d in dims])


class KB:
    def __init__(self, nc):
        self.nc = nc
        self.es = ExitStack()
        self.engs = {"pe": nc.tensor, "act": nc.scalar, "dve": nc.vector, "pool": nc.gpsimd, "sp": nc.sync}
        self.esem = {}
        self.waited = {k: {} for k in self.engs}
        self.all_sems = []
        for k in self.engs:
            self.esem[k] = self.new_sem("e_" + k)
        self.n_instr = 0
        self.n_wait = 0
        self.marks = []

    def new_sem(self, name):
        h = self.es.enter_context(self.nc.semaphore(name))
        s = Sem(h, name)
        self.all_sems.append(s)
        return s

    def sb(self, name, shape, dt, es=None):
        t = (es or self.es).enter_context(self.nc.sbuf_tensor(name, list(shape), dt))
        return T(t, name, shape, dt, "sb")

    def ps(self, name, shape, dt, es=None):
        t = (es or self.es).enter_context(self.nc.psum_tensor(name, list(shape), dt))
        return T(t, name, shape, dt, "ps")

    def dram(self, name, shape, dt, kind=None):
        if kind is None:
            t = self.nc.dram_tensor(name, list(shape), dt)
        else:
            t = self.nc.dram_tensor(name, list(shape), dt, kind=kind)
        return T(t, name, shape, dt, "dram")

    def _wait(self, e, sem, val):
        if val <= 0:
            return
        w = self.waited[e]
        if w.get(sem, 0) >= val:
            return
        self.engs[e].wait_ge(sem.h, val)
        w[sem] = val
        self.n_wait += 1

    def _deps(self, e, reads, writes):
        mysem = self.esem[e]
        for b in reads:
            for s, v in b.w.items():
                if s is mysem and e == "pe":
                    continue
                self._wait(e, s, v)
        for b in writes:
            for s, v in b.w.items():
                if s is mysem and e == "pe":
                    continue
                self._wait(e, s, v)
            for s, v in b.r.items():
                if s is mysem and e == "pe":
                    continue
                self._wait(e, s, v)

    def op(self, e, fn, reads, writes, *a, **kw):
        self._deps(e, reads, writes)
        ins = getattr(self.engs[e], fn)(*a, **kw)
        s = self.esem[e]
        s.count += 1
        ins.then_inc(s.h, 1)
        for b in reads:
            b.r[s] = s.count
        for b in writes:
            b.w[s] = s.count
        self.n_instr += 1
        return ins

    def dma(self, q, out_ap, in_ap, reads, writes, sem=None, **kw):
        if sem is None:
            tgt = writes[0]
            if tgt.dsem is None:
                tgt.dsem = self.new_sem("d_" + tgt.name)
            sem = tgt.dsem
        self._deps(q, reads, writes)
        ins = self.engs[q].dma_start(out=out_ap, in_=in_ap, **kw)
        sem.count += 16
        ins.then_inc(sem.h, 16)
        for b in reads:
            b.r[sem] = sem.count
        for b in writes:
            b.w[sem] = sem.count
        self.n_instr += 1
        return ins

    def mark(self, name):
        self.marks.append((name, self.n_instr, self.n_wait))

    def barrier(self):
        for e in self.engs:
            for s in self.all_sems:
                self._wait(e, s, s.count)

    def wait_all(self, e):
        for s in self.all_sems:
            self._wait(e, s, s.count)


def build(S, with_peer=True, with_nsa=True, stop_after=None):
    nc = bass.Bass("TRN2", target_bir_lowering=False)
    kb = KB(nc)
    NS = S // 128

    def din(name, shape, dt=F32):
        return kb.dram(name, shape, dt, kind="ExternalInput")

    x = din("x", [S, D])
    c_col = din("c_col", [128, 8])
    w_ada = din("w_ada", [1024, 6144])
    b_ada = din("b_ada", [1, 6144])
    rows = din("rows", [1, 5120])
    wz = din("wz", [1024, 1024])
    w_merge = din("w_merge", [1024, 2048])
    b_merge_col = din("b_merge_col", [128, 16])
    w_b1 = din("w_b1", [512, 1024])
    w_out = din("w_out", [1024, 1024])
    wsT = din("wsT", [128, 1024])
    sgub_col = din("sgub_col", [128, 8])
    trilm = din("trilm", [128, 128])
    peer_wq = din("peer_wq", [1024, 2048])
    keysT = din("keysT", [128, 2048])
    UT = din("UT", [128, 128, 1024])
    VJ = din("VJ", [128, 128, 1024])
    y = kb.dram("y", [S, D], F32, kind="ExternalOutput")
    x1s = kb.dram("x1s", [S, D], F32)
    UTb = kb.dram("UTb", [128, 128, 1024], BF16)
    VJb = kb.dram("VJb", [128, 128, 1024], BF16)
    NEG = -30000.0
    if with_nsa:
        WAf = din("WAf", [1024, 1920])
        WAt = din("WAt", [1024, 280])
        cosT = din("cosT", [128, S])
        sinS = din("sinS", [128, S])
        w1d = din("w1d", [2, 128, 4096])
        posTd = din("posTd", [128, 64])
        b1col = din("b1col", [128, 2])
        w2kd = din("w2kd", [128, 128])
        w2v = din("w2v", [128, 64])
        b2kcol = din("b2kcol", [128, 1])
        b2vrow = din("b2vrow", [1, 64])
        aggd = din("aggd", [128, 512])
        cmpb = din("cmpb", [128, 2048])
        caus = din("caus", [128, 256])
        based = din("based", [128, 382])
        ebig = din("ebig", [128, S])
        w_b0 = din("w_b0", [512, 1024])
        QTd = kb.dram("QTd", [128, 4 * S], BF16)
        ONd = kb.dram("ONd", [128, 4 * S], BF16)

    ident_f = kb.sb("ident_f", [128, 128], F32)
    ident_b = kb.sb("ident_b", [128, 128], BF16)
    ones_row = kb.sb("ones_row", [1, 128], F32)
    eps_col = kb.sb("eps_col", [128, 1], F32)
    modcol = kb.sb("modcol", [128, 32], F32)
    gt_bc = kb.sb("gt_bc", [128, 2048], F32)
    ln_bc = kb.sb("ln_bc", [128, 5120], F32)
    iota128 = kb.sb("iota128", [128, 128], F32)
    thr15 = kb.sb("thr15", [128, 15], F32)

    kb.op("pool", "memset", [], [ident_f], ident_f[:], 0.0)
    kb.op("pool", "affine_select", [ident_f], [ident_f], out=ident_f[:], in_=ident_f[:], pattern=[[-1, 128]],
          compare_op=ALU.not_equal, fill=1.0, base=0, channel_multiplier=1)
    kb.op("dve", "tensor_copy", [ident_f], [ident_b], out=ident_b[:], in_=ident_f[:])
    kb.op("pool", "memset", [], [ones_row], ones_row[:], 1.0)
    kb.op("pool", "memset", [], [eps_col], eps_col[:], EPS)
    kb.op("pool", "iota", [], [iota128], iota128[:], [[1, 128]], base=0, channel_multiplier=0,
          allow_small_or_imprecise_dtypes=True)
    kb.op("pool", "iota", [], [thr15], thr15[:], [[16, 15]], base=16, channel_multiplier=0,
          allow_small_or_imprecise_dtypes=True)

    def wslab(src, k, n, c0, w):
        return src.ap(c0, [[n, 128], [128 * n, k], [1, w]])

    kb.mark("P")
    with ExitStack() as es:
        wa = [kb.sb("wa%d" % i, [128, 8, 512], F32, es) for i in range(2)]
        mod_row = kb.sb("mod_row", [1, 6144], F32, es)
        b_row = kb.sb("b_row", [1, 6144], F32, es)
        rows_sb = kb.sb("rows_sb", [1, 5120], F32, es)
        ccol = kb.sb("ccol", [128, 8], F32, es)
        scol = kb.sb("scol", [128, 8], F32, es)
        P0 = kb.ps("pP0", [128, 512], F32, es)
        P1 = kb.ps("pP1", [128, 512], F32, es)
        kb.dma("sp", ccol[:], c_col[:], [c_col], [ccol])
        kb.dma("sp", b_row[:], b_ada[:], [b_ada], [b_row])
        kb.dma("sp", rows_sb[:], rows[:], [rows], [rows_sb])
        kb.op("act", "activation", [ccol], [scol], out=scol[:], in_=ccol[:], func=AF.Silu)
        for blk in range(12):
            wt = wa[blk % 2]
            kb.dma("sp", wt[:], wslab(w_ada, 8, 6144, blk * 512, 512), [w_ada], [wt])
            Pb = P0 if blk % 2 == 0 else P1
            for k in range(8):
                kb.op("pe", "matmul", [scol, wt], [Pb], Pb[0:1, 0:512], lhsT=scol[:, k:k + 1], rhs=wt[:, k, :],
                      start=(k == 0), stop=(k == 7))
            kb.op("dve", "tensor_tensor", [b_row], [mod_row, Pb], out=mod_row[0:1, blk * 512:(blk + 1) * 512],
                  in0=Pb[0:1, 0:512], in1=b_row[0:1, blk * 512:(blk + 1) * 512], op=ALU.add)
        for i, c0 in enumerate([2048, 2560, 5120, 5632]):
            Pb = P0 if i % 2 == 0 else P1
            kb.op("pe", "matmul", [ones_row, mod_row], [Pb], Pb[:, 0:512], lhsT=ones_row[0:1, :],
                  rhs=mod_row[0:1, c0:c0 + 512], start=True, stop=True)
            kb.op("act", "copy", [], [gt_bc, Pb], out=gt_bc[:, i * 512:(i + 1) * 512], in_=Pb[:, 0:512])
        for i in range(10):
            Pb = P0 if i % 2 == 0 else P1
            kb.op("pe", "matmul", [ones_row, rows_sb], [Pb], Pb[:, 0:512], lhsT=ones_row[0:1, :],
                  rhs=rows_sb[0:1, i * 512:(i + 1) * 512], start=True, stop=True)
            kb.op("act", "copy", [], [ln_bc, Pb], out=ln_bc[:, i * 512:(i + 1) * 512], in_=Pb[:, 0:512])
        chunks = list(range(0, 8)) + list(range(8, 16)) + list(range(24, 32)) + list(range(32, 40))
        for j, cch in enumerate(chunks):
            kb.op("pe", "matmul", [ones_row, mod_row], [P0], P0[:, j:j + 1], lhsT=mod_row[0:1, cch * 128:(cch + 1) * 128],
                  rhs=ones_row[0:1, 0:1], start=True, stop=True)
        kb.op("dve", "tensor_copy", [], [modcol, P0], out=modcol[:], in_=P0[:, 0:32])
        kb.op("dve", "tensor_scalar", [modcol], [modcol], out=modcol[:, 8:16], in0=modcol[:, 8:16], scalar1=1.0,
              scalar2=None, op0=ALU.add)
        kb.op("dve", "tensor_scalar", [modcol], [modcol], out=modcol[:, 24:32], in0=modcol[:, 24:32], scalar1=1.0,
              scalar2=None, op0=ALU.add)
        kb.barrier()

    def layernorm_stats(src, st, mv, sd, rstd):
        kb.op("dve", "bn_stats", [src], [st], out=st[:, 0:6], in_=src[:, 0:512])
        kb.op("dve", "bn_stats", [src], [st], out=st[:, 6:12], in_=src[:, 512:1024])
        kb.op("dve", "bn_aggr", [st], [mv], out=mv[:, 0:2], in_=st[:, 0:12])
        kb.op("act", "activation", [mv, eps_col], [sd], out=sd[:, 0:1], in_=mv[:, 1:2], func=AF.Sqrt,
              bias=eps_col[:, 0:1], scale=1.0)
        kb.op("dve", "reciprocal", [sd], [rstd], out=rstd[:, 0:1], in_=sd[:, 0:1])

    def ln_transpose(src, xn_b, PT, dstT, col0, ncols, sc_off, sh_off, st, mv, sd, rstd):
        layernorm_stats(src, st, mv, sd, rstd)
        kb.op("dve", "tensor_scalar", [src, mv, rstd], [xn_b], out=xn_b[:], in0=src[:], scalar1=mv[:, 0:1],
              scalar2=rstd[:, 0:1], op0=ALU.subtract, op1=ALU.mult)
        for k in range(8):
            kb.op("pe", "transpose", [xn_b, ident_b], [PT], PT[:, k * 128:(k + 1) * 128], xn_b[:, k * 128:(k + 1) * 128],
                  ident_b[:])
        for k in range(8):
            kb.op("act", "activation", [modcol], [dstT, PT], out=dstT[:, k, col0:col0 + 128],
                  in_=PT[:, k * 128:(k + 1) * 128], func=AF.Identity, bias=modcol[:, sh_off + k:sh_off + k + 1],
                  scale=modcol[:, sc_off + k:sc_off + k + 1])

    def resid_ln(xsrc, Pa, Pb, gt_off, g_off, b_off, ybuf, obuf, st, mv, sd, rstd):
        for half, Pp in enumerate([Pa, Pb]):
            kb.op("dve", "tensor_tensor", [gt_bc], [ybuf, Pp], out=ybuf[:, half * 512:(half + 1) * 512], in0=Pp[:, 0:512],
                  in1=gt_bc[:, gt_off + half * 512:gt_off + (half + 1) * 512], op=ALU.mult)
        kb.op("dve", "scalar_tensor_tensor", [xsrc, ybuf], [ybuf], out=ybuf[:], in0=xsrc[:], scalar=ALPHA, in1=ybuf[:],
              op0=ALU.mult, op1=ALU.add)
        layernorm_stats(ybuf, st, mv, sd, rstd)
        kb.op("dve", "scalar_tensor_tensor", [ybuf, mv, ln_bc], [obuf], out=obuf[:], in0=ybuf[:], scalar=mv[:, 0:1],
              in1=ln_bc[:, g_off:g_off + 1024], op0=ALU.subtract, op1=ALU.mult)
        kb.op("dve", "scalar_tensor_tensor", [obuf, rstd, ln_bc], [obuf], out=obuf[:], in0=obuf[:], scalar=rstd[:, 0:1],
              in1=ln_bc[:, b_off:b_off + 1024], op0=ALU.mult, op1=ALU.add)


    kb.mark("A")
    if with_nsa:
        NQ = S // 128
        es1 = ExitStack()
        KsT = kb.sb("KsT", [128, S], BF16, es1)
        KwT = kb.sb("KwT", [128, S], BF16, es1)
        VS = kb.sb("VS", [128, NQ * 130], BF16, es1)
        VW = kb.sb("VW", [128, NQ * 130], BF16, es1)
        Gt = kb.sb("Gt", [128, NQ * 24], F32, es1)
        KCT = kb.sb("KCT", [128, 512], BF16, es1)
        VC = kb.sb("VC", [128, 4 * 130], BF16, es1)
        kb.op("pool", "memset", [], [VS], VS[:], 1.0)
        kb.op("pool", "memset", [], [VW], VW[:], 1.0)
        kb.op("pool", "memset", [], [VC], VC[:], 1.0)
        kb.op("pool", "memset", [], [KCT], KCT[:], 0.0)
        with ExitStack() as es:
            KcR = kb.sb("KcR", [128, S], BF16, es)
            VcR = kb.sb("VcR", [128, S], BF16, es)
            xs1 = kb.sb("xsA", [128, 1024], F32, es)
            xn_b = kb.sb("xn_bA", [128, 1024], BF16, es)
            hT = kb.sb("hTA", [128, 8, 512], BF16, es)
            WAt_b = kb.sb("WAt_b", [128, 8, 280], BF16, es)
            wch = [kb.sb("wch%d" % i, [128, 8, 128], BF16, es) for i in range(4)]
            cs = kb.sb("cs", [128, 512], F32, es)
            sn = kb.sb("sn", [128, 512], F32, es)
            t1 = kb.sb("t1", [128, 512], F32, es)
            t2 = kb.sb("t2", [128, 512], F32, es)
            qtmp = [kb.sb("qtmp%d" % i, [128, 512], BF16, es) for i in range(2)]
            st = kb.sb("stA", [128, 12], F32, es)
            mv = kb.sb("mvA", [128, 2], F32, es)
            sd = kb.sb("sdA", [128, 1], F32, es)
            rstd = kb.sb("rstdA", [128, 1], F32, es)
            w1_b = [kb.sb("w1_b%d" % i, [128, 4096], BF16, es) for i in range(2)]
            posT_b = kb.sb("posT_b", [128, 64], BF16, es)
            b1c = kb.sb("b1c", [128, 2], F32, es)
            w2k_b = kb.sb("w2k_b", [128, 128], BF16, es)
            w2v_b = kb.sb("w2v_b", [128, 64], BF16, es)
            b2kc = kb.sb("b2kc", [128, 1], F32, es)
            b2v_b = kb.sb("b2v_b", [1, 64], BF16, es)
            ones_b = kb.sb("ones_b", [1, 128], BF16, es)
            hidT = kb.sb("hidT", [128, 512], BF16, es)
            biasv = kb.sb("biasv", [128, 1], F32, es)
            PT = kb.ps("PTA", [128, 1024], BF16, es)
            Pq = kb.ps("Pq", [128, 512], F32, es)
            Pqs = kb.ps("Pqs", [128, 512], F32, es)
            PV = kb.ps("PV", [128, 512], F32, es)

            kb.dma("pool", WAt_b[:], wslab(WAt, 8, 280, 0, 280), [WAt], [WAt_b])
            nw = 0
            for tg in range(S // 512):
                t0 = tg * 512
                for s in range(4):
                    kb.dma("sp", xs1[:], x.ap((t0 + s * 128) * D, [[D, 128], [1, D]]), [x], [xs1])
                    ln_transpose(xs1, xn_b, PT, hT, s * 128, 128, 8, 0, st, mv, sd, rstd)
                kb.dma("sp", cs[:], cosT.ap(t0, [[S, 128], [1, 512]]), [cosT], [cs])
                kb.dma("sp", sn[:], sinS.ap(t0, [[S, 128], [1, 512]]), [sinS], [sn])
                for s in range(4):
                    tile = tg * 4 + s
                    for k in range(8):
                        kb.op("pe", "matmul", [hT, WAt_b], [PV], PV[:, 0:280], lhsT=hT[:, k, s * 128:(s + 1) * 128],
                              rhs=WAt_b[:, k, :], start=(k == 0), stop=(k == 7))
                    kb.op("act", "copy", [], [VS, PV], out=VS.ap(tile * 130, [[65, 2], [1, 64]]),
                          in_=PV.ap(0, [[64, 2], [1, 64]]))
                    kb.op("act", "copy", [], [VW, PV], out=VW.ap(tile * 130, [[65, 2], [1, 64]]),
                          in_=PV.ap(128, [[64, 2], [1, 64]]))
                    kb.op("act", "activation", [], [Gt, PV], out=Gt.ap(tile * 24, [[1, 8], [8, 3]]),
                          in_=PV.ap(256, [[3, 8], [1, 3]]), func=AF.Sigmoid)
                jobs = [(c, c + 4, "q", c) for c in range(4)] + [(8, 11, "kc", 0), (9, 12, "ks", 0), (10, 13, "kw", 0)]
                for ji, (ca, cb_, kind, r) in enumerate(jobs):
                    wa_ = wch[nw % 4]
                    wb_ = wch[(nw + 1) % 4]
                    nw += 2
                    kb.dma("pool", wa_[:], wslab(WAf, 8, 1920, ca * 128, 128), [WAf], [wa_])
                    kb.dma("pool", wb_[:], wslab(WAf, 8, 1920, cb_ * 128, 128), [WAf], [wb_])
                    for (wt_, Pp) in ((wa_, Pq), (wb_, Pqs)):
                        for k in range(8):
                            kb.op("pe", "matmul", [hT, wt_], [Pp], Pp[:, 0:512], lhsT=wt_[:, k, :], rhs=hT[:, k, :],
                                  start=(k == 0), stop=(k == 7))
                    kb.op("dve", "tensor_tensor", [cs], [t1, Pq], out=t1[:], in0=Pq[:, 0:512], in1=cs[:], op=ALU.mult)
                    kb.op("dve", "tensor_tensor", [sn], [t2, Pqs], out=t2[:], in0=Pqs[:, 0:512], in1=sn[:], op=ALU.mult)
                    if kind == "q":
                        qb = qtmp[ji % 2]
                        kb.op("dve", "tensor_tensor", [t1, t2], [qb], out=qb[:], in0=t1[:], in1=t2[:], op=ALU.add)
                        kb.dma("sp", QTd.ap(r * S + t0, [[4 * S, 128], [1, 512]]), qb[:], [qb], [QTd])
                    else:
                        dstT = {"kc": KcR, "ks": KsT, "kw": KwT}[kind]
                        kb.op("dve", "tensor_tensor", [t1, t2], [dstT], out=dstT[:, t0:t0 + 512], in0=t1[:], in1=t2[:],
                              op=ALU.add)
                wv_ = wch[nw % 4]
                nw += 1
                kb.dma("pool", wv_[:], wslab(WAf, 8, 1920, 14 * 128, 128), [WAf], [wv_])
                for k in range(8):
                    kb.op("pe", "matmul", [hT, wv_], [Pq], Pq[:, 0:512], lhsT=wv_[:, k, :], rhs=hT[:, k, :],
                          start=(k == 0), stop=(k == 7))
                kb.op("act", "copy", [], [VcR, Pq], out=VcR[:, t0:t0 + 512], in_=Pq[:, 0:512])

            kb.mark("B")
            if S >= 512:
                ncmp = (S - 32) // 16 + 1
                for kv in range(2):
                    kb.dma("pool", w1_b[kv][:], w1d.ap(kv * 128 * 4096, [[4096, 128], [1, 4096]]), [w1d], [w1_b[kv]])
                kb.dma("pool", posT_b[:], posTd[:], [posTd], [posT_b])
                kb.dma("sp", b1c[:], b1col[:], [b1col], [b1c])
                kb.dma("pool", w2k_b[:], w2kd[:], [w2kd], [w2k_b])
                kb.dma("pool", w2v_b[:], w2v[:], [w2v], [w2v_b])
                kb.dma("sp", b2kc[:], b2kcol[:], [b2kcol], [b2kc])
                kb.dma("pool", b2v_b[:], b2vrow[:], [b2vrow], [b2v_b])
                kb.op("pool", "memset", [], [ones_b], ones_b[:], 1.0)
                kb.op("pool", "memset", [], [hidT], hidT[:], 0.0)
                nc_pad = min(ncmp, 511)
                for kv in range(2):
                    raw = KcR if kv == 0 else VcR
                    for p in range(32):
                        kb.op("pe", "matmul", [w1_b[kv], posT_b], [PV], PV[:, 0:1],
                              lhsT=w1_b[kv].ap(p * 128, [[1, 128]], parts=64), rhs=posT_b.ap(kv * 32 + p, [[1, 1]], parts=64),
                              start=(p == 0), stop=(p == 31))
                    kb.op("dve", "tensor_tensor", [b1c], [biasv, PV], out=biasv[:], in0=PV[:, 0:1], in1=b1c[:, kv:kv + 1],
                          op=ALU.add)
                    for g in range(2):
                        for p in range(32):
                            kb.op("pe", "matmul", [w1_b[kv], raw], [Pq], Pq[:, 0:nc_pad],
                                  lhsT=w1_b[kv].ap(p * 128, [[1, 128]], parts=64, pstart=g * 64),
                                  rhs=raw.ap(p, [[16, nc_pad]], parts=64, pstart=g * 64), start=(p == 0), stop=(p == 31))
                        kb.op("act", "activation", [biasv], [hidT, Pq], out=hidT[:, 0:nc_pad], in_=Pq[:, 0:nc_pad], func=AF.Gelu,
                              bias=biasv[:, 0:1], scale=1.0)
                        if kv == 0:
                            kb.op("pe", "matmul", [w2k_b, hidT], [Pqs], Pqs[:, 0:nc_pad], lhsT=w2k_b[:], rhs=hidT[:, 0:nc_pad],
                                  start=True, stop=True)
                            kb.op("act", "activation", [b2kc], [KCT, Pqs], out=KCT.ap(0, [[1, nc_pad]], parts=64, pstart=g * 64),
                                  in_=Pqs.ap(0, [[1, nc_pad]], parts=64, pstart=g * 64), func=AF.Identity,
                                  bias=b2kc.ap(0, [[1, 1]], parts=64, pstart=g * 64), scale=1.0)
                        else:
                            for nt in range(4):
                                kb.op("pe", "matmul", [hidT, w2v_b], [PV], PV[:, 0:64], lhsT=hidT[:, nt * 128:(nt + 1) * 128],
                                      rhs=w2v_b[:], start=True, stop=False)
                                kb.op("pe", "matmul", [ones_b, b2v_b], [PV], PV[:, 0:64], lhsT=ones_b[0:1, :], rhs=b2v_b[0:1, :],
                                      start=False, stop=True)
                                kb.op("act", "copy", [], [VC, PV], out=VC.ap((nt * 2 + g) * 65, [[1, 64]]), in_=PV[:, 0:64])
            kb.barrier()

        kb.mark("C1")
        with ExitStack() as es:
            E_b = kb.sb("E_b", [128, S], BF16, es)
            CMPB = kb.sb("CMPB", [128, 2048], BF16, es)
            CAUS = kb.sb("CAUS", [128, 256], BF16, es)
            AGG = kb.sb("AGG", [128, 512], BF16, es)
            BASE = kb.sb("BASE", [128, 382], F32, es)
            Qt = kb.sb("Qt", [128, 512], BF16, es)
            Pt = [kb.sb("Pt%d" % i, [128, 512], BF16, es) for i in range(2)]
            Osb = kb.sb("Osb", [128, 3 * 512], F32, es)
            onsa = kb.sb("onsa", [128, 512], F32, es)
            onT = kb.sb("onT", [128, 512], BF16, es)
            imp = kb.sb("imp", [128, 128], F32, es)
            val = kb.sb("val", [128, 128], F32, es)
            val2 = kb.sb("val2", [128, 128], F32, es)
            negs = kb.sb("negs", [128, 128], F32, es)
            negT = kb.sb("negT", [128, 128], BF16, es)
            m8 = kb.sb("m8", [128, 16], F32, es)
            zc = kb.sb("zc", [128, 4], F32, es)
            rzc = kb.sb("rzc", [128, 4], F32, es)
            zt = kb.sb("zt", [128, 4], F32, es)
            coef = kb.sb("coef", [128, 4], F32, es)
            PS = [kb.ps("PS%d" % i, [128, 512], F32, es) for i in range(2)]
            PO3 = [kb.ps("PO3_%d" % i, [128, 512], F32, es) for i in range(3)]
            PU = kb.ps("PU", [128, 512], F32, es)
            PTr = kb.ps("PTr", [128, 512], F32, es)

            kb.dma("pool", E_b[:], ebig[:], [ebig], [E_b])
            kb.dma("pool", CMPB[:], cmpb[:], [cmpb], [CMPB])
            kb.dma("pool", CAUS[:], caus[:], [caus], [CAUS])
            kb.dma("pool", AGG[:], aggd[:], [aggd], [AGG])
            kb.dma("sp", BASE[:], based[:], [based], [BASE])
            npt = [0]

            pend = [None]

            def flush():
                if pend[0] is not None:
                    f = pend[0]
                    pend[0] = None
                    f()

            def attn_tile(Pout, first, last, kT, kcol, Vt, voff, g, biases, extra=None):
                Ps = PS[npt[0] % 2]
                Pb = Pt[npt[0] % 2]
                npt[0] += 1
                nb = len(biases)
                for bi, (lt, lap, rt, rap) in enumerate(biases):
                    kb.op("pe", "matmul", [lt, rt], [Ps], Ps[:, 0:512], lhsT=lap, rhs=rap, start=(bi == 0), stop=False)
                kb.op("pe", "matmul", [kT, Qt], [Ps], Ps[:, 0:512], lhsT=kT.ap(kcol, [[1, 128]], parts=64, pstart=g * 64),
                      rhs=Qt.ap(0, [[128, 4], [1, 128]], parts=64, pstart=g * 64), start=(nb == 0), stop=True)
                kb.op("act", "activation", [], [Pb, Ps], out=Pb[:], in_=Ps[:, 0:512], func=AF.Exp, scale=0.125)
                flush()

                def pv():
                    kb.op("pe", "matmul", [Vt, Pb], [Pout], Pout.ap(0, [[1, 512]], parts=65), lhsT=Vt.ap(voff, [[1, 65]]),
                          rhs=Pb[:], start=first, stop=last)
                    if extra is not None:
                        extra(Pb)
                pend[0] = pv

            for qt in range(NQ):
                kb.dma("sp", Qt.ap(0, [[128, 4], [1, 128]]), QTd.ap(qt * 128, [[4 * S, 128], [S, 4], [1, 128]]), [QTd], [Qt])
                for g in range(2):
                    ntmax = min(3, (8 * qt + 6) // 128)
                    for nt in range(ntmax + 1):
                        m = qt - 16 * nt
                        biases = []
                        if m <= 15:
                            biases.append((ident_b, ident_b[:], CMPB, CMPB.ap(m * 128, [[0, 4], [1, 128]])))
                        def imp_mm(Pb, nt=nt, ntmax=ntmax):
                            for r in range(4):
                                kb.op("pe", "matmul", [Pb, AGG], [PU], PU[:, r * 128:(r + 1) * 128], lhsT=Pb[:, r * 128:(r + 1) * 128],
                                      rhs=AGG[:, nt * 128:(nt + 1) * 128], start=(nt == 0 and r == 0), stop=(nt == ntmax and r == 3),
                                      skip_group_check=True)
                        attn_tile(PO3[0], nt == 0, nt == ntmax, KCT, nt * 128, VC, (nt * 2 + g) * 65, g, biases, extra=imp_mm)
                    flush()
                    kb.op("dve", "tensor_reduce", [], [zc, PU], out=zc[:], in_=PU.ap(0, [[128, 4], [1, 128]]), axis=AX.X, op=ALU.add)
                    kb.op("dve", "tensor_scalar", [zc], [zc], out=zc[:], in0=zc[:], scalar1=1e-30, scalar2=None, op0=ALU.max)
                    kb.op("dve", "reciprocal", [zc], [rzc], out=rzc[:], in_=zc[:])
                    kb.op("dve", "tensor_scalar", [rzc], [imp, PU], out=imp[:], in0=PU[:, 0:128], scalar1=rzc[:, 0:1], scalar2=None,
                          op0=ALU.mult)
                    for r in range(1, 4):
                        kb.op("dve", "scalar_tensor_tensor", [rzc, imp], [imp, PU], out=imp[:], in0=PU[:, r * 128:(r + 1) * 128],
                              scalar=rzc[:, r:r + 1], in1=imp[:], op0=ALU.mult, op1=ALU.add)
                    kb.op("dve", "tensor_tensor", [imp, BASE], [val], out=val[:], in0=imp[:], in1=BASE[:, 126 - 2 * qt:254 - 2 * qt],
                          op=ALU.add)
                    kb.op("dve", "tensor_tensor", [val, BASE], [val], out=val[:], in0=val[:], in1=BASE[:, 254:382], op=ALU.add)
                    kb.op("dve", "max", [val], [m8], out=m8[:, 0:8], in_=val[:])
                    kb.op("dve", "match_replace", [m8, val], [val2], out=val2[:], in_to_replace=m8[:, 0:8], in_values=val[:],
                          imm_value=-3e38)
                    kb.op("dve", "max", [val2], [m8], out=m8[:, 8:16], in_=val2[:])
                    kb.op("dve", "tensor_scalar", [val, m8], [negs], out=negs[:], in0=val[:], scalar1=m8[:, 15:16], scalar2=NEG,
                          op0=ALU.is_lt, op1=ALU.mult)
                    kb.op("pe", "transpose", [negs, ident_f], [PTr], PTr[:, 0:128], negs[:], ident_f[:])
                    kb.op("act", "copy", [], [negT, PTr], out=negT[:], in_=PTr[:, 0:128])
                    for kt in range(qt + 1):
                        biases = [(E_b, E_b[:, kt * 128:(kt + 1) * 128], negT, negT.ap(0, [[0, 4], [1, 128]]))]
                        if kt == qt:
                            biases.append((ident_b, ident_b[:], CAUS, CAUS.ap(0, [[0, 4], [1, 128]])))
                        attn_tile(PO3[1], kt == 0, kt == qt, KsT, kt * 128, VS, (kt * 2 + g) * 65, g, biases)
                    k0 = max(0, qt - 4)
                    for kt in range(k0, qt + 1):
                        biases = []
                        if kt == qt:
                            biases.append((ident_b, ident_b[:], CAUS, CAUS.ap(0, [[0, 4], [1, 128]])))
                        elif kt == qt - 4:
                            biases.append((ident_b, ident_b[:], CAUS, CAUS.ap(128, [[0, 4], [1, 128]])))
                        attn_tile(PO3[2], kt == k0, kt == qt, KwT, kt * 128, VW, (kt * 2 + g) * 65, g, biases)
                    flush()
                    for br in range(3):
                        kb.op("act", "copy", [], [Osb, PO3[br]], out=Osb.ap(br * 512, [[1, 512]], parts=65),
                              in_=PO3[br].ap(0, [[1, 512]], parts=65))
                    for br in range(3):
                        for r in range(4):
                            kb.op("pe", "transpose", [Osb, ident_f], [PTr], PTr[:, r * 65:(r + 1) * 65],
                                  Osb.ap(br * 512 + r * 128, [[1, 128]], parts=65), ident_f.ap(0, [[1, 65]], parts=65))
                        kb.op("dve", "tensor_scalar", [], [zt, PTr], out=zt[:], in0=PTr.ap(64, [[65, 4]]), scalar1=1e-30, scalar2=None,
                              op0=ALU.max)
                        kb.op("dve", "reciprocal", [zt], [zt], out=zt[:], in_=zt[:])
                        kb.op("dve", "tensor_tensor", [zt, Gt], [coef], out=coef[:], in0=zt[:],
                              in1=Gt[:, qt * 24 + br * 8 + g * 4:qt * 24 + br * 8 + g * 4 + 4], op=ALU.mult)
                        for r in range(4):
                            h = g * 4 + r
                            if br == 0:
                                kb.op("dve", "tensor_scalar", [coef], [onsa, PTr], out=onsa[:, h * 64:(h + 1) * 64],
                                      in0=PTr[:, r * 65:r * 65 + 64], scalar1=coef[:, r:r + 1], scalar2=None, op0=ALU.mult)
                            else:
                                kb.op("dve", "scalar_tensor_tensor", [coef, onsa], [onsa, PTr], out=onsa[:, h * 64:(h + 1) * 64],
                                      in0=PTr[:, r * 65:r * 65 + 64], scalar=coef[:, r:r + 1], in1=onsa[:, h * 64:(h + 1) * 64],
                                      op0=ALU.mult, op1=ALU.add)
                for kc in range(4):
                    kb.op("pe", "transpose", [onsa, ident_f], [PTr], PTr[:, kc * 128:(kc + 1) * 128], onsa[:, kc * 128:(kc + 1) * 128],
                          ident_f[:])
                kb.op("act", "copy", [], [onT, PTr], out=onT[:], in_=PTr[:, 0:512])
                kb.dma("sp", ONd.ap(qt * 128, [[4 * S, 128], [S, 4], [1, 128]]), onT.ap(0, [[128, 4], [1, 128]]), [onT], [ONd])
            kb.barrier()
        es1.close()

    kb.mark("W")
    if with_peer:
        with ExitStack() as es:
            cb = [kb.sb("cb%d" % i, [128, 2048], BF16, es) for i in range(4)]
            n = 0
            for src, dst in ((UT, UTb), (VJ, VJb)):
                for j2 in range(64):
                    b = cb[n % 4]
                    n += 1
                    kb.dma("pool", b.ap(0, [[1024, 2], [1, 1024]]), src.ap(j2 * 2 * 131072, [[1024, 128], [131072, 2], [1, 1024]]),
                           [src], [b])
                    kb.dma("sp", dst.ap(j2 * 2 * 131072, [[1024, 128], [131072, 2], [1, 1024]]), b.ap(0, [[1024, 2], [1, 1024]]),
                           [b], [dst])
            kb.barrier()

    kb.mark("C2")
    with ExitStack() as es:
        Wz_b = kb.sb("Wz_b", [128, 8, 1024], BF16, es)
        Wm_b = kb.sb("Wm_b", [128, 8, 2048], BF16, es)
        Wb1_b = kb.sb("Wb1_b", [128, 4, 1024], BF16, es)
        Wo_b = kb.sb("Wo_b", [128, 8, 1024], BF16, es)
        WsT_f = kb.sb("WsT_f", [128, 1024], F32, es)
        WsT_b = kb.sb("WsT_b", [128, 8, 128], BF16, es)
        tril_sb = kb.sb("tril_sb", [128, 128], F32, es)
        sgub = kb.sb("sgub", [128, 8], F32, es)
        bmcol = kb.sb("bmcol", [128, 16], F32, es)
        xs = [kb.sb("xs%d" % i, [128, 1024], F32, es) for i in range(4)]
        xn_b = kb.sb("xn_b", [128, 1024], BF16, es)
        hT = kb.sb("hT", [128, 8, 512], BF16, es)
        gab = kb.sb("gab", [128, 2, 512], BF16, es)
        u_b = kb.sb("u_b", [128, 512], BF16, es)
        v_f = kb.sb("v_f", [128, 512], F32, es)
        vb = kb.sb("vb", [128, 512], BF16, es)
        tmp = kb.sb("tmp", [128, 512], F32, es)
        osgu = kb.sb("osgu", [128, 512], BF16, es)
        osT = kb.sb("osT", [128, 4, 512], BF16, es)
        mT = kb.sb("mT", [128, 8, 512], BF16, es)
        ybuf = kb.sb("ybuf", [128, 1024], F32, es)
        x1 = kb.sb("x1", [128, 1024], F32, es)
        st = kb.sb("st", [128, 12], F32, es)
        mv = kb.sb("mv", [128, 2], F32, es)
        sd = kb.sb("sd", [128, 1], F32, es)
        rstd = kb.sb("rstd", [128, 1], F32, es)
        PT = kb.ps("PT", [128, 1024], BF16, es)
        PGA = kb.ps("PGA", [128, 512], F32, es)
        PGB = kb.ps("PGB", [128, 512], F32, es)
        PB_ = kb.ps("PB_", [128, 512], F32, es)
        if with_nsa:
            PA_ = kb.ps("PA_", [128, 512], F32, es)
            Wb0_b = kb.sb("Wb0_b", [128, 4, 1024], BF16, es)
            onsaT = kb.sb("onsaT", [128, 4, 512], BF16, es)
            tmpa = kb.sb("tmpa", [128, 512], F32, es)
            kb.dma("pool", Wb0_b[:], wslab(w_b0, 4, 1024, 0, 1024), [w_b0], [Wb0_b])
        PZ0 = kb.ps("PZ0", [128, 512], F32, es)
        PZ1 = kb.ps("PZ1", [128, 512], F32, es)
        PM = kb.ps("PM", [128, 512], F32, es)

        kb.dma("pool", Wz_b[:], wslab(wz, 8, 1024, 0, 1024), [wz], [Wz_b])
        kb.dma("pool", Wm_b[:], wslab(w_merge, 8, 2048, 0, 2048), [w_merge], [Wm_b])
        kb.dma("pool", Wb1_b[:], wslab(w_b1, 4, 1024, 0, 1024), [w_b1], [Wb1_b])
        kb.dma("pool", Wo_b[:], wslab(w_out, 8, 1024, 0, 1024), [w_out], [Wo_b])
        kb.dma("sp", WsT_f[:], wsT[:], [wsT], [WsT_f])
        kb.dma("sp", tril_sb[:], trilm[:], [trilm], [tril_sb])
        kb.dma("sp", sgub[:], sgub_col[:], [sgub_col], [sgub])
        kb.dma("sp", bmcol[:], b_merge_col[:], [b_merge_col], [bmcol])
        kb.op("dve", "tensor_tensor", [WsT_f, tril_sb], [WsT_b], out=WsT_b.ap(0, [[128, 8], [1, 128]]),
              in0=WsT_f.ap(0, [[128, 8], [1, 128]]), in1=tril_sb.ap(0, [[0, 8], [1, 128]]), op=ALU.mult)

        for tg in range(S // 512):
            t0 = tg * 512
            for s in range(4):
                kb.dma("sp", xs[s][:], x.ap((t0 + s * 128) * D, [[D, 128], [1, D]]), [x], [xs[s]])
            for s in range(4):
                ln_transpose(xs[s], xn_b, PT, hT, s * 128, 128, 8, 0, st, mv, sd, rstd)
            if with_nsa:
                kb.dma("sp", onsaT.ap(0, [[512, 4], [1, 512]]), ONd.ap(t0, [[4 * S, 128], [S, 4], [1, 512]]), [ONd], [onsaT])
            for s in range(4):
                for half, Pz in enumerate([PZ0, PZ1]):
                    for k in range(8):
                        kb.op("pe", "matmul", [hT, Wz_b], [Pz], Pz[:, 0:512], lhsT=hT[:, k, s * 128:(s + 1) * 128],
                              rhs=Wz_b[:, k, half * 512:(half + 1) * 512], start=(k == 0), stop=(k == 7))
                kb.op("act", "activation", [], [u_b, PZ0], out=u_b[:], in_=PZ0[:, 0:512], func=AF.Gelu)
                kb.op("act", "activation", [], [v_f, PZ1], out=v_f[:], in_=PZ1[:, 0:512], func=AF.Gelu)
                kb.op("dve", "bn_stats", [v_f], [st], out=st[:, 0:6], in_=v_f[:])
                kb.op("dve", "bn_aggr", [st], [mv], out=mv[:, 0:2], in_=st[:, 0:6])
                kb.op("act", "activation", [mv, eps_col], [sd], out=sd[:, 0:1], in_=mv[:, 1:2], func=AF.Sqrt,
                      bias=eps_col[:, 0:1], scale=1.0)
                kb.op("dve", "reciprocal", [sd], [rstd], out=rstd[:, 0:1], in_=sd[:, 0:1])
                kb.op("dve", "scalar_tensor_tensor", [v_f, mv, ln_bc], [tmp], out=tmp[:], in0=v_f[:], scalar=mv[:, 0:1],
                      in1=ln_bc[:, 4096:4608], op0=ALU.subtract, op1=ALU.mult)
                kb.op("dve", "scalar_tensor_tensor", [tmp, rstd, ln_bc], [vb], out=vb[:], in0=tmp[:], scalar=rstd[:, 0:1],
                      in1=ln_bc[:, 4608:5120], op0=ALU.mult, op1=ALU.add)
                for g in range(8):
                    kb.op("pe", "matmul", [WsT_b, vb], [PM], PM[:, g * 64:(g + 1) * 64], lhsT=WsT_b[:, g, :],
                          rhs=vb[:, g * 64:(g + 1) * 64], start=True, stop=True)
                kb.op("dve", "tensor_tensor", [sgub], [tmp, PM], out=tmp.ap(0, [[64, 8], [1, 64]]),
                      in0=PM.ap(0, [[64, 8], [1, 64]]), in1=sgub.ap(0, [[1, 8], [0, 64]]), op=ALU.add)
                kb.op("dve", "tensor_tensor", [tmp, u_b], [osgu], out=osgu[:], in0=tmp[:], in1=u_b[:], op=ALU.mult)
                for kc in range(4):
                    kb.op("pe", "transpose", [osgu, ident_b], [PT], PT[:, kc * 128:(kc + 1) * 128],
                          osgu[:, kc * 128:(kc + 1) * 128], ident_b[:])
                kb.op("act", "copy", [], [osT, PT], out=osT.ap(s * 128, [[512, 4], [1, 128]]),
                      in_=PT.ap(0, [[128, 4], [1, 128]]))
            for cch in range(8):
                for gi, (Pg, col) in enumerate([(PGA, cch), (PGB, 8 + cch)]):
                    if gi == 0 and not with_nsa:
                        continue
                    for k in range(8):
                        kb.op("pe", "matmul", [hT, Wm_b], [Pg], Pg[:, 0:512], lhsT=Wm_b[:, k, col * 128:(col + 1) * 128],
                              rhs=hT[:, k, :], start=(k == 0), stop=(k == 7))
                for kc in range(4):
                    kb.op("pe", "matmul", [osT, Wb1_b], [PB_], PB_[:, 0:512], lhsT=Wb1_b[:, kc, cch * 128:(cch + 1) * 128],
                          rhs=osT[:, kc, :], start=(kc == 0), stop=(kc == 3))
                kb.op("act", "activation", [bmcol], [gab, PGB], out=gab[:, 1, :], in_=PGB[:, 0:512], func=AF.Sigmoid,
                      bias=bmcol[:, 8 + cch:9 + cch], scale=1.0)
                if with_nsa:
                    for kc in range(4):
                        kb.op("pe", "matmul", [onsaT, Wb0_b], [PA_], PA_[:, 0:512], lhsT=Wb0_b[:, kc, cch * 128:(cch + 1) * 128],
                              rhs=onsaT[:, kc, :], start=(kc == 0), stop=(kc == 3))
                    kb.op("act", "activation", [bmcol], [gab, PGA], out=gab[:, 0, :], in_=PGA[:, 0:512], func=AF.Sigmoid,
                          bias=bmcol[:, cch:cch + 1], scale=1.0)
                    kb.op("dve", "tensor_tensor", [gab], [tmpa, PA_], out=tmpa[:], in0=PA_[:, 0:512], in1=gab[:, 0, :], op=ALU.mult)
                    kb.op("dve", "tensor_tensor", [gab], [tmp, PB_], out=tmp[:], in0=PB_[:, 0:512], in1=gab[:, 1, :], op=ALU.mult)
                    kb.op("dve", "tensor_tensor", [tmp, tmpa], [mT], out=mT[:, cch, :], in0=tmp[:], in1=tmpa[:], op=ALU.add)
                else:
                    kb.op("dve", "tensor_tensor", [gab], [mT, PB_], out=mT[:, cch, :], in0=PB_[:, 0:512], in1=gab[:, 1, :],
                          op=ALU.mult)
            for s in range(4):
                for half, Pz in enumerate([PZ0, PZ1]):
                    for k in range(8):
                        kb.op("pe", "matmul", [mT, Wo_b], [Pz], Pz[:, 0:512], lhsT=mT[:, k, s * 128:(s + 1) * 128],
                              rhs=Wo_b[:, k, half * 512:(half + 1) * 512], start=(k == 0), stop=(k == 7))
                resid_ln(xs[s], PZ0, PZ1, 0, 0, 1024, ybuf, x1, st, mv, sd, rstd)
                dst = x1s if with_peer else y
                kb.dma("pool", dst.ap((t0 + s * 128) * D, [[D, 128], [1, D]]), x1[:], [x1], [dst])
        kb.barrier()

    if not with_peer:
        kb.wait_all("sp")
        return nc, kb

    kb.mark("D")
    NG = S // 256
    GTd2 = [kb.dram("GTd%d" % i, [128, 128 * 256], BF16) for i in range(2)]
    with ExitStack() as es:
        Wq_b = kb.sb("Wq_b", [128, 8, 2048], BF16, es)
        keys_b = kb.sb("keys_b", [128, 16, 128], BF16, es)
        GT = kb.sb("GT", [128, 128 * 128], BF16, es)
        NBUF = 3
        Ubuf = [kb.sb("Ubuf%d" % i, [128, 2, 1024], BF16, es) for i in range(NBUF)]
        Vbuf = [kb.sb("Vbuf%d" % i, [128, 2, 1024], BF16, es) for i in range(NBUF)]
        Gs = [kb.sb("Gs%d" % i, [128, 2, 256], BF16, es) for i in range(NBUF)]
        x1t = [kb.sb("x1t%d" % i, [128, 1024], F32, es) for i in range(2)]
        xr = kb.sb("xr", [128, 1024], F32, es)
        h2T2 = [kb.sb("h2T%d" % i, [128, 8, 256], BF16, es) for i in range(2)]
        qT = kb.sb("qT", [128, 16, 256], BF16, es)
        sc = kb.sb("sc", [128, 4, 128], F32, es)
        scrA = kb.sb("scrA", [128, 2048], F32, es)
        scrB = kb.sb("scrB", [128, 2048], F32, es)
        t16 = kb.sb("t16", [128, 256], F32, es)
        i16 = kb.sb("i16", [128, 256], U32, es)
        i16f = kb.sb("i16f", [128, 256], F32, es)
        tv = kb.sb("tv", [128, 128], F32, es)
        pv = kb.sb("pv", [128, 128], U32, es)
        pvf = kb.sb("pvf", [128, 128], F32, es)
        ee = kb.sb("ee", [128, 128], F32, es)
        zz = kb.sb("zz", [128, 8], F32, es)
        rz = kb.sb("rz", [128, 8], F32, es)
        ak = kb.sb("ak", [128, 128], F32, es)
        bk = kb.sb("bk", [128, 128], F32, es)
        III = kb.sb("III", [128, 384], F32, es)
        ITs = kb.sb("ITs", [128, 384], F32, es)
        iota16 = kb.sb("iota16", [128, 16], F32, es)
        CH = 16
        Lb = kb.sb("Lb", [128, CH * 128], BF16, es)
        Rb = kb.sb("Rb", [128, CH * 128], BF16, es)
        actg = [kb.sb("actg%d" % i, [128, 512], BF16, es) for i in range(2)]
        wd = [kb.sb("wd%d" % i, [128, 512], BF16, es) for i in range(2)]
        ybuf = kb.sb("ybuf2", [128, 1024], F32, es)
        st = kb.sb("st2", [128, 12], F32, es)
        mv = kb.sb("mv2", [128, 2], F32, es)
        sd = kb.sb("sd2", [128, 1], F32, es)
        rstd = kb.sb("rstd2", [128, 1], F32, es)
        stf = kb.sb("st3", [128, 12], F32, es)
        mvf = kb.sb("mv3", [128, 2], F32, es)
        sdf = kb.sb("sd3", [128, 1], F32, es)
        rstdf = kb.sb("rstd3", [128, 1], F32, es)
        PO = [kb.ps("PO%d" % i, [128, 512], F32, es) for i in range(4)]
        PA = [kb.ps("PA%d" % i, [128, 512], F32, es) for i in range(2)]
        PG = [kb.ps("PG%d" % i, [128, 512], F32, es) for i in range(2)]

        kb.dma("pool", Wq_b[:], wslab(peer_wq, 8, 2048, 0, 2048), [peer_wq], [Wq_b])
        kb.dma("pool", keys_b.ap(0, [[1, 2048]]), keysT[:], [keysT], [keys_b])
        kb.op("pool", "iota", [], [iota16], iota16[:], [[1, 16]], base=0, channel_multiplier=0,
              allow_small_or_imprecise_dtypes=True)

        def ln_transpose_f(src, dstT, col0, sc_off, sh_off):
            layernorm_stats(src, st, mv, sd, rstd)
            kb.op("dve", "tensor_scalar", [src, mv, rstd], [scrA], out=scrA[:, 0:1024], in0=src[:], scalar1=mv[:, 0:1],
                  scalar2=rstd[:, 0:1], op0=ALU.subtract, op1=ALU.mult)
            for k in range(8):
                Pp = PG[k // 4]
                kb.op("pe", "transpose", [scrA, ident_f], [Pp], Pp[:, (k % 4) * 128:(k % 4 + 1) * 128],
                      scrA[:, k * 128:(k + 1) * 128], ident_f[:])
            for k in range(8):
                Pp = PG[k // 4]
                kb.op("act", "activation", [modcol], [dstT, Pp], out=dstT[:, k, col0:col0 + 128],
                      in_=Pp[:, (k % 4) * 128:(k % 4 + 1) * 128], func=AF.Identity,
                      bias=modcol[:, sh_off + k:sh_off + k + 1], scale=modcol[:, sc_off + k:sc_off + k + 1])

        def prep(tg):
            t0 = tg * 256
            h2T = h2T2[tg % 2]
            GTd = GTd2[tg % 2]
            for s in range(2):
                kb.dma("sp", x1t[s][:], x1s.ap((t0 + s * 128) * D, [[D, 128], [1, D]]), [x1s], [x1t[s]])
                ln_transpose_f(x1t[s], h2T, s * 128, 24, 16)
                yield
            for cch in range(16):
                Pq = PG[cch % 2]
                for k in range(8):
                    kb.op("pe", "matmul", [h2T, Wq_b], [Pq], Pq[:, 0:256], lhsT=Wq_b[:, k, cch * 128:(cch + 1) * 128],
                          rhs=h2T[:, k, :], start=(k == 0), stop=(k == 7))
                kb.op("act", "copy", [], [qT, Pq], out=qT[:, cch, :], in_=Pq[:, 0:256])
                yield
            for s in range(2):
                ts = slice(s * 128, (s + 1) * 128)
                for r4 in range(4):
                    Ps = PG[r4 % 2]
                    for rr in range(4):
                        r = r4 * 4 + rr
                        kb.op("pe", "matmul", [qT, keys_b], [Ps], Ps[:, rr * 128:(rr + 1) * 128], lhsT=qT[:, r, ts],
                              rhs=keys_b[:, r, :], start=True, stop=True)
                    kb.op("act", "copy", [], [sc, Ps], out=sc.ap(0, [[1, 512]]), in_=Ps[:, 0:512])
                    yield
                    for rr in range(4):
                        r = r4 * 4 + rr
                        kb.op("dve", "max", [sc], [t16], out=t16[:, r * 16:r * 16 + 8], in_=sc[:, rr, :])
                        kb.op("dve", "max_index", [t16, sc], [i16], out=i16[:, r * 16:r * 16 + 8],
                              in_max=t16[:, r * 16:r * 16 + 8], in_values=sc[:, rr, :])
                        kb.op("dve", "match_replace", [t16, sc], [scrA], out=scrA[:, rr * 128:(rr + 1) * 128],
                              in_to_replace=t16[:, r * 16:r * 16 + 8], in_values=sc[:, rr, :], imm_value=-1e30)
                        kb.op("dve", "max", [scrA], [t16], out=t16[:, r * 16 + 8:r * 16 + 16],
                              in_=scrA[:, rr * 128:(rr + 1) * 128])
                        kb.op("dve", "max_index", [t16, scrA], [i16], out=i16[:, r * 16 + 8:r * 16 + 16],
                              in_max=t16[:, r * 16 + 8:r * 16 + 16], in_values=scrA[:, rr * 128:(rr + 1) * 128])
                        yield
                kb.op("dve", "tensor_copy", [i16], [i16f], out=i16f[:], in_=i16[:])
                kb.op("dve", "tensor_tensor", [t16], [scrB], out=scrB.ap(0, [[256, 8], [16, 16], [1, 16]]),
                      in0=t16.ap(0, [[32, 8], [1, 16], [0, 16]]), in1=t16.ap(16, [[32, 8], [0, 16], [1, 16]]), op=ALU.add)
                yield
                for h in range(8):
                    cs_ = slice(h * 256, (h + 1) * 256)
                    kb.op("dve", "max", [scrB], [tv], out=tv[:, h * 16:h * 16 + 8], in_=scrB[:, cs_])
                    kb.op("dve", "max_index", [tv, scrB], [pv], out=pv[:, h * 16:h * 16 + 8], in_max=tv[:, h * 16:h * 16 + 8],
                          in_values=scrB[:, cs_])
                    kb.op("dve", "match_replace", [tv, scrB], [scrA], out=scrA[:, cs_], in_to_replace=tv[:, h * 16:h * 16 + 8],
                          in_values=scrB[:, cs_], imm_value=-1e30)
                    kb.op("dve", "max", [scrA], [tv], out=tv[:, h * 16 + 8:h * 16 + 16], in_=scrA[:, cs_])
                    kb.op("dve", "max_index", [tv, scrA], [pv], out=pv[:, h * 16 + 8:h * 16 + 16],
                          in_max=tv[:, h * 16 + 8:h * 16 + 16], in_values=scrA[:, cs_])
                    yield
                kb.op("dve", "tensor_tensor", [tv], [ee], out=ee.ap(0, [[16, 8], [1, 16]]), in0=tv.ap(0, [[16, 8], [1, 16]]),
                      in1=tv.ap(0, [[16, 8], [0, 16]]), op=ALU.subtract)
                kb.op("act", "activation", [ee], [ee], out=ee[:], in_=ee[:], func=AF.Exp)
                kb.op("dve", "tensor_reduce", [ee], [zz], out=zz[:], in_=ee.ap(0, [[16, 8], [1, 16]]), axis=AX.X, op=ALU.add)
                kb.op("dve", "reciprocal", [zz], [rz], out=rz[:], in_=zz[:])
                kb.op("dve", "tensor_tensor", [ee, rz], [III], out=III.ap(256, [[16, 8], [1, 16]]),
                      in0=ee.ap(0, [[16, 8], [1, 16]]), in1=rz.ap(0, [[1, 8], [0, 16]]), op=ALU.mult)
                yield
                kb.op("dve", "tensor_copy", [pv], [pvf], out=pvf[:], in_=pv[:])
                kb.op("dve", "tensor_tensor", [pvf, thr15], [scrA], out=scrA.ap(0, [[15, 128], [1, 15]]),
                      in0=pvf.ap(0, [[1, 128], [0, 15]]), in1=thr15.ap(0, [[0, 128], [1, 15]]), op=ALU.is_ge)
                kb.op("dve", "tensor_reduce", [scrA], [ak], out=ak[:], in_=scrA.ap(0, [[15, 128], [1, 15]]), axis=AX.X, op=ALU.add)
                kb.op("dve", "scalar_tensor_tensor", [ak, pvf], [bk], out=bk[:], in0=ak[:], scalar=-16.0, in1=pvf[:],
                      op0=ALU.mult, op1=ALU.add)
                yield
                for which, (sel, off) in enumerate([(ak, 0), (bk, 16)]):
                    kb.op("dve", "tensor_tensor", [iota16, sel], [scrA], out=scrA.ap(0, [[256, 8], [16, 16], [1, 16]]),
                          in0=iota16.ap(0, [[0, 8], [0, 16], [1, 16]]), in1=sel.ap(0, [[16, 8], [1, 16], [0, 16]]),
                          op=ALU.is_equal)
                    kb.op("dve", "tensor_tensor", [scrA, i16f], [scrB], out=scrB.ap(0, [[256, 8], [16, 16], [1, 16]]),
                          in0=scrA.ap(0, [[256, 8], [16, 16], [1, 16]]), in1=i16f.ap(off, [[32, 8], [0, 16], [1, 16]]),
                          op=ALU.mult)
                    kb.op("dve", "tensor_reduce", [scrB], [III], out=III.ap(which * 128, [[16, 8], [1, 16]]),
                          in_=scrB.ap(0, [[256, 8], [16, 16], [1, 16]]), axis=AX.X, op=ALU.add)
                    yield
                for i3 in range(3):
                    kb.op("pe", "transpose", [III, ident_f], [PG[0]], PG[0][:, i3 * 128:(i3 + 1) * 128],
                          III[:, i3 * 128:(i3 + 1) * 128], ident_f[:])
                kb.op("act", "copy", [], [ITs, PG[0]], out=ITs[:], in_=PG[0][:, 0:384])
                yield
                for ch in range(128 // CH):
                    kb.op("dve", "tensor_tensor", [iota128, ITs], [Lb], out=Lb.ap(0, [[128, CH], [1, 128]]),
                          in0=iota128.ap(0, [[0, CH], [1, 128]]), in1=ITs.ap(ch * CH, [[1, CH], [0, 128]]), op=ALU.is_equal)
                    kb.op("dve", "tensor_tensor", [iota128, ITs], [Rb], out=Rb.ap(0, [[128, CH], [1, 128]]),
                          in0=iota128.ap(0, [[0, CH], [1, 128]]), in1=ITs.ap(128 + ch * CH, [[1, CH], [0, 128]]), op=ALU.is_equal)
                    kb.op("dve", "tensor_tensor", [Rb, ITs], [Rb], out=Rb.ap(0, [[128, CH], [1, 128]]),
                          in0=Rb.ap(0, [[128, CH], [1, 128]]), in1=ITs.ap(256 + ch * CH, [[1, CH], [0, 128]]), op=ALU.mult)
                    yield
                    for t4 in range(CH // 4):
                        Pg = PG[t4 % 2]
                        for tt in range(4):
                            tl = t4 * 4 + tt
                            kb.op("pe", "matmul", [Lb, Rb], [Pg], Pg[:, tt * 128:(tt + 1) * 128], lhsT=Lb[:, tl * 128:(tl + 1) * 128],
                                  rhs=Rb[:, tl * 128:(tl + 1) * 128], start=True, stop=True)
                        tokb = ch * CH + t4 * 4
                        kb.op("act", "copy", [], [GT, Pg], out=GT.ap(tokb, [[1, 4], [128, 128]]), in_=Pg.ap(0, [[128, 4], [1, 128]]))
                        yield
                for jb in range(8):
                    kb.dma("pool", GTd.ap(jb * 16 * 256 + s * 128, [[128 * 256, 128], [256, 16], [1, 128]]),
                           GT.ap(jb * 16 * 128, [[128, 16], [1, 128]]), [GT], [GTd])
                yield

        def run_steps(gen, n):
            if gen is None:
                return None
            for _ in range(n):
                try:
                    next(gen)
                except StopIteration:
                    return None
            return gen

        gen = prep(0)
        while gen is not None:
            gen = run_steps(gen, 1000)
        for tg in range(NG):
            t0 = tg * 256
            h2T = h2T2[tg % 2]
            GTd = GTd2[tg % 2]
            gen = prep(tg + 1) if tg + 1 < NG else None
            for jp in range(64):
                bi = jp % NBUF
                j0 = jp * 2
                kb.dma("sp", Ubuf[bi].ap(0, [[1024, 2], [1, 1024]]), UTb.ap(j0 * 131072, [[1024, 128], [131072, 2], [1, 1024]]),
                       [UTb], [Ubuf[bi]])
                kb.dma("act", Vbuf[bi].ap(0, [[1024, 2], [1, 1024]]), VJb.ap(j0 * 131072, [[1024, 128], [131072, 2], [1, 1024]]),
                       [VJb], [Vbuf[bi]])
                kb.dma("sp", Gs[bi].ap(0, [[256, 2], [1, 256]]), GTd.ap(j0 * 256, [[128 * 256, 128], [256, 2], [1, 256]]),
                       [GTd], [Gs[bi]])
                Pa = PA[jp % 2]
                for jj in range(2):
                    for k in range(8):
                        kb.op("pe", "matmul", [h2T, Ubuf[bi]], [Pa], Pa[:, jj * 256:(jj + 1) * 256],
                              lhsT=Ubuf[bi][:, jj, k * 128:(k + 1) * 128], rhs=h2T[:, k, :], start=(k == 0), stop=(k == 7))
                ag = actg[jp % 2]
                wdd = wd[jp % 2]
                kb.op("act", "activation", [], [ag, Pa], out=ag[:], in_=Pa[:, 0:512], func=AF.Gelu)
                kb.op("dve", "tensor_tensor", [ag, Gs[bi]], [wdd], out=wdd[:], in0=ag[:], in1=Gs[bi].ap(0, [[1, 512]]), op=ALU.mult)
                for jj in range(2):
                    j = j0 + jj
                    for s in range(2):
                        for half in range(2):
                            Pp = PO[s * 2 + half]
                            kb.op("pe", "matmul", [wdd, Vbuf[bi]], [Pp], Pp[:, 0:512],
                                  lhsT=wdd[:, jj * 256 + s * 128:jj * 256 + (s + 1) * 128],
                                  rhs=Vbuf[bi][:, jj, half * 512:(half + 1) * 512], start=(j == 0), stop=(j == 127))
                gen = run_steps(gen, 4)
            for s in range(2):
                kb.dma("sp", xr[:], x1s.ap((t0 + s * 128) * D, [[D, 128], [1, D]]), [x1s], [xr])
                resid_ln(xr, PO[s * 2], PO[s * 2 + 1], 1024, 2048, 3072, ybuf, ybuf, stf, mvf, sdf, rstdf)
                kb.dma("pool", y.ap((t0 + s * 128) * D, [[D, 128], [1, D]]), ybuf[:], [ybuf], [y])
            while gen is not None:
                gen = run_steps(gen, 1000)
        kb.barrier()
    kb.mark("end")
    kb.wait_all("sp")
    kb.wait_all("pool")
    return nc, kb


def prep_shared(inp):
    f = lambda a: np.ascontiguousarray(a, dtype=np.float32)
    sh = {}
    sh["w_ada"] = f(inp["w_ada"][0])
    sh["b_ada"] = f(inp["b_ada"][0][None, :])
    sh["rows"] = f(np.concatenate([inp["ln1_g"][0], inp["ln1_b"][0], inp["ln2_g"][0], inp["ln2_b"][0],
                                   inp["sgu_ln_g"][0], inp["sgu_ln_b"][0]])[None, :])
    w_in = inp["w_in"][0]
    sh["wz"] = f(w_in[:, 1304:2328])
    sh["w_merge"] = f(inp["w_merge"][0])
    sh["b_merge_col"] = f(inp["b_merge"][0].reshape(16, 128).T)
    sh["w_b1"] = f(inp["w_branch"][0, 1])
    sh["w_out"] = f(inp["w_out"][0])
    sh["wsT"] = f(inp["sgu_w"][0].transpose(2, 0, 1).reshape(128, 1024))
    sh["sgub_col"] = f(inp["sgu_b"][0].T)
    jj, ii = np.meshgrid(np.arange(128), np.arange(128), indexing="ij")
    sh["trilm"] = f((jj <= ii).astype(np.float32))
    sh["peer_wq"] = f(inp["peer_wq"][0])
    sh["keysT"] = f(inp["peer_keys"][0].reshape(16, 128, 128).transpose(2, 0, 1).reshape(128, 2048))
    pu = inp["peer_u"][0].reshape(128, 128, 8, 128)
    sh["UT"] = f(pu.transpose(1, 3, 2, 0).reshape(128, 128, 1024))
    pv = inp["peer_v"][0].reshape(128, 128, 1024)
    sh["VJ"] = f(pv.transpose(1, 0, 2))
    def swp(c):
        blocks = [np.concatenate([c[:, i * 64 + 32:(i + 1) * 64], c[:, i * 64:i * 64 + 32]], axis=1) for i in range(c.shape[1] // 64)]
        return np.concatenate(blocks, axis=1)
    qch = [np.concatenate([w_in[:, r * 64:(r + 1) * 64], w_in[:, (4 + r) * 64:(5 + r) * 64]], axis=1) for r in range(4)]
    kcs = [w_in[:, 512:640], w_in[:, 768:896], w_in[:, 1024:1152]]
    chunks = qch + [swp(c) for c in qch] + kcs + [swp(c) for c in kcs] + [w_in[:, 640:768]]
    sh["WAf"] = f(np.concatenate(chunks, axis=1))
    sh["WAt"] = f(np.concatenate([w_in[:, 896:1024], w_in[:, 1152:1280], w_in[:, 1280:1304]], axis=1))
    dup = lambda a: np.concatenate([a, a], axis=0)
    w1 = inp["cmp_w1"][0]
    sh["w1d"] = f(np.stack([dup(w1[kv].reshape(32, 64, 128).transpose(1, 0, 2).reshape(64, 4096)) for kv in range(2)]))
    pos = inp["cmp_pos"][0]
    sh["posTd"] = f(dup(np.concatenate([pos[0].T, pos[1].T], axis=1)))
    sh["b1col"] = f(inp["cmp_b1"][0].T)
    w2 = inp["cmp_w2"][0]
    sh["w2kd"] = f(np.concatenate([w2[0], w2[0]], axis=1))
    sh["w2v"] = f(w2[1])
    sh["b2kcol"] = f(dup(inp["cmp_b2"][0][0][:, None]))
    sh["b2vrow"] = f(inp["cmp_b2"][0][1][None, :])
    sh["w_b0"] = f(inp["w_branch"][0, 0])
    return sh


def prep_consts(S):
    f = lambda a: np.ascontiguousarray(a, dtype=np.float32)
    NEG = -30000.0
    cst = {}
    p = np.arange(128)
    d = p % 64
    inv_freq = (np.float32(10000.0) ** (-(np.arange(32, dtype=np.float32)) / np.float32(32))).astype(np.float32)
    ang = (np.arange(S, dtype=np.float32)[None, :] * inv_freq[d % 32][:, None]).astype(np.float32)
    cst["cosT"] = f(np.cos(ang))
    sgn = np.where(d < 32, -1.0, 1.0).astype(np.float32)[:, None]
    cst["sinS"] = f(np.sin(ang) * sgn)
    ncmp = (S - 32) // 16 + 1
    nsel = S // 64
    c0 = np.arange(ncmp)[:, None] * 16
    s0 = np.arange(nsel)[None, :] * 64
    ov = np.clip(np.minimum(c0 + 32, s0 + 64) - np.maximum(c0, s0), 0, None) / 32.0
    agg = np.zeros((512, 128), np.float32)
    agg[:ncmp, :nsel] = ov
    cst["aggd"] = f(agg.reshape(4, 128, 128).transpose(1, 0, 2).reshape(128, 512))
    nl = np.arange(128)[:, None, None]
    m = np.arange(16)[None, :, None]
    ql = np.arange(128)[None, None, :]
    cst["cmpb"] = f(np.where(16 * nl + 31 - ql <= 128 * m, 0.0, NEG).reshape(128, 2048))
    kk = np.arange(128)[:, None]
    qq = np.arange(128)[None, :]
    cst["caus"] = f(np.concatenate([np.where(kk <= qq, 0.0, NEG), np.where(kk > qq, 0.0, NEG)], axis=1))
    q = np.arange(128)[:, None]
    c = np.arange(254)[None, :]
    dd = c - 126 - (q >= 64)
    base = np.where(dd > 0, -1e30, np.where(dd == 0, 2e9, np.where(dd == -1, 1e9, 0.0)))
    j0 = np.zeros((128, 128))
    j0[:, 0] = 3e9
    cst["based"] = f(np.concatenate([base, j0], axis=1))
    key = np.arange(S)[None, :]
    cst["ebig"] = f((key // 64 == np.arange(128)[:, None]).astype(np.float32))
    return cst


def kernel(**inputs):
    B, S = inputs["x"].shape[0], inputs["x"].shape[1]
    nc, kb = build(S)
    sh = prep_shared(inputs)
    sh.update(prep_consts(S))
    in_maps = []
    for b in range(B):
        m = dict(sh)
        m["x"] = np.ascontiguousarray(inputs["x"][b], dtype=np.float32)
        m["c_col"] = np.ascontiguousarray(inputs["c"][b].reshape(8, 128).T, dtype=np.float32)
        in_maps.append(m)
    res = run_bass_kernel_spmd(nc, in_maps, core_ids=list(range(B)))
    return np.stack([np.asarray(r["y"], dtype=np.float32) for r in res.results], axis=0)
```

```python
import numpy as np
import concourse.bass as bass
import concourse.mybir as mybir
from concourse.bass_utils import run_bass_kernel_spmd
from contextlib import ExitStack

F32 = mybir.dt.float32
BF16 = mybir.dt.bfloat16
U32 = mybir.dt.uint32
AF = mybir.ActivationFunctionType
ALU = mybir.AluOpType
AX = mybir.AxisListType

D = 1024
ALPHA = 2.0 ** 0.25
EPS = 1e-5


class Sem:
    def __init__(self, h, name):
        self.h = h
        self.name = name
        self.count = 0


class T:
    def __init__(self, t, name, shape, dt, space):
        self.t = t
        self.name = name
        self.shape = list(shape)
        self.dt = dt
        self.space = space
        self.w = {}
        self.r = {}
        self.dsem = None
        self.fsize = int(np.prod(shape[1:]))

    def __getitem__(self, idx):
        return self.t[idx]

    def ap(self, off, dims, parts=None, pstart=0):
        if self.space == "dram":
            return bass.AP(self.t, off, [list(d) for d in dims])
        if parts is None:
            parts = self.shape[0]
        return bass.AP(self.t, pstart * self.fsize + off, [[self.fsize, parts]] + [list(d) for d in dims])


class KB:
    def __init__(self, nc):
        self.nc = nc
        self.es = ExitStack()
        self.engs = {"pe": nc.tensor, "act": nc.scalar, "dve": nc.vector, "pool": nc.gpsimd, "sp": nc.sync}
        self.esem = {}
        self.waited = {k: {} for k in self.engs}
        self.all_sems = []
        for k in self.engs:
            self.esem[k] = self.new_sem("e_" + k)
        self.n_instr = 0
        self.n_wait = 0
        self.marks = []

    def new_sem(self, name):
        h = self.es.enter_context(self.nc.semaphore(name))
        s = Sem(h, name)
        self.all_sems.append(s)
        return s

    def sb(self, name, shape, dt, es=None):
        t = (es or self.es).enter_context(self.nc.sbuf_tensor(name, list(shape), dt))
        return T(t, name, shape, dt, "sb")

    def ps(self, name, shape, dt, es=None):
        t = (es or self.es).enter_context(self.nc.psum_tensor(name, list(shape), dt))
        return T(t, name, shape, dt, "ps")

    def dram(self, name, shape, dt, kind=None):
        if kind is None:
            t = self.nc.dram_tensor(name, list(shape), dt)
        else:
            t = self.nc.dram_tensor(name, list(shape), dt, kind=kind)
        return T(t, name, shape, dt, "dram")

    def _wait(self, e, sem, val):
        if val <= 0:
            return
        w = self.waited[e]
        if w.get(sem, 0) >= val:
            return
        self.engs[e].wait_ge(sem.h, val)
        w[sem] = val
        self.n_wait += 1

    def _deps(self, e, reads, writes):
        mysem = self.esem[e]
        for b in reads:
            for s, v in b.w.items():
                if s is mysem and e == "pe":
                    continue
                self._wait(e, s, v)
        for b in writes:
            for s, v in b.w.items():
                if s is mysem and e == "pe":
                    continue
                self._wait(e, s, v)
            for s, v in b.r.items():
                if s is mysem and e == "pe":
                    continue
                self._wait(e, s, v)

    def op(self, e, fn, reads, writes, *a, **kw):
        self._deps(e, reads, writes)
        ins = getattr(self.engs[e], fn)(*a, **kw)
        s = self.esem[e]
        s.count += 1
        ins.then_inc(s.h, 1)
        for b in reads:
            b.r[s] = s.count
        for b in writes:
            b.w[s] = s.count
        self.n_instr += 1
        return ins

    def dma(self, q, out_ap, in_ap, reads, writes, sem=None, **kw):
        if sem is None:
            tgt = writes[0]
            if tgt.dsem is None:
                tgt.dsem = self.new_sem("d_" + tgt.name)
            sem = tgt.dsem
        self._deps(q, reads, writes)
        ins = self.engs[q].dma_start(out=out_ap, in_=in_ap, **kw)
        sem.count += 16
        ins.then_inc(sem.h, 16)
        for b in reads:
            b.r[sem] = sem.count
        for b in writes:
            b.w[sem] = sem.count
        self.n_instr += 1
        return ins

    def mark(self, name):
        self.marks.append((name, self.n_instr, self.n_wait))

    def barrier(self):
        for e in self.engs:
            for s in self.all_sems:
                self._wait(e, s, s.count)

    def wait_all(self, e):
        for s in self.all_sems:
            self._wait(e, s, s.count)


def build(S, with_peer=True, with_nsa=True, stop_after=None):
    nc = bass.Bass("TRN2", target_bir_lowering=False)
    kb = KB(nc)
    NS = S // 128

    def din(name, shape, dt=F32):
        return kb.dram(name, shape, dt, kind="ExternalInput")

    x = din("x", [S, D])
    c_col = din("c_col", [128, 8])
    w_ada = din("w_ada", [1024, 6144])
    b_ada = din("b_ada", [1, 6144])
    rows = din("rows", [1, 5120])
    wz = din("wz", [1024, 1024])
    w_merge = din("w_merge", [1024, 2048])
    b_merge_col = din("b_merge_col", [128, 16])
    w_b1 = din("w_b1", [512, 1024])
    w_out = din("w_out", [1024, 1024])
    wsT = din("wsT", [128, 1024])
    sgub_col = din("sgub_col", [128, 8])
    trilm = din("trilm", [128, 128])
    peer_wq = din("peer_wq", [1024, 2048])
    keysT = din("keysT", [128, 2048])
    UT = din("UT", [128, 128, 1024])
    VJ = din("VJ", [128, 128, 1024])
    y = kb.dram("y", [S, D], F32, kind="ExternalOutput")
    x1s = kb.dram("x1s", [S, D], F32)
    UTb = kb.dram("UTb", [128, 128, 1024], BF16)
    VJb = kb.dram("VJb", [128, 128, 1024], BF16)
    NEG = -30000.0
    if with_nsa:
        WAf = din("WAf", [1024, 1920])
        WAt = din("WAt", [1024, 280])
        cosT = din("cosT", [128, S])
        sinS = din("sinS", [128, S])
        w1d = din("w1d", [2, 128, 4096])
        posTd = din("posTd", [128, 64])
        b1col = din("b1col", [128, 2])
        w2kd = din("w2kd", [128, 128])
        w2v = din("w2v", [128, 64])
        b2kcol = din("b2kcol", [128, 1])
        b2vrow = din("b2vrow", [1, 64])
        aggd = din("aggd", [128, 512])
        cmpb = din("cmpb", [128, 2048])
        caus = din("caus", [128, 256])
        based = din("based", [128, 382])
        ebig = din("ebig", [128, S])
        w_b0 = din("w_b0", [512, 1024])
        QTd = kb.dram("QTd", [128, 4 * S], BF16)
        ONd = kb.dram("ONd", [128, 4 * S], BF16)

    ident_f = kb.sb("ident_f", [128, 128], F32)
    ident_b = kb.sb("ident_b", [128, 128], BF16)
    ones_row = kb.sb("ones_row", [1, 128], F32)
    eps_col = kb.sb("eps_col", [128, 1], F32)
    modcol = kb.sb("modcol", [128, 32], F32)
    gt_bc = kb.sb("gt_bc", [128, 2048], F32)
    ln_bc = kb.sb("ln_bc", [128, 5120], F32)
    iota128 = kb.sb("iota128", [128, 128], F32)
    thr15 = kb.sb("thr15", [128, 15], F32)

    kb.op("pool", "memset", [], [ident_f], ident_f[:], 0.0)
    kb.op("pool", "affine_select", [ident_f], [ident_f], out=ident_f[:], in_=ident_f[:], pattern=[[-1, 128]],
          compare_op=ALU.not_equal, fill=1.0, base=0, channel_multiplier=1)
    kb.op("dve", "tensor_copy", [ident_f], [ident_b], out=ident_b[:], in_=ident_f[:])
    kb.op("pool", "memset", [], [ones_row], ones_row[:], 1.0)
    kb.op("pool", "memset", [], [eps_col], eps_col[:], EPS)
    kb.op("pool", "iota", [], [iota128], iota128[:], [[1, 128]], base=0, channel_multiplier=0,
          allow_small_or_imprecise_dtypes=True)
    kb.op("pool", "iota", [], [thr15], thr15[:], [[16, 15]], base=16, channel_multiplier=0,
          allow_small_or_imprecise_dtypes=True)

    def wslab(src, k, n, c0, w):
        return src.ap(c0, [[n, 128], [128 * n, k], [1, w]])

    kb.mark("P")
    with ExitStack() as es:
        wa = [kb.sb("wa%d" % i, [128, 8, 512], F32, es) for i in range(2)]
        mod_row = kb.sb("mod_row", [1, 6144], F32, es)
        b_row = kb.sb("b_row", [1, 6144], F32, es)
        rows_sb = kb.sb("rows_sb", [1, 5120], F32, es)
        ccol = kb.sb("ccol", [128, 8], F32, es)
        scol = kb.sb("scol", [128, 8], F32, es)
        P0 = kb.ps("pP0", [128, 512], F32, es)
        P1 = kb.ps("pP1", [128, 512], F32, es)
        kb.dma("sp", ccol[:], c_col[:], [c_col], [ccol])
        kb.dma("sp", b_row[:], b_ada[:], [b_ada], [b_row])
        kb.dma("sp", rows_sb[:], rows[:], [rows], [rows_sb])
        kb.op("act", "activation", [ccol], [scol], out=scol[:], in_=ccol[:], func=AF.Silu)
        for blk in range(12):
            wt = wa[blk % 2]
            kb.dma("sp", wt[:], wslab(w_ada, 8, 6144, blk * 512, 512), [w_ada], [wt])
            Pb = P0 if blk % 2 == 0 else P1
            for k in range(8):
                kb.op("pe", "matmul", [scol, wt], [Pb], Pb[0:1, 0:512], lhsT=scol[:, k:k + 1], rhs=wt[:, k, :],
                      start=(k == 0), stop=(k == 7))
            kb.op("dve", "tensor_tensor", [b_row], [mod_row, Pb], out=mod_row[0:1, blk * 512:(blk + 1) * 512],
                  in0=Pb[0:1, 0:512], in1=b_row[0:1, blk * 512:(blk + 1) * 512], op=ALU.add)
        for i, c0 in enumerate([2048, 2560, 5120, 5632]):
            Pb = P0 if i % 2 == 0 else P1
            kb.op("pe", "matmul", [ones_row, mod_row], [Pb], Pb[:, 0:512], lhsT=ones_row[0:1, :],
                  rhs=mod_row[0:1, c0:c0 + 512], start=True, stop=True)
            kb.op("act", "copy", [], [gt_bc, Pb], out=gt_bc[:, i * 512:(i + 1) * 512], in_=Pb[:, 0:512])
        for i in range(10):
            Pb = P0 if i % 2 == 0 else P1
            kb.op("pe", "matmul", [ones_row, rows_sb], [Pb], Pb[:, 0:512], lhsT=ones_row[0:1, :],
                  rhs=rows_sb[0:1, i * 512:(i + 1) * 512], start=True, stop=True)
            kb.op("act", "copy", [], [ln_bc, Pb], out=ln_bc[:, i * 512:(i + 1) * 512], in_=Pb[:, 0:512])
        chunks = list(range(0, 8)) + list(range(8, 16)) + list(range(24, 32)) + list(range(32, 40))
        for j, cch in enumerate(chunks):
            kb.op("pe", "matmul", [ones_row, mod_row], [P0], P0[:, j:j + 1], lhsT=mod_row[0:1, cch * 128:(cch + 1) * 128],
                  rhs=ones_row[0:1, 0:1], start=True, stop=True)
        kb.op("dve", "tensor_copy", [], [modcol, P0], out=modcol[:], in_=P0[:, 0:32])
        kb.op("dve", "tensor_scalar", [modcol], [modcol], out=modcol[:, 8:16], in0=modcol[:, 8:16], scalar1=1.0,
              scalar2=None, op0=ALU.add)
        kb.op("dve", "tensor_scalar", [modcol], [modcol], out=modcol[:, 24:32], in0=modcol[:, 24:32], scalar1=1.0,
              scalar2=None, op0=ALU.add)
        kb.barrier()

    def layernorm_stats(src, st, mv, sd, rstd):
        kb.op("dve", "bn_stats", [src], [st], out=st[:, 0:6], in_=src[:, 0:512])
        kb.op("dve", "bn_stats", [src], [st], out=st[:, 6:12], in_=src[:, 512:1024])
        kb.op("dve", "bn_aggr", [st], [mv], out=mv[:, 0:2], in_=st[:, 0:12])
        kb.op("act", "activation", [mv, eps_col], [sd], out=sd[:, 0:1], in_=mv[:, 1:2], func=AF.Sqrt,
              bias=eps_col[:, 0:1], scale=1.0)
        kb.op("dve", "reciprocal", [sd], [rstd], out=rstd[:, 0:1], in_=sd[:, 0:1])

    def ln_transpose(src, xn_b, PT, dstT, col0, ncols, sc_off, sh_off, st, mv, sd, rstd):
        layernorm_stats(src, st, mv, sd, rstd)
        kb.op("dve", "tensor_scalar", [src, mv, rstd], [xn_b], out=xn_b[:], in0=src[:], scalar1=mv[:, 0:1],
              scalar2=rstd[:, 0:1], op0=ALU.subtract, op1=ALU.mult)
        for k in range(8):
            kb.op("pe", "transpose", [xn_b, ident_b], [PT], PT[:, k * 128:(k + 1) * 128], xn_b[:, k * 128:(k + 1) * 128],
                  ident_b[:])
        for k in range(8):
            kb.op("act", "activation", [modcol], [dstT, PT], out=dstT[:, k, col0:col0 + 128],
                  in_=PT[:, k * 128:(k + 1) * 128], func=AF.Identity, bias=modcol[:, sh_off + k:sh_off + k + 1],
                  scale=modcol[:, sc_off + k:sc_off + k + 1])

    def resid_ln(xsrc, Pa, Pb, gt_off, g_off, b_off, ybuf, obuf, st, mv, sd, rstd):
        for half, Pp in enumerate([Pa, Pb]):
            kb.op("dve", "tensor_tensor", [gt_bc], [ybuf, Pp], out=ybuf[:, half * 512:(half + 1) * 512], in0=Pp[:, 0:512],
                  in1=gt_bc[:, gt_off + half * 512:gt_off + (half + 1) * 512], op=ALU.mult)
        kb.op("dve", "scalar_tensor_tensor", [xsrc, ybuf], [ybuf], out=ybuf[:], in0=xsrc[:], scalar=ALPHA, in1=ybuf[:],
              op0=ALU.mult, op1=ALU.add)
        layernorm_stats(ybuf, st, mv, sd, rstd)
        kb.op("dve", "scalar_tensor_tensor", [ybuf, mv, ln_bc], [obuf], out=obuf[:], in0=ybuf[:], scalar=mv[:, 0:1],
              in1=ln_bc[:, g_off:g_off + 1024], op0=ALU.subtract, op1=ALU.mult)
        kb.op("dve", "scalar_tensor_tensor", [obuf, rstd, ln_bc], [obuf], out=obuf[:], in0=obuf[:], scalar=rstd[:, 0:1],
              in1=ln_bc[:, b_off:b_off + 1024], op0=ALU.mult, op1=ALU.add)


    kb.mark("A")
    if with_nsa:
        NQ = S // 128
        es1 = ExitStack()
        KsT = kb.sb("KsT", [128, S], BF16, es1)
        KwT = kb.sb("KwT", [128, S], BF16, es1)
        VS = kb.sb("VS", [128, NQ * 130], BF16, es1)
        VW = kb.sb("VW", [128, NQ * 130], BF16, es1)
        Gt = kb.sb("Gt", [128, NQ * 24], F32, es1)
        KCT = kb.sb("KCT", [128, 512], BF16, es1)
        VC = kb.sb("VC", [128, 4 * 130], BF16, es1)
        kb.op("pool", "memset", [], [VS], VS[:], 1.0)
        kb.op("pool", "memset", [], [VW], VW[:], 1.0)
        kb.op("pool", "memset", [], [VC], VC[:], 1.0)
        kb.op("pool", "memset", [], [KCT], KCT[:], 0.0)
        with ExitStack() as es:
            KcR = kb.sb("KcR", [128, S], BF16, es)
            VcR = kb.sb("VcR", [128, S], BF16, es)
            xs1 = kb.sb("xsA", [128, 1024], F32, es)
            xn_b = kb.sb("xn_bA", [128, 1024], BF16, es)
            hT = kb.sb("hTA", [128, 8, 512], BF16, es)
            WAt_b = kb.sb("WAt_b", [128, 8, 280], BF16, es)
            wch = [kb.sb("wch%d" % i, [128, 8, 128], BF16, es) for i in range(4)]
            cs = kb.sb("cs", [128, 512], F32, es)
            sn = kb.sb("sn", [128, 512], F32, es)
            t1 = kb.sb("t1", [128, 512], F32, es)
            t2 = kb.sb("t2", [128, 512], F32, es)
            qtmp = [kb.sb("qtmp%d" % i, [128, 512], BF16, es) for i in range(2)]
            st = kb.sb("stA", [128, 12], F32, es)
            mv = kb.sb("mvA", [128, 2], F32, es)
            sd = kb.sb("sdA", [128, 1], F32, es)
            rstd = kb.sb("rstdA", [128, 1], F32, es)
            w1_b = [kb.sb("w1_b%d" % i, [128, 4096], BF16, es) for i in range(2)]
            posT_b = kb.sb("posT_b", [128, 64], BF16, es)
            b1c = kb.sb("b1c", [128, 2], F32, es)
            w2k_b = kb.sb("w2k_b", [128, 128], BF16, es)
            w2v_b = kb.sb("w2v_b", [128, 64], BF16, es)
            b2kc = kb.sb("b2kc", [128, 1], F32, es)
            b2v_b = kb.sb("b2v_b", [1, 64], BF16, es)
            ones_b = kb.sb("ones_b", [1, 128], BF16, es)
            hidT = kb.sb("hidT", [128, 512], BF16, es)
            biasv = kb.sb("biasv", [128, 1], F32, es)
            PT = kb.ps("PTA", [128, 1024], BF16, es)
            Pq = kb.ps("Pq", [128, 512], F32, es)
            Pqs = kb.ps("Pqs", [128, 512], F32, es)
            PV = kb.ps("PV", [128, 512], F32, es)

            kb.dma("pool", WAt_b[:], wslab(WAt, 8, 280, 0, 280), [WAt], [WAt_b])
            nw = 0
            for tg in range(S // 512):
                t0 = tg * 512
                for s in range(4):
                    kb.dma("sp", xs1[:], x.ap((t0 + s * 128) * D, [[D, 128], [1, D]]), [x], [xs1])
                    ln_transpose(xs1, xn_b, PT, hT, s * 128, 128, 8, 0, st, mv, sd, rstd)
                kb.dma("sp", cs[:], cosT.ap(t0, [[S, 128], [1, 512]]), [cosT], [cs])
                kb.dma("sp", sn[:], sinS.ap(t0, [[S, 128], [1, 512]]), [sinS], [sn])
                for s in range(4):
                    tile = tg * 4 + s
                    for k in range(8):
                        kb.op("pe", "matmul", [hT, WAt_b], [PV], PV[:, 0:280], lhsT=hT[:, k, s * 128:(s + 1) * 128],
                              rhs=WAt_b[:, k, :], start=(k == 0), stop=(k == 7))
                    kb.op("act", "copy", [], [VS, PV], out=VS.ap(tile * 130, [[65, 2], [1, 64]]),
                          in_=PV.ap(0, [[64, 2], [1, 64]]))
                    kb.op("act", "copy", [], [VW, PV], out=VW.ap(tile * 130, [[65, 2], [1, 64]]),
                          in_=PV.ap(128, [[64, 2], [1, 64]]))
                    kb.op("act", "activation", [], [Gt, PV], out=Gt.ap(tile * 24, [[1, 8], [8, 3]]),
                          in_=PV.ap(256, [[3, 8], [1, 3]]), func=AF.Sigmoid)
                jobs = [(c, c + 4, "q", c) for c in range(4)] + [(8, 11, "kc", 0), (9, 12, "ks", 0), (10, 13, "kw", 0)]
                for ji, (ca, cb_, kind, r) in enumerate(jobs):
                    wa_ = wch[nw % 4]
                    wb_ = wch[(nw + 1) % 4]
                    nw += 2
                    kb.dma("pool", wa_[:], wslab(WAf, 8, 1920, ca * 128, 128), [WAf], [wa_])
                    kb.dma("pool", wb_[:], wslab(WAf, 8, 1920, cb_ * 128, 128), [WAf], [wb_])
                    for (wt_, Pp) in ((wa_, Pq), (wb_, Pqs)):
                        for k in range(8):
                            kb.op("pe", "matmul", [hT, wt_], [Pp], Pp[:, 0:512], lhsT=wt_[:, k, :], rhs=hT[:, k, :],
                                  start=(k == 0), stop=(k == 7))
                    kb.op("dve", "tensor_tensor", [cs], [t1, Pq], out=t1[:], in0=Pq[:, 0:512], in1=cs[:], op=ALU.mult)
                    kb.op("dve", "tensor_tensor", [sn], [t2, Pqs], out=t2[:], in0=Pqs[:, 0:512], in1=sn[:], op=ALU.mult)
                    if kind == "q":
                        qb = qtmp[ji % 2]
                        kb.op("dve", "tensor_tensor", [t1, t2], [qb], out=qb[:], in0=t1[:], in1=t2[:], op=ALU.add)
                        kb.dma("sp", QTd.ap(r * S + t0, [[4 * S, 128], [1, 512]]), qb[:], [qb], [QTd])
                    else:
                        dstT = {"kc": KcR, "ks": KsT, "kw": KwT}[kind]
                        kb.op("dve", "tensor_tensor", [t1, t2], [dstT], out=dstT[:, t0:t0 + 512], in0=t1[:], in1=t2[:],
                              op=ALU.add)
                wv_ = wch[nw % 4]
                nw += 1
                kb.dma("pool", wv_[:], wslab(WAf, 8, 1920, 14 * 128, 128), [WAf], [wv_])
                for k in range(8):
                    kb.op("pe", "matmul", [hT, wv_], [Pq], Pq[:, 0:512], lhsT=wv_[:, k, :], rhs=hT[:, k, :],
                          start=(k == 0), stop=(k == 7))
                kb.op("act", "copy", [], [VcR, Pq], out=VcR[:, t0:t0 + 512], in_=Pq[:, 0:512])

            kb.mark("B")
            if S >= 512:
                ncmp = (S - 32) // 16 + 1
                for kv in range(2):
                    kb.dma("pool", w1_b[kv][:], w1d.ap(kv * 128 * 4096, [[4096, 128], [1, 4096]]), [w1d], [w1_b[kv]])
                kb.dma("pool", posT_b[:], posTd[:], [posTd], [posT_b])
                kb.dma("sp", b1c[:], b1col[:], [b1col], [b1c])
                kb.dma("pool", w2k_b[:], w2kd[:], [w2kd], [w2k_b])
                kb.dma("pool", w2v_b[:], w2v[:], [w2v], [w2v_b])
                kb.dma("sp", b2kc[:], b2kcol[:], [b2kcol], [b2kc])
                kb.dma("pool", b2v_b[:], b2vrow[:], [b2vrow], [b2v_b])
                kb.op("pool", "memset", [], [ones_b], ones_b[:], 1.0)
                kb.op("pool", "memset", [], [hidT], hidT[:], 0.0)
                nc_pad = min(ncmp, 511)
                for kv in range(2):
                    raw = KcR if kv == 0 else VcR
                    for p in range(32):
                        kb.op("pe", "matmul", [w1_b[kv], posT_b], [PV], PV[:, 0:1],
                              lhsT=w1_b[kv].ap(p * 128, [[1, 128]], parts=64), rhs=posT_b.ap(kv * 32 + p, [[1, 1]], parts=64),
                              start=(p == 0), stop=(p == 31))
                    kb.op("dve", "tensor_tensor", [b1c], [biasv, PV], out=biasv[:], in0=PV[:, 0:1], in1=b1c[:, kv:kv + 1],
                          op=ALU.add)
                    for g in range(2):
                        for p in range(32):
                            kb.op("pe", "matmul", [w1_b[kv], raw], [Pq], Pq[:, 0:nc_pad],
                                  lhsT=w1_b[kv].ap(p * 128, [[1, 128]], parts=64, pstart=g * 64),
                                  rhs=raw.ap(p, [[16, nc_pad]], parts=64, pstart=g * 64), start=(p == 0), stop=(p == 31))
                        kb.op("act", "activation", [biasv], [hidT, Pq], out=hidT[:, 0:nc_pad], in_=Pq[:, 0:nc_pad], func=AF.Gelu,
                              bias=biasv[:, 0:1], scale=1.0)
                        if kv == 0:
                            kb.op("pe", "matmul", [w2k_b, hidT], [Pqs], Pqs[:, 0:nc_pad], lhsT=w2k_b[:], rhs=hidT[:, 0:nc_pad],
                                  start=True, stop=True)
                            kb.op("act", "activation", [b2kc], [KCT, Pqs], out=KCT.ap(0, [[1, nc_pad]], parts=64, pstart=g * 64),
                                  in_=Pqs.ap(0, [[1, nc_pad]], parts=64, pstart=g * 64), func=AF.Identity,
                                  bias=b2kc.ap(0, [[1, 1]], parts=64, pstart=g * 64), scale=1.0)
                        else:
                            for nt in range(4):
                                kb.op("pe", "matmul", [hidT, w2v_b], [PV], PV[:, 0:64], lhsT=hidT[:, nt * 128:(nt + 1) * 128],
                                      rhs=w2v_b[:], start=True, stop=False)
                                kb.op("pe", "matmul", [ones_b, b2v_b], [PV], PV[:, 0:64], lhsT=ones_b[0:1, :], rhs=b2v_b[0:1, :],
                                      start=False, stop=True)
                                kb.op("act", "copy", [], [VC, PV], out=VC.ap((nt * 2 + g) * 65, [[1, 64]]), in_=PV[:, 0:64])
            kb.barrier()

        kb.mark("C1")
        with ExitStack() as es:
            E_b = kb.sb("E_b", [128, S], BF16, es)
            CMPB = kb.sb("CMPB", [128, 2048], BF16, es)
            CAUS = kb.sb("CAUS", [128, 256], BF16, es)
            AGG = kb.sb("AGG", [128, 512], BF16, es)
            BASE = kb.sb("BASE", [128, 382], F32, es)
            Qt = kb.sb("Qt", [128, 512], BF16, es)
            Pt = [kb.sb("Pt%d" % i, [128, 512], BF16, es) for i in range(3)]
            Osb = kb.sb("Osb", [128, 3 * 512], F32, es)
            onsa = kb.sb("onsa", [128, 512], F32, es)
            onT = kb.sb("onT", [128, 512], BF16, es)
            imp = kb.sb("imp", [128, 128], F32, es)
            val = kb.sb("val", [128, 128], F32, es)
            val2 = kb.sb("val2", [128, 128], F32, es)
            negs = kb.sb("negs", [128, 128], F32, es)
            negT = kb.sb("negT", [128, 128], BF16, es)
            m8 = kb.sb("m8", [128, 16], F32, es)
            zc = kb.sb("zc", [128, 4], F32, es)
            rzc = kb.sb("rzc", [128, 4], F32, es)
            zt = kb.sb("zt", [128, 4], F32, es)
            coef = kb.sb("coef", [128, 4], F32, es)
            PS = [kb.ps("PS%d" % i, [128, 512], F32, es) for i in range(3)]
            PO3 = [kb.ps("PO3_%d" % i, [128, 512], F32, es) for i in range(3)]
            PU = kb.ps("PU", [128, 512], F32, es)
            PTr = kb.ps("PTr", [128, 512], F32, es)

            kb.dma("pool", E_b[:], ebig[:], [ebig], [E_b])
            kb.dma("pool", CMPB[:], cmpb[:], [cmpb], [CMPB])
            kb.dma("pool", CAUS[:], caus[:], [caus], [CAUS])
            kb.dma("pool", AGG[:], aggd[:], [aggd], [AGG])
            kb.dma("sp", BASE[:], based[:], [based], [BASE])
            npt = [0]

            pend = []

            def flush(keep=0):
                while len(pend) > keep:
                    pend.pop(0)()

            def attn_tile(Pout, first, last, kT, kcol, Vt, voff, g, biases, extra=None):
                Ps = PS[npt[0] % 3]
                Pb = Pt[npt[0] % 3]
                npt[0] += 1
                nb = len(biases)
                for bi, (lt, lap, rt, rap) in enumerate(biases):
                    kb.op("pe", "matmul", [lt, rt], [Ps], Ps[:, 0:512], lhsT=lap, rhs=rap, start=(bi == 0), stop=False)
                kb.op("pe", "matmul", [kT, Qt], [Ps], Ps[:, 0:512], lhsT=kT.ap(kcol, [[1, 128]], parts=64, pstart=g * 64),
                      rhs=Qt.ap(0, [[128, 4], [1, 128]], parts=64, pstart=g * 64), start=(nb == 0), stop=True)
                kb.op("act", "activation", [], [Pb, Ps], out=Pb[:], in_=Ps[:, 0:512], func=AF.Exp, scale=0.125)
                flush(keep=1)

                def pv():
                    kb.op("pe", "matmul", [Vt, Pb], [Pout], Pout.ap(0, [[1, 512]], parts=65), lhsT=Vt.ap(voff, [[1, 65]]),
                          rhs=Pb[:], start=first, stop=last)
                    if extra is not None:
                        extra(Pb)
                pend.append(pv)

            for qt in range(NQ):
                kb.dma("sp", Qt.ap(0, [[128, 4], [1, 128]]), QTd.ap(qt * 128, [[4 * S, 128], [S, 4], [1, 128]]), [QTd], [Qt])
                for g in range(2):
                    ntmax = min(3, (8 * qt + 6) // 128)
                    for nt in range(ntmax + 1):
                        m = qt - 16 * nt
                        biases = []
                        if m <= 15:
                            biases.append((ident_b, ident_b[:], CMPB, CMPB.ap(m * 128, [[0, 4], [1, 128]])))
                        def imp_mm(Pb, nt=nt, ntmax=ntmax):
                            for r in range(4):
                                kb.op("pe", "matmul", [Pb, AGG], [PU], PU[:, r * 128:(r + 1) * 128], lhsT=Pb[:, r * 128:(r + 1) * 128],
                                      rhs=AGG[:, nt * 128:(nt + 1) * 128], start=(nt == 0 and r == 0), stop=(nt == ntmax and r == 3),
                                      skip_group_check=True)
                        attn_tile(PO3[0], nt == 0, nt == ntmax, KCT, nt * 128, VC, (nt * 2 + g) * 65, g, biases, extra=imp_mm)
                    flush()
                    kb.op("dve", "tensor_reduce", [], [zc, PU], out=zc[:], in_=PU.ap(0, [[128, 4], [1, 128]]), axis=AX.X, op=ALU.add)
                    kb.op("dve", "tensor_scalar", [zc], [zc], out=zc[:], in0=zc[:], scalar1=1e-30, scalar2=None, op0=ALU.max)
                    kb.op("dve", "reciprocal", [zc], [rzc], out=rzc[:], in_=zc[:])
                    kb.op("dve", "tensor_scalar", [rzc], [imp, PU], out=imp[:], in0=PU[:, 0:128], scalar1=rzc[:, 0:1], scalar2=None,
                          op0=ALU.mult)
                    for r in range(1, 4):
                        kb.op("dve", "scalar_tensor_tensor", [rzc, imp], [imp, PU], out=imp[:], in0=PU[:, r * 128:(r + 1) * 128],
                              scalar=rzc[:, r:r + 1], in1=imp[:], op0=ALU.mult, op1=ALU.add)
                    kb.op("dve", "tensor_tensor", [imp, BASE], [val], out=val[:], in0=imp[:], in1=BASE[:, 126 - 2 * qt:254 - 2 * qt],
                          op=ALU.add)
                    kb.op("dve", "tensor_tensor", [val, BASE], [val], out=val[:], in0=val[:], in1=BASE[:, 254:382], op=ALU.add)
                    kb.op("dve", "max", [val], [m8], out=m8[:, 0:8], in_=val[:])
                    kb.op("dve", "match_replace", [m8, val], [val2], out=val2[:], in_to_replace=m8[:, 0:8], in_values=val[:],
                          imm_value=-3e38)
                    kb.op("dve", "max", [val2], [m8], out=m8[:, 8:16], in_=val2[:])
                    kb.op("dve", "tensor_scalar", [val, m8], [negs], out=negs[:], in0=val[:], scalar1=m8[:, 15:16], scalar2=NEG,
                          op0=ALU.is_lt, op1=ALU.mult)
                    kb.op("pe", "transpose", [negs, ident_f], [PTr], PTr[:, 0:128], negs[:], ident_f[:])
                    kb.op("act", "copy", [], [negT, PTr], out=negT[:], in_=PTr[:, 0:128])
                    for kt in range(qt + 1):
                        biases = [(E_b, E_b[:, kt * 128:(kt + 1) * 128], negT, negT.ap(0, [[0, 4], [1, 128]]))]
                        if kt == qt:
                            biases.append((ident_b, ident_b[:], CAUS, CAUS.ap(0, [[0, 4], [1, 128]])))
                        attn_tile(PO3[1], kt == 0, kt == qt, KsT, kt * 128, VS, (kt * 2 + g) * 65, g, biases)
                    k0 = max(0, qt - 4)
                    for kt in range(k0, qt + 1):
                        biases = []
                        if kt == qt:
                            biases.append((ident_b, ident_b[:], CAUS, CAUS.ap(0, [[0, 4], [1, 128]])))
                        elif kt == qt - 4:
                            biases.append((ident_b, ident_b[:], CAUS, CAUS.ap(128, [[0, 4], [1, 128]])))
                        attn_tile(PO3[2], kt == k0, kt == qt, KwT, kt * 128, VW, (kt * 2 + g) * 65, g, biases)
                    flush()
                    for br in range(3):
                        kb.op("act", "copy", [], [Osb, PO3[br]], out=Osb.ap(br * 512, [[1, 512]], parts=65),
                              in_=PO3[br].ap(0, [[1, 512]], parts=65))
                    for br in range(3):
                        for r in range(4):
                            kb.op("pe", "transpose", [Osb, ident_f], [PTr], PTr[:, r * 65:(r + 1) * 65],
                                  Osb.ap(br * 512 + r * 128, [[1, 128]], parts=65), ident_f.ap(0, [[1, 65]], parts=65))
                        kb.op("dve", "tensor_scalar", [], [zt, PTr], out=zt[:], in0=PTr.ap(64, [[65, 4]]), scalar1=1e-30, scalar2=None,
                              op0=ALU.max)
                        kb.op("dve", "reciprocal", [zt], [zt], out=zt[:], in_=zt[:])
                        kb.op("dve", "tensor_tensor", [zt, Gt], [coef], out=coef[:], in0=zt[:],
                              in1=Gt[:, qt * 24 + br * 8 + g * 4:qt * 24 + br * 8 + g * 4 + 4], op=ALU.mult)
                        for r in range(4):
                            h = g * 4 + r
                            if br == 0:
                                kb.op("dve", "tensor_scalar", [coef], [onsa, PTr], out=onsa[:, h * 64:(h + 1) * 64],
                                      in0=PTr[:, r * 65:r * 65 + 64], scalar1=coef[:, r:r + 1], scalar2=None, op0=ALU.mult)
                            else:
                                kb.op("dve", "scalar_tensor_tensor", [coef, onsa], [onsa, PTr], out=onsa[:, h * 64:(h + 1) * 64],
                                      in0=PTr[:, r * 65:r * 65 + 64], scalar=coef[:, r:r + 1], in1=onsa[:, h * 64:(h + 1) * 64],
                                      op0=ALU.mult, op1=ALU.add)
                for kc in range(4):
                    kb.op("pe", "transpose", [onsa, ident_f], [PTr], PTr[:, kc * 128:(kc + 1) * 128], onsa[:, kc * 128:(kc + 1) * 128],
                          ident_f[:])
                kb.op("act", "copy", [], [onT, PTr], out=onT[:], in_=PTr[:, 0:512])
                kb.dma("sp", ONd.ap(qt * 128, [[4 * S, 128], [S, 4], [1, 128]]), onT.ap(0, [[128, 4], [1, 128]]), [onT], [ONd])
            kb.barrier()
        es1.close()

    kb.mark("W")
    if with_peer:
        with ExitStack() as es:
            cb = [kb.sb("cb%d" % i, [128, 2048], BF16, es) for i in range(4)]
            n = 0
            for src, dst in ((UT, UTb), (VJ, VJb)):
                for j2 in range(64):
                    b = cb[n % 4]
                    n += 1
                    kb.dma("pool", b.ap(0, [[1024, 2], [1, 1024]]), src.ap(j2 * 2 * 131072, [[1024, 128], [131072, 2], [1, 1024]]),
                           [src], [b])
                    kb.dma("sp", dst.ap(j2 * 2 * 131072, [[1024, 128], [131072, 2], [1, 1024]]), b.ap(0, [[1024, 2], [1, 1024]]),
                           [b], [dst])
            kb.barrier()

    kb.mark("C2")
    with ExitStack() as es:
        Wz_b = kb.sb("Wz_b", [128, 8, 1024], BF16, es)
        Wm_b = kb.sb("Wm_b", [128, 8, 2048], BF16, es)
        Wb1_b = kb.sb("Wb1_b", [128, 4, 1024], BF16, es)
        Wo_b = kb.sb("Wo_b", [128, 8, 1024], BF16, es)
        WsT_f = kb.sb("WsT_f", [128, 1024], F32, es)
        WsT_b = kb.sb("WsT_b", [128, 8, 128], BF16, es)
        tril_sb = kb.sb("tril_sb", [128, 128], F32, es)
        sgub = kb.sb("sgub", [128, 8], F32, es)
        bmcol = kb.sb("bmcol", [128, 16], F32, es)
        xs = [kb.sb("xs%d" % i, [128, 1024], F32, es) for i in range(4)]
        xn_b = kb.sb("xn_b", [128, 1024], BF16, es)
        hT = kb.sb("hT", [128, 8, 512], BF16, es)
        gab = kb.sb("gab", [128, 2, 512], BF16, es)
        u_b = kb.sb("u_b", [128, 512], BF16, es)
        v_f = kb.sb("v_f", [128, 512], F32, es)
        vb = kb.sb("vb", [128, 512], BF16, es)
        tmp = kb.sb("tmp", [128, 512], F32, es)
        osgu = kb.sb("osgu", [128, 512], BF16, es)
        osT = kb.sb("osT", [128, 4, 512], BF16, es)
        mT = kb.sb("mT", [128, 8, 512], BF16, es)
        ybuf = kb.sb("ybuf", [128, 1024], F32, es)
        x1 = kb.sb("x1", [128, 1024], F32, es)
        st = kb.sb("st", [128, 12], F32, es)
        mv = kb.sb("mv", [128, 2], F32, es)
        sd = kb.sb("sd", [128, 1], F32, es)
        rstd = kb.sb("rstd", [128, 1], F32, es)
        PT = kb.ps("PT", [128, 1024], BF16, es)
        PGA = kb.ps("PGA", [128, 512], F32, es)
        PGB = kb.ps("PGB", [128, 512], F32, es)
        PB_ = kb.ps("PB_", [128, 512], F32, es)
        if with_nsa:
            PA_ = kb.ps("PA_", [128, 512], F32, es)
            Wb0_b = kb.sb("Wb0_b", [128, 4, 1024], BF16, es)
            onsaT = kb.sb("onsaT", [128, 4, 512], BF16, es)
            tmpa = kb.sb("tmpa", [128, 512], F32, es)
            kb.dma("pool", Wb0_b[:], wslab(w_b0, 4, 1024, 0, 1024), [w_b0], [Wb0_b])
        PZ0 = kb.ps("PZ0", [128, 512], F32, es)
        PZ1 = kb.ps("PZ1", [128, 512], F32, es)
        PM = kb.ps("PM", [128, 512], F32, es)

        kb.dma("pool", Wz_b[:], wslab(wz, 8, 1024, 0, 1024), [wz], [Wz_b])
        kb.dma("pool", Wm_b[:], wslab(w_merge, 8, 2048, 0, 2048), [w_merge], [Wm_b])
        kb.dma("pool", Wb1_b[:], wslab(w_b1, 4, 1024, 0, 1024), [w_b1], [Wb1_b])
        kb.dma("pool", Wo_b[:], wslab(w_out, 8, 1024, 0, 1024), [w_out], [Wo_b])
        kb.dma("sp", WsT_f[:], wsT[:], [wsT], [WsT_f])
        kb.dma("sp", tril_sb[:], trilm[:], [trilm], [tril_sb])
        kb.dma("sp", sgub[:], sgub_col[:], [sgub_col], [sgub])
        kb.dma("sp", bmcol[:], b_merge_col[:], [b_merge_col], [bmcol])
        kb.op("dve", "tensor_tensor", [WsT_f, tril_sb], [WsT_b], out=WsT_b.ap(0, [[128, 8], [1, 128]]),
              in0=WsT_f.ap(0, [[128, 8], [1, 128]]), in1=tril_sb.ap(0, [[0, 8], [1, 128]]), op=ALU.mult)

        for tg in range(S // 512):
            t0 = tg * 512
            for s in range(4):
                kb.dma("sp", xs[s][:], x.ap((t0 + s * 128) * D, [[D, 128], [1, D]]), [x], [xs[s]])
            for s in range(4):
                ln_transpose(xs[s], xn_b, PT, hT, s * 128, 128, 8, 0, st, mv, sd, rstd)
            if with_nsa:
                kb.dma("sp", onsaT.ap(0, [[512, 4], [1, 512]]), ONd.ap(t0, [[4 * S, 128], [S, 4], [1, 512]]), [ONd], [onsaT])
            for s in range(4):
                for half, Pz in enumerate([PZ0, PZ1]):
                    for k in range(8):
                        kb.op("pe", "matmul", [hT, Wz_b], [Pz], Pz[:, 0:512], lhsT=hT[:, k, s * 128:(s + 1) * 128],
                              rhs=Wz_b[:, k, half * 512:(half + 1) * 512], start=(k == 0), stop=(k == 7))
                kb.op("act", "activation", [], [u_b, PZ0], out=u_b[:], in_=PZ0[:, 0:512], func=AF.Gelu)
                kb.op("act", "activation", [], [v_f, PZ1], out=v_f[:], in_=PZ1[:, 0:512], func=AF.Gelu)
                kb.op("dve", "bn_stats", [v_f], [st], out=st[:, 0:6], in_=v_f[:])
                kb.op("dve", "bn_aggr", [st], [mv], out=mv[:, 0:2], in_=st[:, 0:6])
                kb.op("act", "activation", [mv, eps_col], [sd], out=sd[:, 0:1], in_=mv[:, 1:2], func=AF.Sqrt,
                      bias=eps_col[:, 0:1], scale=1.0)
                kb.op("dve", "reciprocal", [sd], [rstd], out=rstd[:, 0:1], in_=sd[:, 0:1])
                kb.op("dve", "scalar_tensor_tensor", [v_f, mv, ln_bc], [tmp], out=tmp[:], in0=v_f[:], scalar=mv[:, 0:1],
                      in1=ln_bc[:, 4096:4608], op0=ALU.subtract, op1=ALU.mult)
                kb.op("dve", "scalar_tensor_tensor", [tmp, rstd, ln_bc], [vb], out=vb[:], in0=tmp[:], scalar=rstd[:, 0:1],
                      in1=ln_bc[:, 4608:5120], op0=ALU.mult, op1=ALU.add)
                for g in range(8):
                    kb.op("pe", "matmul", [WsT_b, vb], [PM], PM[:, g * 64:(g + 1) * 64], lhsT=WsT_b[:, g, :],
                          rhs=vb[:, g * 64:(g + 1) * 64], start=True, stop=True)
                kb.op("dve", "tensor_tensor", [sgub], [tmp, PM], out=tmp.ap(0, [[64, 8], [1, 64]]),
                      in0=PM.ap(0, [[64, 8], [1, 64]]), in1=sgub.ap(0, [[1, 8], [0, 64]]), op=ALU.add)
                kb.op("dve", "tensor_tensor", [tmp, u_b], [osgu], out=osgu[:], in0=tmp[:], in1=u_b[:], op=ALU.mult)
                for kc in range(4):
                    kb.op("pe", "transpose", [osgu, ident_b], [PT], PT[:, kc * 128:(kc + 1) * 128],
                          osgu[:, kc * 128:(kc + 1) * 128], ident_b[:])
                kb.op("act", "copy", [], [osT, PT], out=osT.ap(s * 128, [[512, 4], [1, 128]]),
                      in_=PT.ap(0, [[128, 4], [1, 128]]))
            for cch in range(8):
                for gi, (Pg, col) in enumerate([(PGA, cch), (PGB, 8 + cch)]):
                    if gi == 0 and not with_nsa:
                        continue
                    for k in range(8):
                        kb.op("pe", "matmul", [hT, Wm_b], [Pg], Pg[:, 0:512], lhsT=Wm_b[:, k, col * 128:(col + 1) * 128],
                              rhs=hT[:, k, :], start=(k == 0), stop=(k == 7))
                for kc in range(4):
                    kb.op("pe", "matmul", [osT, Wb1_b], [PB_], PB_[:, 0:512], lhsT=Wb1_b[:, kc, cch * 128:(cch + 1) * 128],
                          rhs=osT[:, kc, :], start=(kc == 0), stop=(kc == 3))
                kb.op("act", "activation", [bmcol], [gab, PGB], out=gab[:, 1, :], in_=PGB[:, 0:512], func=AF.Sigmoid,
                      bias=bmcol[:, 8 + cch:9 + cch], scale=1.0)
                if with_nsa:
                    for kc in range(4):
                        kb.op("pe", "matmul", [onsaT, Wb0_b], [PA_], PA_[:, 0:512], lhsT=Wb0_b[:, kc, cch * 128:(cch + 1) * 128],
                              rhs=onsaT[:, kc, :], start=(kc == 0), stop=(kc == 3))
                    kb.op("act", "activation", [bmcol], [gab, PGA], out=gab[:, 0, :], in_=PGA[:, 0:512], func=AF.Sigmoid,
                          bias=bmcol[:, cch:cch + 1], scale=1.0)
                    kb.op("dve", "tensor_tensor", [gab], [tmpa, PA_], out=tmpa[:], in0=PA_[:, 0:512], in1=gab[:, 0, :], op=ALU.mult)
                    kb.op("dve", "tensor_tensor", [gab], [tmp, PB_], out=tmp[:], in0=PB_[:, 0:512], in1=gab[:, 1, :], op=ALU.mult)
                    kb.op("dve", "tensor_tensor", [tmp, tmpa], [mT], out=mT[:, cch, :], in0=tmp[:], in1=tmpa[:], op=ALU.add)
                else:
                    kb.op("dve", "tensor_tensor", [gab], [mT, PB_], out=mT[:, cch, :], in0=PB_[:, 0:512], in1=gab[:, 1, :],
                          op=ALU.mult)
            for s in range(4):
                for half, Pz in enumerate([PZ0, PZ1]):
                    for k in range(8):
                        kb.op("pe", "matmul", [mT, Wo_b], [Pz], Pz[:, 0:512], lhsT=mT[:, k, s * 128:(s + 1) * 128],
                              rhs=Wo_b[:, k, half * 512:(half + 1) * 512], start=(k == 0), stop=(k == 7))
                resid_ln(xs[s], PZ0, PZ1, 0, 0, 1024, ybuf, x1, st, mv, sd, rstd)
                dst = x1s if with_peer else y
                kb.dma("pool", dst.ap((t0 + s * 128) * D, [[D, 128], [1, D]]), x1[:], [x1], [dst])
        kb.barrier()

    if not with_peer:
        kb.wait_all("sp")
        return nc, kb

    kb.mark("D")
    NG = S // 256
    GTd2 = [kb.dram("GTd%d" % i, [128, 128 * 256], BF16) for i in range(2)]
    with ExitStack() as es:
        Wq_b = kb.sb("Wq_b", [128, 8, 2048], BF16, es)
        keys_b = kb.sb("keys_b", [128, 16, 128], BF16, es)
        GT = kb.sb("GT", [128, 128 * 128], BF16, es)
        NBUF = 3
        Ubuf = [kb.sb("Ubuf%d" % i, [128, 2, 1024], BF16, es) for i in range(NBUF)]
        Vbuf = [kb.sb("Vbuf%d" % i, [128, 2, 1024], BF16, es) for i in range(NBUF)]
        Gs = [kb.sb("Gs%d" % i, [128, 2, 256], BF16, es) for i in range(NBUF)]
        x1t = [kb.sb("x1t%d" % i, [128, 1024], F32, es) for i in range(2)]
        xr = kb.sb("xr", [128, 1024], F32, es)
        h2T2 = [kb.sb("h2T%d" % i, [128, 8, 256], BF16, es) for i in range(2)]
        qT = kb.sb("qT", [128, 16, 256], BF16, es)
        sc = kb.sb("sc", [128, 4, 128], F32, es)
        scrA = kb.sb("scrA", [128, 2048], F32, es)
        scrB = kb.sb("scrB", [128, 2048], F32, es)
        t16 = kb.sb("t16", [128, 256], F32, es)
        i16 = kb.sb("i16", [128, 256], U32, es)
        i16f = kb.sb("i16f", [128, 256], F32, es)
        tv = kb.sb("tv", [128, 128], F32, es)
        pv = kb.sb("pv", [128, 128], U32, es)
        pvf = kb.sb("pvf", [128, 128], F32, es)
        ee = kb.sb("ee", [128, 128], F32, es)
        zz = kb.sb("zz", [128, 8], F32, es)
        rz = kb.sb("rz", [128, 8], F32, es)
        ak = kb.sb("ak", [128, 128], F32, es)
        bk = kb.sb("bk", [128, 128], F32, es)
        III = kb.sb("III", [128, 384], F32, es)
        ITs = kb.sb("ITs", [128, 384], F32, es)
        iota16 = kb.sb("iota16", [128, 16], F32, es)
        CH = 16
        Lb = kb.sb("Lb", [128, CH * 128], BF16, es)
        Rb = kb.sb("Rb", [128, CH * 128], BF16, es)
        actg = [kb.sb("actg%d" % i, [128, 512], BF16, es) for i in range(2)]
        wd = [kb.sb("wd%d" % i, [128, 512], BF16, es) for i in range(2)]
        ybuf = kb.sb("ybuf2", [128, 1024], F32, es)
        st = kb.sb("st2", [128, 12], F32, es)
        mv = kb.sb("mv2", [128, 2], F32, es)
        sd = kb.sb("sd2", [128, 1], F32, es)
        rstd = kb.sb("rstd2", [128, 1], F32, es)
        stf = kb.sb("st3", [128, 12], F32, es)
        mvf = kb.sb("mv3", [128, 2], F32, es)
        sdf = kb.sb("sd3", [128, 1], F32, es)
        rstdf = kb.sb("rstd3", [128, 1], F32, es)
        PO = [kb.ps("PO%d" % i, [128, 512], F32, es) for i in range(4)]
        PA = [kb.ps("PA%d" % i, [128, 512], F32, es) for i in range(2)]
        PG = [kb.ps("PG%d" % i, [128, 512], F32, es) for i in range(2)]

        kb.dma("pool", Wq_b[:], wslab(peer_wq, 8, 2048, 0, 2048), [peer_wq], [Wq_b])
        kb.dma("pool", keys_b.ap(0, [[1, 2048]]), keysT[:], [keysT], [keys_b])
        kb.op("pool", "iota", [], [iota16], iota16[:], [[1, 16]], base=0, channel_multiplier=0,
              allow_small_or_imprecise_dtypes=True)

        def ln_transpose_f(src, dstT, col0, sc_off, sh_off):
            layernorm_stats(src, st, mv, sd, rstd)
            kb.op("dve", "tensor_scalar", [src, mv, rstd], [scrA], out=scrA[:, 0:1024], in0=src[:], scalar1=mv[:, 0:1],
                  scalar2=rstd[:, 0:1], op0=ALU.subtract, op1=ALU.mult)
            for k in range(8):
                Pp = PG[k // 4]
                kb.op("pe", "transpose", [scrA, ident_f], [Pp], Pp[:, (k % 4) * 128:(k % 4 + 1) * 128],
                      scrA[:, k * 128:(k + 1) * 128], ident_f[:])
            for k in range(8):
                Pp = PG[k // 4]
                kb.op("act", "activation", [modcol], [dstT, Pp], out=dstT[:, k, col0:col0 + 128],
                      in_=Pp[:, (k % 4) * 128:(k % 4 + 1) * 128], func=AF.Identity,
                      bias=modcol[:, sh_off + k:sh_off + k + 1], scale=modcol[:, sc_off + k:sc_off + k + 1])

        def prep(tg):
            t0 = tg * 256
            h2T = h2T2[tg % 2]
            GTd = GTd2[tg % 2]
            for s in range(2):
                kb.dma("sp", x1t[s][:], x1s.ap((t0 + s * 128) * D, [[D, 128], [1, D]]), [x1s], [x1t[s]])
                ln_transpose_f(x1t[s], h2T, s * 128, 24, 16)
                yield
            for cch in range(16):
                Pq = PG[cch % 2]
                for k in range(8):
                    kb.op("pe", "matmul", [h2T, Wq_b], [Pq], Pq[:, 0:256], lhsT=Wq_b[:, k, cch * 128:(cch + 1) * 128],
                          rhs=h2T[:, k, :], start=(k == 0), stop=(k == 7))
                kb.op("act", "copy", [], [qT, Pq], out=qT[:, cch, :], in_=Pq[:, 0:256])
                yield
            for s in range(2):
                ts = slice(s * 128, (s + 1) * 128)
                for r4 in range(4):
                    Ps = PG[r4 % 2]
                    for rr in range(4):
                        r = r4 * 4 + rr
                        kb.op("pe", "matmul", [qT, keys_b], [Ps], Ps[:, rr * 128:(rr + 1) * 128], lhsT=qT[:, r, ts],
                              rhs=keys_b[:, r, :], start=True, stop=True)
                    kb.op("act", "copy", [], [sc, Ps], out=sc.ap(0, [[1, 512]]), in_=Ps[:, 0:512])
                    yield
                    for rr in range(4):
                        r = r4 * 4 + rr
                        kb.op("dve", "max", [sc], [t16], out=t16[:, r * 16:r * 16 + 8], in_=sc[:, rr, :])
                        kb.op("dve", "max_index", [t16, sc], [i16], out=i16[:, r * 16:r * 16 + 8],
                              in_max=t16[:, r * 16:r * 16 + 8], in_values=sc[:, rr, :])
                        kb.op("dve", "match_replace", [t16, sc], [scrA], out=scrA[:, rr * 128:(rr + 1) * 128],
                              in_to_replace=t16[:, r * 16:r * 16 + 8], in_values=sc[:, rr, :], imm_value=-1e30)
                        kb.op("dve", "max", [scrA], [t16], out=t16[:, r * 16 + 8:r * 16 + 16],
                              in_=scrA[:, rr * 128:(rr + 1) * 128])
                        kb.op("dve", "max_index", [t16, scrA], [i16], out=i16[:, r * 16 + 8:r * 16 + 16],
                              in_max=t16[:, r * 16 + 8:r * 16 + 16], in_values=scrA[:, rr * 128:(rr + 1) * 128])
                        yield
                kb.op("dve", "tensor_copy", [i16], [i16f], out=i16f[:], in_=i16[:])
                kb.op("dve", "tensor_tensor", [t16], [scrB], out=scrB.ap(0, [[256, 8], [16, 16], [1, 16]]),
                      in0=t16.ap(0, [[32, 8], [1, 16], [0, 16]]), in1=t16.ap(16, [[32, 8], [0, 16], [1, 16]]), op=ALU.add)
                yield
                for h in range(8):
                    cs_ = slice(h * 256, (h + 1) * 256)
                    kb.op("dve", "max", [scrB], [tv], out=tv[:, h * 16:h * 16 + 8], in_=scrB[:, cs_])
                    kb.op("dve", "max_index", [tv, scrB], [pv], out=pv[:, h * 16:h * 16 + 8], in_max=tv[:, h * 16:h * 16 + 8],
                          in_values=scrB[:, cs_])
                    kb.op("dve", "match_replace", [tv, scrB], [scrA], out=scrA[:, cs_], in_to_replace=tv[:, h * 16:h * 16 + 8],
                          in_values=scrB[:, cs_], imm_value=-1e30)
                    kb.op("dve", "max", [scrA], [tv], out=tv[:, h * 16 + 8:h * 16 + 16], in_=scrA[:, cs_])
                    kb.op("dve", "max_index", [tv, scrA], [pv], out=pv[:, h * 16 + 8:h * 16 + 16],
                          in_max=tv[:, h * 16 + 8:h * 16 + 16], in_values=scrA[:, cs_])
                    yield
                kb.op("dve", "tensor_tensor", [tv], [ee], out=ee.ap(0, [[16, 8], [1, 16]]), in0=tv.ap(0, [[16, 8], [1, 16]]),
                      in1=tv.ap(0, [[16, 8], [0, 16]]), op=ALU.subtract)
                kb.op("act", "activation", [ee], [ee], out=ee[:], in_=ee[:], func=AF.Exp)
                kb.op("dve", "tensor_reduce", [ee], [zz], out=zz[:], in_=ee.ap(0, [[16, 8], [1, 16]]), axis=AX.X, op=ALU.add)
                kb.op("dve", "reciprocal", [zz], [rz], out=rz[:], in_=zz[:])
                kb.op("dve", "tensor_tensor", [ee, rz], [III], out=III.ap(256, [[16, 8], [1, 16]]),
                      in0=ee.ap(0, [[16, 8], [1, 16]]), in1=rz.ap(0, [[1, 8], [0, 16]]), op=ALU.mult)
                yield
                kb.op("dve", "tensor_copy", [pv], [pvf], out=pvf[:], in_=pv[:])
                kb.op("dve", "tensor_tensor", [pvf, thr15], [scrA], out=scrA.ap(0, [[15, 128], [1, 15]]),
                      in0=pvf.ap(0, [[1, 128], [0, 15]]), in1=thr15.ap(0, [[0, 128], [1, 15]]), op=ALU.is_ge)
                kb.op("dve", "tensor_reduce", [scrA], [ak], out=ak[:], in_=scrA.ap(0, [[15, 128], [1, 15]]), axis=AX.X, op=ALU.add)
                kb.op("dve", "scalar_tensor_tensor", [ak, pvf], [bk], out=bk[:], in0=ak[:], scalar=-16.0, in1=pvf[:],
                      op0=ALU.mult, op1=ALU.add)
                yield
                for which, (sel, off) in enumerate([(ak, 0), (bk, 16)]):
                    kb.op("dve", "tensor_tensor", [iota16, sel], [scrA], out=scrA.ap(0, [[256, 8], [16, 16], [1, 16]]),
                          in0=iota16.ap(0, [[0, 8], [0, 16], [1, 16]]), in1=sel.ap(0, [[16, 8], [1, 16], [0, 16]]),
                          op=ALU.is_equal)
                    kb.op("dve", "tensor_tensor", [scrA, i16f], [scrB], out=scrB.ap(0, [[256, 8], [16, 16], [1, 16]]),
                          in0=scrA.ap(0, [[256, 8], [16, 16], [1, 16]]), in1=i16f.ap(off, [[32, 8], [0, 16], [1, 16]]),
                          op=ALU.mult)
                    kb.op("dve", "tensor_reduce", [scrB], [III], out=III.ap(which * 128, [[16, 8], [1, 16]]),
                          in_=scrB.ap(0, [[256, 8], [16, 16], [1, 16]]), axis=AX.X, op=ALU.add)
                    yield
                for i3 in range(3):
                    kb.op("pe", "transpose", [III, ident_f], [PG[0]], PG[0][:, i3 * 128:(i3 + 1) * 128],
                          III[:, i3 * 128:(i3 + 1) * 128], ident_f[:])
                kb.op("act", "copy", [], [ITs, PG[0]], out=ITs[:], in_=PG[0][:, 0:384])
                yield
                for ch in range(128 // CH):
                    kb.op("dve", "tensor_tensor", [iota128, ITs], [Lb], out=Lb.ap(0, [[128, CH], [1, 128]]),
                          in0=iota128.ap(0, [[0, CH], [1, 128]]), in1=ITs.ap(ch * CH, [[1, CH], [0, 128]]), op=ALU.is_equal)
                    kb.op("dve", "tensor_tensor", [iota128, ITs], [Rb], out=Rb.ap(0, [[128, CH], [1, 128]]),
                          in0=iota128.ap(0, [[0, CH], [1, 128]]), in1=ITs.ap(128 + ch * CH, [[1, CH], [0, 128]]), op=ALU.is_equal)
                    kb.op("dve", "tensor_tensor", [Rb, ITs], [Rb], out=Rb.ap(0, [[128, CH], [1, 128]]),
                          in0=Rb.ap(0, [[128, CH], [1, 128]]), in1=ITs.ap(256 + ch * CH, [[1, CH], [0, 128]]), op=ALU.mult)
                    yield
                    for t4 in range(CH // 4):
                        Pg = PG[t4 % 2]
                        for tt in range(4):
                            tl = t4 * 4 + tt
                            kb.op("pe", "matmul", [Lb, Rb], [Pg], Pg[:, tt * 128:(tt + 1) * 128], lhsT=Lb[:, tl * 128:(tl + 1) * 128],
                                  rhs=Rb[:, tl * 128:(tl + 1) * 128], start=True, stop=True)
                        tokb = ch * CH + t4 * 4
                        kb.op("act", "copy", [], [GT, Pg], out=GT.ap(tokb, [[1, 4], [128, 128]]), in_=Pg.ap(0, [[128, 4], [1, 128]]))
                        yield
                for jb in range(8):
                    kb.dma("pool", GTd.ap(jb * 16 * 256 + s * 128, [[128 * 256, 128], [256, 16], [1, 128]]),
                           GT.ap(jb * 16 * 128, [[128, 16], [1, 128]]), [GT], [GTd])
                yield

        def run_steps(gen, n):
            if gen is None:
                return None
            for _ in range(n):
                try:
                    next(gen)
                except StopIteration:
                    return None
            return gen

        gen = prep(0)
        while gen is not None:
            gen = run_steps(gen, 1000)
        for tg in range(NG):
            t0 = tg * 256
            h2T = h2T2[tg % 2]
            GTd = GTd2[tg % 2]
            gen = prep(tg + 1) if tg + 1 < NG else None
            for jp in range(64):
                bi = jp % NBUF
                j0 = jp * 2
                kb.dma("sp", Ubuf[bi].ap(0, [[1024, 2], [1, 1024]]), UTb.ap(j0 * 131072, [[1024, 128], [131072, 2], [1, 1024]]),
                       [UTb], [Ubuf[bi]])
                kb.dma("act", Vbuf[bi].ap(0, [[1024, 2], [1, 1024]]), VJb.ap(j0 * 131072, [[1024, 128], [131072, 2], [1, 1024]]),
                       [VJb], [Vbuf[bi]])
                kb.dma("sp", Gs[bi].ap(0, [[256, 2], [1, 256]]), GTd.ap(j0 * 256, [[128 * 256, 128], [256, 2], [1, 256]]),
                       [GTd], [Gs[bi]])
                Pa = PA[jp % 2]
                for jj in range(2):
                    for k in range(8):
                        kb.op("pe", "matmul", [h2T, Ubuf[bi]], [Pa], Pa[:, jj * 256:(jj + 1) * 256],
                              lhsT=Ubuf[bi][:, jj, k * 128:(k + 1) * 128], rhs=h2T[:, k, :], start=(k == 0), stop=(k == 7))
                ag = actg[jp % 2]
                wdd = wd[jp % 2]
                kb.op("act", "activation", [], [ag, Pa], out=ag[:], in_=Pa[:, 0:512], func=AF.Gelu)
                kb.op("dve", "tensor_tensor", [ag, Gs[bi]], [wdd], out=wdd[:], in0=ag[:], in1=Gs[bi].ap(0, [[1, 512]]), op=ALU.mult)
                for jj in range(2):
                    j = j0 + jj
                    for s in range(2):
                        for half in range(2):
                            Pp = PO[s * 2 + half]
                            kb.op("pe", "matmul", [wdd, Vbuf[bi]], [Pp], Pp[:, 0:512],
                                  lhsT=wdd[:, jj * 256 + s * 128:jj * 256 + (s + 1) * 128],
                                  rhs=Vbuf[bi][:, jj, half * 512:(half + 1) * 512], start=(j == 0), stop=(j == 127))
                gen = run_steps(gen, 4)
            for s in range(2):
                kb.dma("sp", xr[:], x1s.ap((t0 + s * 128) * D, [[D, 128], [1, D]]), [x1s], [xr])
                resid_ln(xr, PO[s * 2], PO[s * 2 + 1], 1024, 2048, 3072, ybuf, ybuf, stf, mvf, sdf, rstdf)
                kb.dma("pool", y.ap((t0 + s * 128) * D, [[D, 128], [1, D]]), ybuf[:], [ybuf], [y])
            while gen is not None:
                gen = run_steps(gen, 1000)
        kb.barrier()
    kb.mark("end")
    kb.wait_all("sp")
    kb.wait_all("pool")
    return nc, kb


def prep_shared(inp):
    f = lambda a: np.ascontiguousarray(a, dtype=np.float32)
    sh = {}
    sh["w_ada"] = f(inp["w_ada"][0])
    sh["b_ada"] = f(inp["b_ada"][0][None, :])
    sh["rows"] = f(np.concatenate([inp["ln1_g"][0], inp["ln1_b"][0], inp["ln2_g"][0], inp["ln2_b"][0],
                                   inp["sgu_ln_g"][0], inp["sgu_ln_b"][0]])[None, :])
    w_in = inp["w_in"][0]
    sh["wz"] = f(w_in[:, 1304:2328])
    sh["w_merge"] = f(inp["w_merge"][0])
    sh["b_merge_col"] = f(inp["b_merge"][0].reshape(16, 128).T)
    sh["w_b1"] = f(inp["w_branch"][0, 1])
    sh["w_out"] = f(inp["w_out"][0])
    sh["wsT"] = f(inp["sgu_w"][0].transpose(2, 0, 1).reshape(128, 1024))
    sh["sgub_col"] = f(inp["sgu_b"][0].T)
    jj, ii = np.meshgrid(np.arange(128), np.arange(128), indexing="ij")
    sh["trilm"] = f((jj <= ii).astype(np.float32))
    sh["peer_wq"] = f(inp["peer_wq"][0])
    sh["keysT"] = f(inp["peer_keys"][0].reshape(16, 128, 128).transpose(2, 0, 1).reshape(128, 2048))
    pu = inp["peer_u"][0].reshape(128, 128, 8, 128)
    sh["UT"] = f(pu.transpose(1, 3, 2, 0).reshape(128, 128, 1024))
    pv = inp["peer_v"][0].reshape(128, 128, 1024)
    sh["VJ"] = f(pv.transpose(1, 0, 2))
    def swp(c):
        blocks = [np.concatenate([c[:, i * 64 + 32:(i + 1) * 64], c[:, i * 64:i * 64 + 32]], axis=1) for i in range(c.shape[1] // 64)]
        return np.concatenate(blocks, axis=1)
    qch = [np.concatenate([w_in[:, r * 64:(r + 1) * 64], w_in[:, (4 + r) * 64:(5 + r) * 64]], axis=1) for r in range(4)]
    kcs = [w_in[:, 512:640], w_in[:, 768:896], w_in[:, 1024:1152]]
    chunks = qch + [swp(c) for c in qch] + kcs + [swp(c) for c in kcs] + [w_in[:, 640:768]]
    sh["WAf"] = f(np.concatenate(chunks, axis=1))
    sh["WAt"] = f(np.concatenate([w_in[:, 896:1024], w_in[:, 1152:1280], w_in[:, 1280:1304]], axis=1))
    dup = lambda a: np.concatenate([a, a], axis=0)
    w1 = inp["cmp_w1"][0]
    sh["w1d"] = f(np.stack([dup(w1[kv].reshape(32, 64, 128).transpose(1, 0, 2).reshape(64, 4096)) for kv in range(2)]))
    pos = inp["cmp_pos"][0]
    sh["posTd"] = f(dup(np.concatenate([pos[0].T, pos[1].T], axis=1)))
    sh["b1col"] = f(inp["cmp_b1"][0].T)
    w2 = inp["cmp_w2"][0]
    sh["w2kd"] = f(np.concatenate([w2[0], w2[0]], axis=1))
    sh["w2v"] = f(w2[1])
    sh["b2kcol"] = f(dup(inp["cmp_b2"][0][0][:, None]))
    sh["b2vrow"] = f(inp["cmp_b2"][0][1][None, :])
    sh["w_b0"] = f(inp["w_branch"][0, 0])
    return sh


def prep_consts(S):
    f = lambda a: np.ascontiguousarray(a, dtype=np.float32)
    NEG = -30000.0
    cst = {}
    p = np.arange(128)
    d = p % 64
    inv_freq = (np.float32(10000.0) ** (-(np.arange(32, dtype=np.float32)) / np.float32(32))).astype(np.float32)
    ang = (np.arange(S, dtype=np.float32)[None, :] * inv_freq[d % 32][:, None]).astype(np.float32)
    cst["cosT"] = f(np.cos(ang))
    sgn = np.where(d < 32, -1.0, 1.0).astype(np.float32)[:, None]
    cst["sinS"] = f(np.sin(ang) * sgn)
    ncmp = (S - 32) // 16 + 1
    nsel = S // 64
    c0 = np.arange(ncmp)[:, None] * 16
    s0 = np.arange(nsel)[None, :] * 64
    ov = np.clip(np.minimum(c0 + 32, s0 + 64) - np.maximum(c0, s0), 0, None) / 32.0
    agg = np.zeros((512, 128), np.float32)
    agg[:ncmp, :nsel] = ov
    cst["aggd"] = f(agg.reshape(4, 128, 128).transpose(1, 0, 2).reshape(128, 512))
    nl = np.arange(128)[:, None, None]
    m = np.arange(16)[None, :, None]
    ql = np.arange(128)[None, None, :]
    cst["cmpb"] = f(np.where(16 * nl + 31 - ql <= 128 * m, 0.0, NEG).reshape(128, 2048))
    kk = np.arange(128)[:, None]
    qq = np.arange(128)[None, :]
    cst["caus"] = f(np.concatenate([np.where(kk <= qq, 0.0, NEG), np.where(kk > qq, 0.0, NEG)], axis=1))
    q = np.arange(128)[:, None]
    c = np.arange(254)[None, :]
    dd = c - 126 - (q >= 64)
    base = np.where(dd > 0, -1e30, np.where(dd == 0, 2e9, np.where(dd == -1, 1e9, 0.0)))
    j0 = np.zeros((128, 128))
    j0[:, 0] = 3e9
    cst["based"] = f(np.concatenate([base, j0], axis=1))
    key = np.arange(S)[None, :]
    cst["ebig"] = f((key // 64 == np.arange(128)[:, None]).astype(np.float32))
    return cst


def kernel(**inputs):
    B, S = inputs["x"].shape[0], inputs["x"].shape[1]
    nc, kb = build(S)
    sh = prep_shared(inputs)
    sh.update(prep_consts(S))
    in_maps = []
    for b in range(B):
        m = dict(sh)
        m["x"] = np.ascontiguousarray(inputs["x"][b], dtype=np.float32)
        m["c_col"] = np.ascontiguousarray(inputs["c"][b].reshape(8, 128).T, dtype=np.float32)
        in_maps.append(m)
    res = run_bass_kernel_spmd(nc, in_maps, core_ids=list(range(B)))
    return np.stack([np.asarray(r["y"], dtype=np.float32) for r in res.results], axis=0)
```

```python
import numpy as np
import concourse.bass as bass
import concourse.mybir as mybir
from concourse.bass_utils import run_bass_kernel_spmd
from contextlib import ExitStack

F32 = mybir.dt.float32
BF16 = mybir.dt.bfloat16
U32 = mybir.dt.uint32
AF = mybir.ActivationFunctionType
ALU = mybir.AluOpType
AX = mybir.AxisListType

import os
PREP_STEPS = int(os.environ.get("PREP_STEPS", "4"))
D = 1024
ALPHA = 2.0 ** 0.25
EPS = 1e-5


class Sem:
    def __init__(self, h, name):
        self.h = h
        self.name = name
        self.count = 0


class T:
    def __init__(self, t, name, shape, dt, space):
        self.t = t
        self.name = name
        self.shape = list(shape)
        self.dt = dt
        self.space = space
        self.w = {}
        self.r = {}
        self.dsem = None
        self.fsize = int(np.prod(shape[1:]))

    def __getitem__(self, idx):
        return self.t[idx]

    def ap(self, off, dims, parts=None, pstart=0):
        if self.space == "dram":
            return bass.AP(self.t, off, [list(d) for d in dims])
        if parts is None:
            parts = self.shape[0]
        return bass.AP(self.t, pstart * self.fsize + off, [[self.fsize, parts]] + [list(d) for d in dims])


class KB:
    def __init__(self, nc):
        self.nc = nc
        self.es = ExitStack()
        self.engs = {"pe": nc.tensor, "act": nc.scalar, "dve": nc.vector, "pool": nc.gpsimd, "sp": nc.sync}
        self.esem = {}
        self.waited = {k: {} for k in self.engs}
        self.all_sems = []
        for k in self.engs:
            self.esem[k] = self.new_sem("e_" + k)
        self.n_instr = 0
        self.n_wait = 0
        self.marks = []
        self.pe_count = 0

    def new_sem(self, name):
        h = self.es.enter_context(self.nc.semaphore(name))
        s = Sem(h, name)
        self.all_sems.append(s)
        return s

    def sb(self, name, shape, dt, es=None):
        t = (es or self.es).enter_context(self.nc.sbuf_tensor(name, list(shape), dt))
        return T(t, name, shape, dt, "sb")

    def ps(self, name, shape, dt, es=None):
        t = (es or self.es).enter_context(self.nc.psum_tensor(name, list(shape), dt))
        return T(t, name, shape, dt, "ps")

    def dram(self, name, shape, dt, kind=None):
        if kind is None:
            t = self.nc.dram_tensor(name, list(shape), dt)
        else:
            t = self.nc.dram_tensor(name, list(shape), dt, kind=kind)
        return T(t, name, shape, dt, "dram")

    def _wait(self, e, sem, val):
        if val <= 0:
            return
        w = self.waited[e]
        if w.get(sem, 0) >= val:
            return
        self.engs[e].wait_ge(sem.h, val)
        w[sem] = val
        self.n_wait += 1

    def _deps(self, e, reads, writes):
        mysem = self.esem[e]
        for b in reads:
            for s, v in b.w.items():
                if s is mysem and e == "pe":
                    continue
                self._wait(e, s, v)
        for b in writes:
            for s, v in b.w.items():
                if s is mysem and e == "pe":
                    continue
                self._wait(e, s, v)
            for s, v in b.r.items():
                if s is mysem and e == "pe":
                    continue
                self._wait(e, s, v)

    def op(self, e, fn, reads, writes, *a, **kw):
        self._deps(e, reads, writes)
        ins = getattr(self.engs[e], fn)(*a, **kw)
        if e == "pe":
            self.pe_count += 1
        s = self.esem[e]
        s.count += 1
        ins.then_inc(s.h, 1)
        for b in reads:
            b.r[s] = s.count
        for b in writes:
            b.w[s] = s.count
        self.n_instr += 1
        return ins

    def dma(self, q, out_ap, in_ap, reads, writes, sem=None, **kw):
        if sem is None:
            tgt = writes[0]
            if tgt.dsem is None:
                tgt.dsem = self.new_sem("d_" + tgt.name)
            sem = tgt.dsem
        self._deps(q, reads, writes)
        ins = self.engs[q].dma_start(out=out_ap, in_=in_ap, **kw)
        sem.count += 16
        ins.then_inc(sem.h, 16)
        for b in reads:
            b.r[sem] = sem.count
        for b in writes:
            b.w[sem] = sem.count
        self.n_instr += 1
        return ins

    def mark(self, name):
        self.marks.append((name, self.n_instr, self.n_wait, self.pe_count))

    def barrier(self):
        for e in self.engs:
            for s in self.all_sems:
                self._wait(e, s, s.count)

    def wait_all(self, e):
        for s in self.all_sems:
            self._wait(e, s, s.count)


def build(S, with_peer=True, with_nsa=True, stop_after=None):
    nc = bass.Bass("TRN2", target_bir_lowering=False)
    kb = KB(nc)
    NS = S // 128

    def din(name, shape, dt=F32):
        return kb.dram(name, shape, dt, kind="ExternalInput")

    x = din("x", [S, D])
    c_col = din("c_col", [128, 8])
    w_ada = din("w_ada", [1024, 6144])
    b_ada = din("b_ada", [1, 6144])
    rows = din("rows", [1, 5120])
    wz = din("wz", [1024, 1024])
    w_merge = din("w_merge", [1024, 2048])
    b_merge_col = din("b_merge_col", [128, 16])
    w_b1 = din("w_b1", [512, 1024])
    w_out = din("w_out", [1024, 1024])
    wsT = din("wsT", [128, 1024])
    sgub_col = din("sgub_col", [128, 8])
    trilm = din("trilm", [128, 128])
    peer_wq = din("peer_wq", [1024, 2048])
    keysT = din("keysT", [128, 2048])
    UT = din("UT", [128, 128, 1024])
    VJ = din("VJ", [128, 128, 1024])
    y = kb.dram("y", [S, D], F32, kind="ExternalOutput")
    x1s = kb.dram("x1s", [S, D], F32)
    UTb = kb.dram("UTb", [128, 128, 1024], BF16)
    VJb = kb.dram("VJb", [128, 128, 1024], BF16)
    NEG = -30000.0
    if with_nsa:
        WAf = din("WAf", [1024, 1920])
        WAt = din("WAt", [1024, 280])
        cosT = din("cosT", [128, S])
        sinS = din("sinS", [128, S])
        w1d = din("w1d", [2, 128, 4096])
        posTd = din("posTd", [128, 64])
        b1col = din("b1col", [128, 2])
        w2kd = din("w2kd", [128, 128])
        w2v = din("w2v", [128, 64])
        b2kcol = din("b2kcol", [128, 1])
        b2vrow = din("b2vrow", [1, 64])
        aggd = din("aggd", [128, 512])
        cmpb = din("cmpb", [128, 2048])
        caus = din("caus", [128, 256])
        based = din("based", [128, 382])
        ebig = din("ebig", [128, S])
        w_b0 = din("w_b0", [512, 1024])
        QTd = kb.dram("QTd", [128, 4 * S], BF16)
        ONd = kb.dram("ONd", [128, 4 * S], BF16)

    ident_f = kb.sb("ident_f", [128, 128], F32)
    ident_b = kb.sb("ident_b", [128, 128], BF16)
    ones_row = kb.sb("ones_row", [1, 128], F32)
    eps_col = kb.sb("eps_col", [128, 1], F32)
    modcol = kb.sb("modcol", [128, 32], F32)
    gt_bc = kb.sb("gt_bc", [128, 2048], F32)
    ln_bc = kb.sb("ln_bc", [128, 5120], F32)
    iota128 = kb.sb("iota128", [128, 128], F32)
    thr15 = kb.sb("thr15", [128, 15], F32)

    kb.op("pool", "memset", [], [ident_f], ident_f[:], 0.0)
    kb.op("pool", "affine_select", [ident_f], [ident_f], out=ident_f[:], in_=ident_f[:], pattern=[[-1, 128]],
          compare_op=ALU.not_equal, fill=1.0, base=0, channel_multiplier=1)
    kb.op("dve", "tensor_copy", [ident_f], [ident_b], out=ident_b[:], in_=ident_f[:])
    kb.op("pool", "memset", [], [ones_row], ones_row[:], 1.0)
    kb.op("pool", "memset", [], [eps_col], eps_col[:], EPS)
    kb.op("pool", "iota", [], [iota128], iota128[:], [[1, 128]], base=0, channel_multiplier=0,
          allow_small_or_imprecise_dtypes=True)
    kb.op("pool", "iota", [], [thr15], thr15[:], [[16, 15]], base=16, channel_multiplier=0,
          allow_small_or_imprecise_dtypes=True)

    def wslab(src, k, n, c0, w):
        return src.ap(c0, [[n, 128], [128 * n, k], [1, w]])

    kb.mark("P")
    with ExitStack() as es:
        wa = [kb.sb("wa%d" % i, [128, 8, 512], F32, es) for i in range(2)]
        mod_row = kb.sb("mod_row", [1, 6144], F32, es)
        b_row = kb.sb("b_row", [1, 6144], F32, es)
        rows_sb = kb.sb("rows_sb", [1, 5120], F32, es)
        ccol = kb.sb("ccol", [128, 8], F32, es)
        scol = kb.sb("scol", [128, 8], F32, es)
        P0 = kb.ps("pP0", [128, 512], F32, es)
        P1 = kb.ps("pP1", [128, 512], F32, es)
        kb.dma("sp", ccol[:], c_col[:], [c_col], [ccol])
        kb.dma("sp", b_row[:], b_ada[:], [b_ada], [b_row])
        kb.dma("sp", rows_sb[:], rows[:], [rows], [rows_sb])
        kb.op("act", "activation", [ccol], [scol], out=scol[:], in_=ccol[:], func=AF.Silu)
        for blk in range(12):
            wt = wa[blk % 2]
            kb.dma("sp", wt[:], wslab(w_ada, 8, 6144, blk * 512, 512), [w_ada], [wt])
            Pb = P0 if blk % 2 == 0 else P1
            for k in range(8):
                kb.op("pe", "matmul", [scol, wt], [Pb], Pb[0:1, 0:512], lhsT=scol[:, k:k + 1], rhs=wt[:, k, :],
                      start=(k == 0), stop=(k == 7))
            kb.op("dve", "tensor_tensor", [b_row], [mod_row, Pb], out=mod_row[0:1, blk * 512:(blk + 1) * 512],
                  in0=Pb[0:1, 0:512], in1=b_row[0:1, blk * 512:(blk + 1) * 512], op=ALU.add)
        for i, c0 in enumerate([2048, 2560, 5120, 5632]):
            Pb = P0 if i % 2 == 0 else P1
            kb.op("pe", "matmul", [ones_row, mod_row], [Pb], Pb[:, 0:512], lhsT=ones_row[0:1, :],
                  rhs=mod_row[0:1, c0:c0 + 512], start=True, stop=True)
            kb.op("act", "copy", [], [gt_bc, Pb], out=gt_bc[:, i * 512:(i + 1) * 512], in_=Pb[:, 0:512])
        for i in range(10):
            Pb = P0 if i % 2 == 0 else P1
            kb.op("pe", "matmul", [ones_row, rows_sb], [Pb], Pb[:, 0:512], lhsT=ones_row[0:1, :],
                  rhs=rows_sb[0:1, i * 512:(i + 1) * 512], start=True, stop=True)
            kb.op("act", "copy", [], [ln_bc, Pb], out=ln_bc[:, i * 512:(i + 1) * 512], in_=Pb[:, 0:512])
        chunks = list(range(0, 8)) + list(range(8, 16)) + list(range(24, 32)) + list(range(32, 40))
        for j, cch in enumerate(chunks):
            kb.op("pe", "matmul", [ones_row, mod_row], [P0], P0[:, j:j + 1], lhsT=mod_row[0:1, cch * 128:(cch + 1) * 128],
                  rhs=ones_row[0:1, 0:1], start=True, stop=True)
        kb.op("dve", "tensor_copy", [], [modcol, P0], out=modcol[:], in_=P0[:, 0:32])
        kb.op("dve", "tensor_scalar", [modcol], [modcol], out=modcol[:, 8:16], in0=modcol[:, 8:16], scalar1=1.0,
              scalar2=None, op0=ALU.add)
        kb.op("dve", "tensor_scalar", [modcol], [modcol], out=modcol[:, 24:32], in0=modcol[:, 24:32], scalar1=1.0,
              scalar2=None, op0=ALU.add)
        kb.barrier()

    def layernorm_stats(src, st, mv, sd, rstd):
        kb.op("dve", "bn_stats", [src], [st], out=st[:, 0:6], in_=src[:, 0:512])
        kb.op("dve", "bn_stats", [src], [st], out=st[:, 6:12], in_=src[:, 512:1024])
        kb.op("dve", "bn_aggr", [st], [mv], out=mv[:, 0:2], in_=st[:, 0:12])
        kb.op("act", "activation", [mv, eps_col], [sd], out=sd[:, 0:1], in_=mv[:, 1:2], func=AF.Sqrt,
              bias=eps_col[:, 0:1], scale=1.0)
        kb.op("dve", "reciprocal", [sd], [rstd], out=rstd[:, 0:1], in_=sd[:, 0:1])

    def ln_transpose(src, xn_b, PT, dstT, col0, ncols, sc_off, sh_off, st, mv, sd, rstd):
        layernorm_stats(src, st, mv, sd, rstd)
        kb.op("dve", "tensor_scalar", [src, mv, rstd], [xn_b], out=xn_b[:], in0=src[:], scalar1=mv[:, 0:1],
              scalar2=rstd[:, 0:1], op0=ALU.subtract, op1=ALU.mult)
        for k in range(8):
            kb.op("pe", "transpose", [xn_b, ident_b], [PT], PT[:, k * 128:(k + 1) * 128], xn_b[:, k * 128:(k + 1) * 128],
                  ident_b[:])
        for k in range(8):
            kb.op("act", "activation", [modcol], [dstT, PT], out=dstT[:, k, col0:col0 + 128],
                  in_=PT[:, k * 128:(k + 1) * 128], func=AF.Identity, bias=modcol[:, sh_off + k:sh_off + k + 1],
                  scale=modcol[:, sc_off + k:sc_off + k + 1])

    def resid_ln(xsrc, Pa, Pb, gt_off, g_off, b_off, ybuf, obuf, st, mv, sd, rstd):
        for half, Pp in enumerate([Pa, Pb]):
            kb.op("dve", "tensor_tensor", [gt_bc], [ybuf, Pp], out=ybuf[:, half * 512:(half + 1) * 512], in0=Pp[:, 0:512],
                  in1=gt_bc[:, gt_off + half * 512:gt_off + (half + 1) * 512], op=ALU.mult)
        kb.op("dve", "scalar_tensor_tensor", [xsrc, ybuf], [ybuf], out=ybuf[:], in0=xsrc[:], scalar=ALPHA, in1=ybuf[:],
              op0=ALU.mult, op1=ALU.add)
        layernorm_stats(ybuf, st, mv, sd, rstd)
        kb.op("dve", "scalar_tensor_tensor", [ybuf, mv, ln_bc], [obuf], out=obuf[:], in0=ybuf[:], scalar=mv[:, 0:1],
              in1=ln_bc[:, g_off:g_off + 1024], op0=ALU.subtract, op1=ALU.mult)
        kb.op("dve", "scalar_tensor_tensor", [obuf, rstd, ln_bc], [obuf], out=obuf[:], in0=obuf[:], scalar=rstd[:, 0:1],
              in1=ln_bc[:, b_off:b_off + 1024], op0=ALU.mult, op1=ALU.add)


    kb.mark("A")
    if with_nsa:
        NQ = S // 128
        es1 = ExitStack()
        KsT = kb.sb("KsT", [128, S], BF16, es1)
        KwT = kb.sb("KwT", [128, S], BF16, es1)
        VS = kb.sb("VS", [128, NQ * 130], BF16, es1)
        VW = kb.sb("VW", [128, NQ * 130], BF16, es1)
        Gt = kb.sb("Gt", [128, NQ * 24], F32, es1)
        KCT = kb.sb("KCT", [128, 512], BF16, es1)
        VC = kb.sb("VC", [128, 4 * 130], BF16, es1)
        kb.op("pool", "memset", [], [VS], VS[:], 1.0)
        kb.op("pool", "memset", [], [VW], VW[:], 1.0)
        kb.op("pool", "memset", [], [VC], VC[:], 1.0)
        kb.op("pool", "memset", [], [KCT], KCT[:], 0.0)
        with ExitStack() as es:
            KcR = kb.sb("KcR", [128, S], BF16, es)
            VcR = kb.sb("VcR", [128, S], BF16, es)
            xs1 = kb.sb("xsA", [128, 1024], F32, es)
            xn_b = kb.sb("xn_bA", [128, 1024], BF16, es)
            hT = kb.sb("hTA", [128, 8, 512], BF16, es)
            WAt_b = kb.sb("WAt_b", [128, 8, 280], BF16, es)
            wch = [kb.sb("wch%d" % i, [128, 8, 128], BF16, es) for i in range(4)]
            cs = kb.sb("cs", [128, 512], F32, es)
            sn = kb.sb("sn", [128, 512], F32, es)
            t1 = kb.sb("t1", [128, 512], F32, es)
            t2 = kb.sb("t2", [128, 512], F32, es)
            qtmp = [kb.sb("qtmp%d" % i, [128, 512], BF16, es) for i in range(2)]
            st = kb.sb("stA", [128, 12], F32, es)
            mv = kb.sb("mvA", [128, 2], F32, es)
            sd = kb.sb("sdA", [128, 1], F32, es)
            rstd = kb.sb("rstdA", [128, 1], F32, es)
            w1_b = [kb.sb("w1_b%d" % i, [128, 4096], BF16, es) for i in range(2)]
            posT_b = kb.sb("posT_b", [128, 64], BF16, es)
            b1c = kb.sb("b1c", [128, 2], F32, es)
            w2k_b = kb.sb("w2k_b", [128, 128], BF16, es)
            w2v_b = kb.sb("w2v_b", [128, 64], BF16, es)
            b2kc = kb.sb("b2kc", [128, 1], F32, es)
            b2v_b = kb.sb("b2v_b", [1, 64], BF16, es)
            ones_b = kb.sb("ones_b", [1, 128], BF16, es)
            hidT = kb.sb("hidT", [128, 512], BF16, es)
            biasv = kb.sb("biasv", [128, 1], F32, es)
            PT = kb.ps("PTA", [128, 1024], BF16, es)
            Pq = kb.ps("Pq", [128, 512], F32, es)
            Pqs = kb.ps("Pqs", [128, 512], F32, es)
            PV = kb.ps("PV", [128, 512], F32, es)

            kb.dma("pool", WAt_b[:], wslab(WAt, 8, 280, 0, 280), [WAt], [WAt_b])
            nw = 0
            for tg in range(S // 512):
                t0 = tg * 512
                for s in range(4):
                    kb.dma("sp", xs1[:], x.ap((t0 + s * 128) * D, [[D, 128], [1, D]]), [x], [xs1])
                    ln_transpose(xs1, xn_b, PT, hT, s * 128, 128, 8, 0, st, mv, sd, rstd)
                kb.dma("sp", cs[:], cosT.ap(t0, [[S, 128], [1, 512]]), [cosT], [cs])
                kb.dma("sp", sn[:], sinS.ap(t0, [[S, 128], [1, 512]]), [sinS], [sn])
                for s in range(4):
                    tile = tg * 4 + s
                    for k in range(8):
                        kb.op("pe", "matmul", [hT, WAt_b], [PV], PV[:, 0:280], lhsT=hT[:, k, s * 128:(s + 1) * 128],
                              rhs=WAt_b[:, k, :], start=(k == 0), stop=(k == 7))
                    kb.op("act", "copy", [], [VS, PV], out=VS.ap(tile * 130, [[65, 2], [1, 64]]),
                          in_=PV.ap(0, [[64, 2], [1, 64]]))
                    kb.op("act", "copy", [], [VW, PV], out=VW.ap(tile * 130, [[65, 2], [1, 64]]),
                          in_=PV.ap(128, [[64, 2], [1, 64]]))
                    kb.op("act", "activation", [], [Gt, PV], out=Gt.ap(tile * 24, [[1, 8], [8, 3]]),
                          in_=PV.ap(256, [[3, 8], [1, 3]]), func=AF.Sigmoid)
                jobs = [(c, c + 4, "q", c) for c in range(4)] + [(8, 11, "kc", 0), (9, 12, "ks", 0), (10, 13, "kw", 0)]
                for ji, (ca, cb_, kind, r) in enumerate(jobs):
                    wa_ = wch[nw % 4]
                    wb_ = wch[(nw + 1) % 4]
                    nw += 2
                    kb.dma("pool", wa_[:], wslab(WAf, 8, 1920, ca * 128, 128), [WAf], [wa_])
                    kb.dma("pool", wb_[:], wslab(WAf, 8, 1920, cb_ * 128, 128), [WAf], [wb_])
                    for (wt_, Pp) in ((wa_, Pq), (wb_, Pqs)):
                        for k in range(8):
                            kb.op("pe", "matmul", [hT, wt_], [Pp], Pp[:, 0:512], lhsT=wt_[:, k, :], rhs=hT[:, k, :],
                                  start=(k == 0), stop=(k == 7))
                    kb.op("dve", "tensor_tensor", [cs], [t1, Pq], out=t1[:], in0=Pq[:, 0:512], in1=cs[:], op=ALU.mult)
                    kb.op("dve", "tensor_tensor", [sn], [t2, Pqs], out=t2[:], in0=Pqs[:, 0:512], in1=sn[:], op=ALU.mult)
                    if kind == "q":
                        qb = qtmp[ji % 2]
                        kb.op("dve", "tensor_tensor", [t1, t2], [qb], out=qb[:], in0=t1[:], in1=t2[:], op=ALU.add)
                        kb.dma("sp", QTd.ap(r * S + t0, [[4 * S, 128], [1, 512]]), qb[:], [qb], [QTd])
                    else:
                        dstT = {"kc": KcR, "ks": KsT, "kw": KwT}[kind]
                        kb.op("dve", "tensor_tensor", [t1, t2], [dstT], out=dstT[:, t0:t0 + 512], in0=t1[:], in1=t2[:],
                              op=ALU.add)
                wv_ = wch[nw % 4]
                nw += 1
                kb.dma("pool", wv_[:], wslab(WAf, 8, 1920, 14 * 128, 128), [WAf], [wv_])
                for k in range(8):
                    kb.op("pe", "matmul", [hT, wv_], [Pq], Pq[:, 0:512], lhsT=wv_[:, k, :], rhs=hT[:, k, :],
                          start=(k == 0), stop=(k == 7))
                kb.op("act", "copy", [], [VcR, Pq], out=VcR[:, t0:t0 + 512], in_=Pq[:, 0:512])

            kb.mark("B")
            if S >= 512:
                ncmp = (S - 32) // 16 + 1
                for kv in range(2):
                    kb.dma("pool", w1_b[kv][:], w1d.ap(kv * 128 * 4096, [[4096, 128], [1, 4096]]), [w1d], [w1_b[kv]])
                kb.dma("pool", posT_b[:], posTd[:], [posTd], [posT_b])
                kb.dma("sp", b1c[:], b1col[:], [b1col], [b1c])
                kb.dma("pool", w2k_b[:], w2kd[:], [w2kd], [w2k_b])
                kb.dma("pool", w2v_b[:], w2v[:], [w2v], [w2v_b])
                kb.dma("sp", b2kc[:], b2kcol[:], [b2kcol], [b2kc])
                kb.dma("pool", b2v_b[:], b2vrow[:], [b2vrow], [b2v_b])
                kb.op("pool", "memset", [], [ones_b], ones_b[:], 1.0)
                kb.op("pool", "memset", [], [hidT], hidT[:], 0.0)
                nc_pad = min(ncmp, 511)
                for kv in range(2):
                    raw = KcR if kv == 0 else VcR
                    for p in range(32):
                        kb.op("pe", "matmul", [w1_b[kv], posT_b], [PV], PV[:, 0:1],
                              lhsT=w1_b[kv].ap(p * 128, [[1, 128]], parts=64), rhs=posT_b.ap(kv * 32 + p, [[1, 1]], parts=64),
                              start=(p == 0), stop=(p == 31))
                    kb.op("dve", "tensor_tensor", [b1c], [biasv, PV], out=biasv[:], in0=PV[:, 0:1], in1=b1c[:, kv:kv + 1],
                          op=ALU.add)
                    for g in range(2):
                        for p in range(32):
                            kb.op("pe", "matmul", [w1_b[kv], raw], [Pq], Pq[:, 0:nc_pad],
                                  lhsT=w1_b[kv].ap(p * 128, [[1, 128]], parts=64, pstart=g * 64),
                                  rhs=raw.ap(p, [[16, nc_pad]], parts=64, pstart=g * 64), start=(p == 0), stop=(p == 31))
                        kb.op("act", "activation", [biasv], [hidT, Pq], out=hidT[:, 0:nc_pad], in_=Pq[:, 0:nc_pad], func=AF.Gelu,
                              bias=biasv[:, 0:1], scale=1.0)
                        if kv == 0:
                            kb.op("pe", "matmul", [w2k_b, hidT], [Pqs], Pqs[:, 0:nc_pad], lhsT=w2k_b[:], rhs=hidT[:, 0:nc_pad],
                                  start=True, stop=True)
                            kb.op("act", "activation", [b2kc], [KCT, Pqs], out=KCT.ap(0, [[1, nc_pad]], parts=64, pstart=g * 64),
                                  in_=Pqs.ap(0, [[1, nc_pad]], parts=64, pstart=g * 64), func=AF.Identity,
                                  bias=b2kc.ap(0, [[1, 1]], parts=64, pstart=g * 64), scale=1.0)
                        else:
                            for nt in range(4):
                                kb.op("pe", "matmul", [hidT, w2v_b], [PV], PV[:, 0:64], lhsT=hidT[:, nt * 128:(nt + 1) * 128],
                                      rhs=w2v_b[:], start=True, stop=False)
                                kb.op("pe", "matmul", [ones_b, b2v_b], [PV], PV[:, 0:64], lhsT=ones_b[0:1, :], rhs=b2v_b[0:1, :],
                                      start=False, stop=True)
                                kb.op("act", "copy", [], [VC, PV], out=VC.ap((nt * 2 + g) * 65, [[1, 64]]), in_=PV[:, 0:64])
            kb.barrier()

        kb.mark("C1")
        with ExitStack() as es:
            E_b = kb.sb("E_b", [128, S], BF16, es)
            CMPB = kb.sb("CMPB", [128, 2048], BF16, es)
            CAUS = kb.sb("CAUS", [128, 256], BF16, es)
            AGG = kb.sb("AGG", [128, 512], BF16, es)
            BASE = kb.sb("BASE", [128, 382], F32, es)
            Qt = kb.sb("Qt", [128, 512], BF16, es)
            Pt = [kb.sb("Pt%d" % i, [128, 512], BF16, es) for i in range(3)]
            Osb = kb.sb("Osb", [128, 3 * 512], F32, es)
            onsa = kb.sb("onsa", [128, 512], F32, es)
            onT = kb.sb("onT", [128, 512], BF16, es)
            imp = kb.sb("imp", [128, 128], F32, es)
            val = kb.sb("val", [128, 128], F32, es)
            val2 = kb.sb("val2", [128, 128], F32, es)
            negs = kb.sb("negs", [128, 128], F32, es)
            negT = kb.sb("negT", [128, 128], BF16, es)
            m8 = kb.sb("m8", [128, 16], F32, es)
            zc = kb.sb("zc", [128, 4], F32, es)
            rzc = kb.sb("rzc", [128, 4], F32, es)
            zt = kb.sb("zt", [128, 4], F32, es)
            coef = kb.sb("coef", [128, 4], F32, es)
            PS = [kb.ps("PS%d" % i, [128, 512], F32, es) for i in range(3)]
            PO3 = [kb.ps("PO3_%d" % i, [128, 512], F32, es) for i in range(3)]
            PU = kb.ps("PU", [128, 512], F32, es)
            PTr = kb.ps("PTr", [128, 512], F32, es)

            kb.dma("pool", E_b[:], ebig[:], [ebig], [E_b])
            kb.dma("pool", CMPB[:], cmpb[:], [cmpb], [CMPB])
            kb.dma("pool", CAUS[:], caus[:], [caus], [CAUS])
            kb.dma("pool", AGG[:], aggd[:], [aggd], [AGG])
            kb.dma("sp", BASE[:], based[:], [based], [BASE])
            npt = [0]

            pend = []

            def flush(keep=0):
                while len(pend) > keep:
                    pend.pop(0)()

            def attn_tile(Pout, first, last, kT, kcol, Vt, voff, g, biases, extra=None):
                Ps = PS[npt[0] % 3]
                Pb = Pt[npt[0] % 3]
                npt[0] += 1
                nb = len(biases)
                for bi, (lt, lap, rt, rap) in enumerate(biases):
                    kb.op("pe", "matmul", [lt, rt], [Ps], Ps[:, 0:512], lhsT=lap, rhs=rap, start=(bi == 0), stop=False)
                kb.op("pe", "matmul", [kT, Qt], [Ps], Ps[:, 0:512], lhsT=kT.ap(kcol, [[1, 128]], parts=64, pstart=g * 64),
                      rhs=Qt.ap(0, [[128, 4], [1, 128]], parts=64, pstart=g * 64), start=(nb == 0), stop=True)
                kb.op("act", "activation", [], [Pb, Ps], out=Pb[:], in_=Ps[:, 0:512], func=AF.Exp, scale=0.125)
                flush(keep=1)

                def pv():
                    kb.op("pe", "matmul", [Vt, Pb], [Pout], Pout.ap(0, [[1, 512]], parts=65), lhsT=Vt.ap(voff, [[1, 65]]),
                          rhs=Pb[:], start=first, stop=last)
                    if extra is not None:
                        extra(Pb)
                pend.append(pv)

            for qt in range(NQ):
                kb.dma("sp", Qt.ap(0, [[128, 4], [1, 128]]), QTd.ap(qt * 128, [[4 * S, 128], [S, 4], [1, 128]]), [QTd], [Qt])
                for g in range(2):
                    ntmax = min(3, (8 * qt + 6) // 128)
                    for nt in range(ntmax + 1):
                        m = qt - 16 * nt
                        biases = []
                        if m <= 15:
                            biases.append((ident_b, ident_b[:], CMPB, CMPB.ap(m * 128, [[0, 4], [1, 128]])))
                        def imp_mm(Pb, nt=nt, ntmax=ntmax):
                            for r in range(4):
                                kb.op("pe", "matmul", [Pb, AGG], [PU], PU[:, r * 128:(r + 1) * 128], lhsT=Pb[:, r * 128:(r + 1) * 128],
                                      rhs=AGG[:, nt * 128:(nt + 1) * 128], start=(nt == 0 and r == 0), stop=(nt == ntmax and r == 3),
                                      skip_group_check=True)
                        attn_tile(PO3[0], nt == 0, nt == ntmax, KCT, nt * 128, VC, (nt * 2 + g) * 65, g, biases, extra=imp_mm)
                    flush()
                    kb.op("dve", "tensor_reduce", [], [zc, PU], out=zc[:], in_=PU.ap(0, [[128, 4], [1, 128]]), axis=AX.X, op=ALU.add)
                    kb.op("dve", "tensor_scalar", [zc], [zc], out=zc[:], in0=zc[:], scalar1=1e-30, scalar2=None, op0=ALU.max)
                    kb.op("dve", "reciprocal", [zc], [rzc], out=rzc[:], in_=zc[:])
                    kb.op("dve", "tensor_scalar", [rzc], [imp, PU], out=imp[:], in0=PU[:, 0:128], scalar1=rzc[:, 0:1], scalar2=None,
                          op0=ALU.mult)
                    for r in range(1, 4):
                        kb.op("dve", "scalar_tensor_tensor", [rzc, imp], [imp, PU], out=imp[:], in0=PU[:, r * 128:(r + 1) * 128],
                              scalar=rzc[:, r:r + 1], in1=imp[:], op0=ALU.mult, op1=ALU.add)
                    kb.op("dve", "tensor_tensor", [imp, BASE], [val], out=val[:], in0=imp[:], in1=BASE[:, 126 - 2 * qt:254 - 2 * qt],
                          op=ALU.add)
                    kb.op("dve", "tensor_tensor", [val, BASE], [val], out=val[:], in0=val[:], in1=BASE[:, 254:382], op=ALU.add)
                    kb.op("dve", "max", [val], [m8], out=m8[:, 0:8], in_=val[:])
                    kb.op("dve", "match_replace", [m8, val], [val2], out=val2[:], in_to_replace=m8[:, 0:8], in_values=val[:],
                          imm_value=-3e38)
                    kb.op("dve", "max", [val2], [m8], out=m8[:, 8:16], in_=val2[:])
                    kb.op("dve", "tensor_scalar", [val, m8], [negs], out=negs[:], in0=val[:], scalar1=m8[:, 15:16], scalar2=NEG,
                          op0=ALU.is_lt, op1=ALU.mult)
                    kb.op("pe", "transpose", [negs, ident_f], [PTr], PTr[:, 0:128], negs[:], ident_f[:])
                    kb.op("act", "copy", [], [negT, PTr], out=negT[:], in_=PTr[:, 0:128])
                    def finalize_branch(br, init):
                        kb.op("act", "copy", [], [Osb, PO3[br]], out=Osb.ap(br * 512, [[1, 512]], parts=65),
                              in_=PO3[br].ap(0, [[1, 512]], parts=65))
                        for r in range(4):
                            kb.op("pe", "transpose", [Osb, ident_f], [PTr], PTr[:, r * 65:(r + 1) * 65],
                                  Osb.ap(br * 512 + r * 128, [[1, 128]], parts=65), ident_f.ap(0, [[1, 65]], parts=65))
                        kb.op("dve", "tensor_scalar", [], [zt, PTr], out=zt[:], in0=PTr.ap(64, [[65, 4]]), scalar1=1e-30, scalar2=None,
                              op0=ALU.max)
                        kb.op("dve", "reciprocal", [zt], [zt], out=zt[:], in_=zt[:])
                        kb.op("dve", "tensor_tensor", [zt, Gt], [coef], out=coef[:], in0=zt[:],
                              in1=Gt[:, qt * 24 + br * 8 + g * 4:qt * 24 + br * 8 + g * 4 + 4], op=ALU.mult)
                        for r in range(4):
                            h = g * 4 + r
                            if init:
                                kb.op("dve", "tensor_scalar", [coef], [onsa, PTr], out=onsa[:, h * 64:(h + 1) * 64],
                                      in0=PTr[:, r * 65:r * 65 + 64], scalar1=coef[:, r:r + 1], scalar2=None, op0=ALU.mult)
                            else:
                                kb.op("dve", "scalar_tensor_tensor", [coef, onsa], [onsa, PTr], out=onsa[:, h * 64:(h + 1) * 64],
                                      in0=PTr[:, r * 65:r * 65 + 64], scalar=coef[:, r:r + 1], in1=onsa[:, h * 64:(h + 1) * 64],
                                      op0=ALU.mult, op1=ALU.add)

                    finalize_branch(0, True)
                    k0 = max(0, qt - 4)
                    for kt in range(k0, qt + 1):
                        biases = []
                        if kt == qt:
                            biases.append((ident_b, ident_b[:], CAUS, CAUS.ap(0, [[0, 4], [1, 128]])))
                        elif kt == qt - 4:
                            biases.append((ident_b, ident_b[:], CAUS, CAUS.ap(128, [[0, 4], [1, 128]])))
                        attn_tile(PO3[2], kt == k0, kt == qt, KwT, kt * 128, VW, (kt * 2 + g) * 65, g, biases)
                    flush()
                    finalize_branch(2, False)
                    for kt in range(qt + 1):
                        biases = [(E_b, E_b[:, kt * 128:(kt + 1) * 128], negT, negT.ap(0, [[0, 4], [1, 128]]))]
                        if kt == qt:
                            biases.append((ident_b, ident_b[:], CAUS, CAUS.ap(0, [[0, 4], [1, 128]])))
                        attn_tile(PO3[1], kt == 0, kt == qt, KsT, kt * 128, VS, (kt * 2 + g) * 65, g, biases)
                    flush()
                    finalize_branch(1, False)
                for kc in range(4):
                    kb.op("pe", "transpose", [onsa, ident_f], [PTr], PTr[:, kc * 128:(kc + 1) * 128], onsa[:, kc * 128:(kc + 1) * 128],
                          ident_f[:])
                kb.op("act", "copy", [], [onT, PTr], out=onT[:], in_=PTr[:, 0:512])
                kb.dma("sp", ONd.ap(qt * 128, [[4 * S, 128], [S, 4], [1, 128]]), onT.ap(0, [[128, 4], [1, 128]]), [onT], [ONd])
            kb.barrier()
        es1.close()

    kb.mark("W")
    if with_peer:
        with ExitStack() as es:
            cb = [kb.sb("cb%d" % i, [128, 2048], BF16, es) for i in range(4)]
            n = 0
            for src, dst in ((UT, UTb), (VJ, VJb)):
                for j2 in range(64):
                    b = cb[n % 4]
                    n += 1
                    kb.dma("pool", b.ap(0, [[1024, 2], [1, 1024]]), src.ap(j2 * 2 * 131072, [[1024, 128], [131072, 2], [1, 1024]]),
                           [src], [b])
                    kb.dma("sp", dst.ap(j2 * 2 * 131072, [[1024, 128], [131072, 2], [1, 1024]]), b.ap(0, [[1024, 2], [1, 1024]]),
                           [b], [dst])
            kb.barrier()

    kb.mark("C2")
    with ExitStack() as es:
        Wz_b = kb.sb("Wz_b", [128, 8, 1024], BF16, es)
        Wm_b = kb.sb("Wm_b", [128, 8, 2048], BF16, es)
        Wb1_b = kb.sb("Wb1_b", [128, 4, 1024], BF16, es)
        Wo_b = kb.sb("Wo_b", [128, 8, 1024], BF16, es)
        WsT_f = kb.sb("WsT_f", [128, 1024], F32, es)
        WsT_b = kb.sb("WsT_b", [128, 8, 128], BF16, es)
        tril_sb = kb.sb("tril_sb", [128, 128], F32, es)
        sgub = kb.sb("sgub", [128, 8], F32, es)
        bmcol = kb.sb("bmcol", [128, 16], F32, es)
        xs = [kb.sb("xs%d" % i, [128, 1024], F32, es) for i in range(4)]
        xn_b = kb.sb("xn_b", [128, 1024], BF16, es)
        hT = kb.sb("hT", [128, 8, 512], BF16, es)
        gab = kb.sb("gab", [128, 2, 512], BF16, es)
        u_b = kb.sb("u_b", [128, 512], BF16, es)
        v_f = kb.sb("v_f", [128, 512], F32, es)
        vb = kb.sb("vb", [128, 512], BF16, es)
        tmp = kb.sb("tmp", [128, 512], F32, es)
        osgu = kb.sb("osgu", [128, 512], BF16, es)
        osT = kb.sb("osT", [128, 4, 512], BF16, es)
        mT = kb.sb("mT", [128, 8, 512], BF16, es)
        ybuf = kb.sb("ybuf", [128, 1024], F32, es)
        x1 = kb.sb("x1", [128, 1024], F32, es)
        st = kb.sb("st", [128, 12], F32, es)
        mv = kb.sb("mv", [128, 2], F32, es)
        sd = kb.sb("sd", [128, 1], F32, es)
        rstd = kb.sb("rstd", [128, 1], F32, es)
        PT = kb.ps("PT", [128, 1024], BF16, es)
        PGA = kb.ps("PGA", [128, 512], F32, es)
        PGB = kb.ps("PGB", [128, 512], F32, es)
        PB_ = kb.ps("PB_", [128, 512], F32, es)
        if with_nsa:
            PA_ = kb.ps("PA_", [128, 512], F32, es)
            Wb0_b = kb.sb("Wb0_b", [128, 4, 1024], BF16, es)
            onsaT = kb.sb("onsaT", [128, 4, 512], BF16, es)
            tmpa = kb.sb("tmpa", [128, 512], F32, es)
            kb.dma("pool", Wb0_b[:], wslab(w_b0, 4, 1024, 0, 1024), [w_b0], [Wb0_b])
        PZ0 = kb.ps("PZ0", [128, 512], F32, es)
        PZ1 = kb.ps("PZ1", [128, 512], F32, es)
        PM = kb.ps("PM", [128, 512], F32, es)

        kb.dma("pool", Wz_b[:], wslab(wz, 8, 1024, 0, 1024), [wz], [Wz_b])
        kb.dma("pool", Wm_b[:], wslab(w_merge, 8, 2048, 0, 2048), [w_merge], [Wm_b])
        kb.dma("pool", Wb1_b[:], wslab(w_b1, 4, 1024, 0, 1024), [w_b1], [Wb1_b])
        kb.dma("pool", Wo_b[:], wslab(w_out, 8, 1024, 0, 1024), [w_out], [Wo_b])
        kb.dma("sp", WsT_f[:], wsT[:], [wsT], [WsT_f])
        kb.dma("sp", tril_sb[:], trilm[:], [trilm], [tril_sb])
        kb.dma("sp", sgub[:], sgub_col[:], [sgub_col], [sgub])
        kb.dma("sp", bmcol[:], b_merge_col[:], [b_merge_col], [bmcol])
        kb.op("dve", "tensor_tensor", [WsT_f, tril_sb], [WsT_b], out=WsT_b.ap(0, [[128, 8], [1, 128]]),
              in0=WsT_f.ap(0, [[128, 8], [1, 128]]), in1=tril_sb.ap(0, [[0, 8], [1, 128]]), op=ALU.mult)

        for tg in range(S // 512):
            t0 = tg * 512
            for s in range(4):
                kb.dma("sp", xs[s][:], x.ap((t0 + s * 128) * D, [[D, 128], [1, D]]), [x], [xs[s]])
            for s in range(4):
                ln_transpose(xs[s], xn_b, PT, hT, s * 128, 128, 8, 0, st, mv, sd, rstd)
            if with_nsa:
                kb.dma("sp", onsaT.ap(0, [[512, 4], [1, 512]]), ONd.ap(t0, [[4 * S, 128], [S, 4], [1, 512]]), [ONd], [onsaT])
            for s in range(4):
                for half, Pz in enumerate([PZ0, PZ1]):
                    for k in range(8):
                        kb.op("pe", "matmul", [hT, Wz_b], [Pz], Pz[:, 0:512], lhsT=hT[:, k, s * 128:(s + 1) * 128],
                              rhs=Wz_b[:, k, half * 512:(half + 1) * 512], start=(k == 0), stop=(k == 7))
                kb.op("act", "activation", [], [u_b, PZ0], out=u_b[:], in_=PZ0[:, 0:512], func=AF.Gelu)
                kb.op("act", "activation", [], [v_f, PZ1], out=v_f[:], in_=PZ1[:, 0:512], func=AF.Gelu)
                kb.op("dve", "bn_stats", [v_f], [st], out=st[:, 0:6], in_=v_f[:])
                kb.op("dve", "bn_aggr", [st], [mv], out=mv[:, 0:2], in_=st[:, 0:6])
                kb.op("act", "activation", [mv, eps_col], [sd], out=sd[:, 0:1], in_=mv[:, 1:2], func=AF.Sqrt,
                      bias=eps_col[:, 0:1], scale=1.0)
                kb.op("dve", "reciprocal", [sd], [rstd], out=rstd[:, 0:1], in_=sd[:, 0:1])
                kb.op("dve", "scalar_tensor_tensor", [v_f, mv, ln_bc], [tmp], out=tmp[:], in0=v_f[:], scalar=mv[:, 0:1],
                      in1=ln_bc[:, 4096:4608], op0=ALU.subtract, op1=ALU.mult)
                kb.op("dve", "scalar_tensor_tensor", [tmp, rstd, ln_bc], [vb], out=vb[:], in0=tmp[:], scalar=rstd[:, 0:1],
                      in1=ln_bc[:, 4608:5120], op0=ALU.mult, op1=ALU.add)
                for g in range(8):
                    kb.op("pe", "matmul", [WsT_b, vb], [PM], PM[:, g * 64:(g + 1) * 64], lhsT=WsT_b[:, g, :],
                          rhs=vb[:, g * 64:(g + 1) * 64], start=True, stop=True)
                kb.op("dve", "tensor_tensor", [sgub], [tmp, PM], out=tmp.ap(0, [[64, 8], [1, 64]]),
                      in0=PM.ap(0, [[64, 8], [1, 64]]), in1=sgub.ap(0, [[1, 8], [0, 64]]), op=ALU.add)
                kb.op("dve", "tensor_tensor", [tmp, u_b], [osgu], out=osgu[:], in0=tmp[:], in1=u_b[:], op=ALU.mult)
                for kc in range(4):
                    kb.op("pe", "transpose", [osgu, ident_b], [PT], PT[:, kc * 128:(kc + 1) * 128],
                          osgu[:, kc * 128:(kc + 1) * 128], ident_b[:])
                kb.op("act", "copy", [], [osT, PT], out=osT.ap(s * 128, [[512, 4], [1, 128]]),
                      in_=PT.ap(0, [[128, 4], [1, 128]]))
            for cch in range(8):
                for gi, (Pg, col) in enumerate([(PGA, cch), (PGB, 8 + cch)]):
                    if gi == 0 and not with_nsa:
                        continue
                    for k in range(8):
                        kb.op("pe", "matmul", [hT, Wm_b], [Pg], Pg[:, 0:512], lhsT=Wm_b[:, k, col * 128:(col + 1) * 128],
                              rhs=hT[:, k, :], start=(k == 0), stop=(k == 7))
                for kc in range(4):
                    kb.op("pe", "matmul", [osT, Wb1_b], [PB_], PB_[:, 0:512], lhsT=Wb1_b[:, kc, cch * 128:(cch + 1) * 128],
                          rhs=osT[:, kc, :], start=(kc == 0), stop=(kc == 3))
                kb.op("act", "activation", [bmcol], [gab, PGB], out=gab[:, 1, :], in_=PGB[:, 0:512], func=AF.Sigmoid,
                      bias=bmcol[:, 8 + cch:9 + cch], scale=1.0)
                if with_nsa:
                    for kc in range(4):
                        kb.op("pe", "matmul", [onsaT, Wb0_b], [PA_], PA_[:, 0:512], lhsT=Wb0_b[:, kc, cch * 128:(cch + 1) * 128],
                              rhs=onsaT[:, kc, :], start=(kc == 0), stop=(kc == 3))
                    kb.op("act", "activation", [bmcol], [gab, PGA], out=gab[:, 0, :], in_=PGA[:, 0:512], func=AF.Sigmoid,
                          bias=bmcol[:, cch:cch + 1], scale=1.0)
                    kb.op("dve", "tensor_tensor", [gab], [tmpa, PA_], out=tmpa[:], in0=PA_[:, 0:512], in1=gab[:, 0, :], op=ALU.mult)
                    kb.op("dve", "tensor_tensor", [gab], [tmp, PB_], out=tmp[:], in0=PB_[:, 0:512], in1=gab[:, 1, :], op=ALU.mult)
                    kb.op("dve", "tensor_tensor", [tmp, tmpa], [mT], out=mT[:, cch, :], in0=tmp[:], in1=tmpa[:], op=ALU.add)
                else:
                    kb.op("dve", "tensor_tensor", [gab], [mT, PB_], out=mT[:, cch, :], in0=PB_[:, 0:512], in1=gab[:, 1, :],
                          op=ALU.mult)
            for s in range(4):
                for half, Pz in enumerate([PZ0, PZ1]):
                    for k in range(8):
                        kb.op("pe", "matmul", [mT, Wo_b], [Pz], Pz[:, 0:512], lhsT=mT[:, k, s * 128:(s + 1) * 128],
                              rhs=Wo_b[:, k, half * 512:(half + 1) * 512], start=(k == 0), stop=(k == 7))
                resid_ln(xs[s], PZ0, PZ1, 0, 0, 1024, ybuf, x1, st, mv, sd, rstd)
                dst = x1s if with_peer else y
                kb.dma("pool", dst.ap((t0 + s * 128) * D, [[D, 128], [1, D]]), x1[:], [x1], [dst])
        kb.barrier()

    if not with_peer:
        kb.wait_all("sp")
        return nc, kb

    kb.mark("D")
    NG = S // 256
    GTd2 = [kb.dram("GTd%d" % i, [128, 128 * 256], BF16) for i in range(2)]
    with ExitStack() as es:
        Wq_b = kb.sb("Wq_b", [128, 8, 2048], BF16, es)
        keys_b = kb.sb("keys_b", [128, 16, 128], BF16, es)
        GT = kb.sb("GT", [128, 128 * 128], BF16, es)
        NBUF = 3
        Ubuf = [kb.sb("Ubuf%d" % i, [128, 2, 1024], BF16, es) for i in range(NBUF)]
        Vbuf = [kb.sb("Vbuf%d" % i, [128, 2, 1024], BF16, es) for i in range(NBUF)]
        Gs = [kb.sb("Gs%d" % i, [128, 2, 256], BF16, es) for i in range(NBUF)]
        x1t = [kb.sb("x1t%d" % i, [128, 1024], F32, es) for i in range(2)]
        xr = kb.sb("xr", [128, 1024], F32, es)
        h2T2 = [kb.sb("h2T%d" % i, [128, 8, 256], BF16, es) for i in range(2)]
        qT = kb.sb("qT", [128, 16, 256], BF16, es)
        sc = kb.sb("sc", [128, 4, 128], F32, es)
        scrA = kb.sb("scrA", [128, 2048], F32, es)
        scrB = kb.sb("scrB", [128, 2048], F32, es)
        t16 = kb.sb("t16", [128, 256], F32, es)
        i16 = kb.sb("i16", [128, 256], U32, es)
        i16f = kb.sb("i16f", [128, 256], F32, es)
        tv = kb.sb("tv", [128, 128], F32, es)
        pv = kb.sb("pv", [128, 128], U32, es)
        pvf = kb.sb("pvf", [128, 128], F32, es)
        ee = kb.sb("ee", [128, 128], F32, es)
        zz = kb.sb("zz", [128, 8], F32, es)
        rz = kb.sb("rz", [128, 8], F32, es)
        ak = kb.sb("ak", [128, 128], F32, es)
        bk = kb.sb("bk", [128, 128], F32, es)
        III = kb.sb("III", [128, 384], F32, es)
        ITs = kb.sb("ITs", [128, 384], F32, es)
        iota16 = kb.sb("iota16", [128, 16], F32, es)
        CH = 16
        Lb = kb.sb("Lb", [128, CH * 128], BF16, es)
        Rb = kb.sb("Rb", [128, CH * 128], BF16, es)
        actg = [kb.sb("actg%d" % i, [128, 512], BF16, es) for i in range(2)]
        wd = [kb.sb("wd%d" % i, [128, 512], BF16, es) for i in range(2)]
        ybuf = kb.sb("ybuf2", [128, 1024], F32, es)
        st = kb.sb("st2", [128, 12], F32, es)
        mv = kb.sb("mv2", [128, 2], F32, es)
        sd = kb.sb("sd2", [128, 1], F32, es)
        rstd = kb.sb("rstd2", [128, 1], F32, es)
        stf = kb.sb("st3", [128, 12], F32, es)
        mvf = kb.sb("mv3", [128, 2], F32, es)
        sdf = kb.sb("sd3", [128, 1], F32, es)
        rstdf = kb.sb("rstd3", [128, 1], F32, es)
        PO = [kb.ps("PO%d" % i, [128, 512], F32, es) for i in range(4)]
        PA = [kb.ps("PA%d" % i, [128, 512], F32, es) for i in range(2)]
        PG = [kb.ps("PG%d" % i, [128, 512], F32, es) for i in range(2)]

        kb.dma("pool", Wq_b[:], wslab(peer_wq, 8, 2048, 0, 2048), [peer_wq], [Wq_b])
        kb.dma("pool", keys_b.ap(0, [[1, 2048]]), keysT[:], [keysT], [keys_b])
        kb.op("pool", "iota", [], [iota16], iota16[:], [[1, 16]], base=0, channel_multiplier=0,
              allow_small_or_imprecise_dtypes=True)

        def ln_transpose_f(src, dstT, col0, sc_off, sh_off):
            layernorm_stats(src, st, mv, sd, rstd)
            kb.op("dve", "tensor_scalar", [src, mv, rstd], [scrA], out=scrA[:, 0:1024], in0=src[:], scalar1=mv[:, 0:1],
                  scalar2=rstd[:, 0:1], op0=ALU.subtract, op1=ALU.mult)
            for k in range(8):
                Pp = PG[k // 4]
                kb.op("pe", "transpose", [scrA, ident_f], [Pp], Pp[:, (k % 4) * 128:(k % 4 + 1) * 128],
                      scrA[:, k * 128:(k + 1) * 128], ident_f[:])
            for k in range(8):
                Pp = PG[k // 4]
                kb.op("act", "activation", [modcol], [dstT, Pp], out=dstT[:, k, col0:col0 + 128],
                      in_=Pp[:, (k % 4) * 128:(k % 4 + 1) * 128], func=AF.Identity,
                      bias=modcol[:, sh_off + k:sh_off + k + 1], scale=modcol[:, sc_off + k:sc_off + k + 1])

        def prep(tg):
            t0 = tg * 256
            h2T = h2T2[tg % 2]
            GTd = GTd2[tg % 2]
            for s in range(2):
                kb.dma("sp", x1t[s][:], x1s.ap((t0 + s * 128) * D, [[D, 128], [1, D]]), [x1s], [x1t[s]])
                ln_transpose_f(x1t[s], h2T, s * 128, 24, 16)
                yield
            for cch in range(16):
                Pq = PG[cch % 2]
                for k in range(8):
                    kb.op("pe", "matmul", [h2T, Wq_b], [Pq], Pq[:, 0:256], lhsT=Wq_b[:, k, cch * 128:(cch + 1) * 128],
                          rhs=h2T[:, k, :], start=(k == 0), stop=(k == 7))
                kb.op("act", "copy", [], [qT, Pq], out=qT[:, cch, :], in_=Pq[:, 0:256])
                yield
            for s in range(2):
                ts = slice(s * 128, (s + 1) * 128)
                for r4 in range(4):
                    Ps = PG[r4 % 2]
                    for rr in range(4):
                        r = r4 * 4 + rr
                        kb.op("pe", "matmul", [qT, keys_b], [Ps], Ps[:, rr * 128:(rr + 1) * 128], lhsT=qT[:, r, ts],
                              rhs=keys_b[:, r, :], start=True, stop=True)
                    kb.op("act", "copy", [], [sc, Ps], out=sc.ap(0, [[1, 512]]), in_=Ps[:, 0:512])
                    yield
                    for rr in range(4):
                        r = r4 * 4 + rr
                        kb.op("dve", "max", [sc], [t16], out=t16[:, r * 16:r * 16 + 8], in_=sc[:, rr, :])
                        kb.op("dve", "max_index", [t16, sc], [i16], out=i16[:, r * 16:r * 16 + 8],
                              in_max=t16[:, r * 16:r * 16 + 8], in_values=sc[:, rr, :])
                        kb.op("dve", "match_replace", [t16, sc], [scrA], out=scrA[:, rr * 128:(rr + 1) * 128],
                              in_to_replace=t16[:, r * 16:r * 16 + 8], in_values=sc[:, rr, :], imm_value=-1e30)
                        kb.op("dve", "max", [scrA], [t16], out=t16[:, r * 16 + 8:r * 16 + 16],
                              in_=scrA[:, rr * 128:(rr + 1) * 128])
                        kb.op("dve", "max_index", [t16, scrA], [i16], out=i16[:, r * 16 + 8:r * 16 + 16],
                              in_max=t16[:, r * 16 + 8:r * 16 + 16], in_values=scrA[:, rr * 128:(rr + 1) * 128])
                        yield
                kb.op("dve", "tensor_copy", [i16], [i16f], out=i16f[:], in_=i16[:])
                kb.op("dve", "tensor_tensor", [t16], [scrB], out=scrB.ap(0, [[256, 8], [16, 16], [1, 16]]),
                      in0=t16.ap(0, [[32, 8], [1, 16], [0, 16]]), in1=t16.ap(16, [[32, 8], [0, 16], [1, 16]]), op=ALU.add)
                yield
                for h in range(8):
                    cs_ = slice(h * 256, (h + 1) * 256)
                    kb.op("dve", "max", [scrB], [tv], out=tv[:, h * 16:h * 16 + 8], in_=scrB[:, cs_])
                    kb.op("dve", "max_index", [tv, scrB], [pv], out=pv[:, h * 16:h * 16 + 8], in_max=tv[:, h * 16:h * 16 + 8],
                          in_values=scrB[:, cs_])
                    kb.op("dve", "match_replace", [tv, scrB], [scrA], out=scrA[:, cs_], in_to_replace=tv[:, h * 16:h * 16 + 8],
                          in_values=scrB[:, cs_], imm_value=-1e30)
                    kb.op("dve", "max", [scrA], [tv], out=tv[:, h * 16 + 8:h * 16 + 16], in_=scrA[:, cs_])
                    kb.op("dve", "max_index", [tv, scrA], [pv], out=pv[:, h * 16 + 8:h * 16 + 16],
                          in_max=tv[:, h * 16 + 8:h * 16 + 16], in_values=scrA[:, cs_])
                    yield
                kb.op("dve", "tensor_tensor", [tv], [ee], out=ee.ap(0, [[16, 8], [1, 16]]), in0=tv.ap(0, [[16, 8], [1, 16]]),
                      in1=tv.ap(0, [[16, 8], [0, 16]]), op=ALU.subtract)
                kb.op("act", "activation", [ee], [ee], out=ee[:], in_=ee[:], func=AF.Exp)
                kb.op("dve", "tensor_reduce", [ee], [zz], out=zz[:], in_=ee.ap(0, [[16, 8], [1, 16]]), axis=AX.X, op=ALU.add)
                kb.op("dve", "reciprocal", [zz], [rz], out=rz[:], in_=zz[:])
                kb.op("dve", "tensor_tensor", [ee, rz], [III], out=III.ap(256, [[16, 8], [1, 16]]),
                      in0=ee.ap(0, [[16, 8], [1, 16]]), in1=rz.ap(0, [[1, 8], [0, 16]]), op=ALU.mult)
                yield
                kb.op("dve", "tensor_copy", [pv], [pvf], out=pvf[:], in_=pv[:])
                kb.op("dve", "tensor_tensor", [pvf, thr15], [scrA], out=scrA.ap(0, [[15, 128], [1, 15]]),
                      in0=pvf.ap(0, [[1, 128], [0, 15]]), in1=thr15.ap(0, [[0, 128], [1, 15]]), op=ALU.is_ge)
                kb.op("dve", "tensor_reduce", [scrA], [ak], out=ak[:], in_=scrA.ap(0, [[15, 128], [1, 15]]), axis=AX.X, op=ALU.add)
                kb.op("dve", "scalar_tensor_tensor", [ak, pvf], [bk], out=bk[:], in0=ak[:], scalar=-16.0, in1=pvf[:],
                      op0=ALU.mult, op1=ALU.add)
                yield
                for which, (sel, off) in enumerate([(ak, 0), (bk, 16)]):
                    kb.op("dve", "tensor_tensor", [iota16, sel], [scrA], out=scrA.ap(0, [[256, 8], [16, 16], [1, 16]]),
                          in0=iota16.ap(0, [[0, 8], [0, 16], [1, 16]]), in1=sel.ap(0, [[16, 8], [1, 16], [0, 16]]),
                          op=ALU.is_equal)
                    kb.op("dve", "tensor_tensor", [scrA, i16f], [scrB], out=scrB.ap(0, [[256, 8], [16, 16], [1, 16]]),
                          in0=scrA.ap(0, [[256, 8], [16, 16], [1, 16]]), in1=i16f.ap(off, [[32, 8], [0, 16], [1, 16]]),
                          op=ALU.mult)
                    kb.op("dve", "tensor_reduce", [scrB], [III], out=III.ap(which * 128, [[16, 8], [1, 16]]),
                          in_=scrB.ap(0, [[256, 8], [16, 16], [1, 16]]), axis=AX.X, op=ALU.add)
                    yield
                for i3 in range(3):
                    kb.op("pe", "transpose", [III, ident_f], [PG[0]], PG[0][:, i3 * 128:(i3 + 1) * 128],
                          III[:, i3 * 128:(i3 + 1) * 128], ident_f[:])
                kb.op("act", "copy", [], [ITs, PG[0]], out=ITs[:], in_=PG[0][:, 0:384])
                yield
                for ch in range(128 // CH):
                    kb.op("dve", "tensor_tensor", [iota128, ITs], [Lb], out=Lb.ap(0, [[128, CH], [1, 128]]),
                          in0=iota128.ap(0, [[0, CH], [1, 128]]), in1=ITs.ap(ch * CH, [[1, CH], [0, 128]]), op=ALU.is_equal)
                    kb.op("dve", "tensor_tensor", [iota128, ITs], [Rb], out=Rb.ap(0, [[128, CH], [1, 128]]),
                          in0=iota128.ap(0, [[0, CH], [1, 128]]), in1=ITs.ap(128 + ch * CH, [[1, CH], [0, 128]]), op=ALU.is_equal)
                    kb.op("dve", "tensor_tensor", [Rb, ITs], [Rb], out=Rb.ap(0, [[128, CH], [1, 128]]),
                          in0=Rb.ap(0, [[128, CH], [1, 128]]), in1=ITs.ap(256 + ch * CH, [[1, CH], [0, 128]]), op=ALU.mult)
                    yield
                    for t4 in range(CH // 4):
                        Pg = PG[t4 % 2]
                        for tt in range(4):
                            tl = t4 * 4 + tt
                            kb.op("pe", "matmul", [Lb, Rb], [Pg], Pg[:, tt * 128:(tt + 1) * 128], lhsT=Lb[:, tl * 128:(tl + 1) * 128],
                                  rhs=Rb[:, tl * 128:(tl + 1) * 128], start=True, stop=True)
                        tokb = ch * CH + t4 * 4
                        kb.op("act", "copy", [], [GT, Pg], out=GT.ap(tokb, [[1, 4], [128, 128]]), in_=Pg.ap(0, [[128, 4], [1, 128]]))
                        yield
                for jb in range(8):
                    kb.dma("pool", GTd.ap(jb * 16 * 256 + s * 128, [[128 * 256, 128], [256, 16], [1, 128]]),
                           GT.ap(jb * 16 * 128, [[128, 16], [1, 128]]), [GT], [GTd])
                yield

        def run_steps(gen, n):
            if gen is None:
                return None
            for _ in range(n):
                try:
                    next(gen)
                except StopIteration:
                    return None
            return gen

        gen = prep(0)
        while gen is not None:
            gen = run_steps(gen, 1000)
        for tg in range(NG):
            kb.mark("Dg%d" % tg)
            t0 = tg * 256
            h2T = h2T2[tg % 2]
            GTd = GTd2[tg % 2]
            gen = prep(tg + 1) if tg + 1 < NG else None
            for jp in range(64):
                bi = jp % NBUF
                j0 = jp * 2
                kb.dma("sp", Ubuf[bi].ap(0, [[1024, 2], [1, 1024]]), UTb.ap(j0 * 131072, [[1024, 128], [131072, 2], [1, 1024]]),
                       [UTb], [Ubuf[bi]])
                kb.dma("act", Vbuf[bi].ap(0, [[1024, 2], [1, 1024]]), VJb.ap(j0 * 131072, [[1024, 128], [131072, 2], [1, 1024]]),
                       [VJb], [Vbuf[bi]])
                kb.dma("sp", Gs[bi].ap(0, [[256, 2], [1, 256]]), GTd.ap(j0 * 256, [[128 * 256, 128], [256, 2], [1, 256]]),
                       [GTd], [Gs[bi]])
                Pa = PA[jp % 2]
                for jj in range(2):
                    for k in range(8):
                        kb.op("pe", "matmul", [h2T, Ubuf[bi]], [Pa], Pa[:, jj * 256:(jj + 1) * 256],
                              lhsT=Ubuf[bi][:, jj, k * 128:(k + 1) * 128], rhs=h2T[:, k, :], start=(k == 0), stop=(k == 7))
                ag = actg[jp % 2]
                wdd = wd[jp % 2]
                kb.op("act", "activation", [], [ag, Pa], out=ag[:], in_=Pa[:, 0:512], func=AF.Gelu)
                kb.op("dve", "tensor_tensor", [ag, Gs[bi]], [wdd], out=wdd[:], in0=ag[:], in1=Gs[bi].ap(0, [[1, 512]]), op=ALU.mult)
                for jj in range(2):
                    j = j0 + jj
                    for s in range(2):
                        for half in range(2):
                            Pp = PO[s * 2 + half]
                            kb.op("pe", "matmul", [wdd, Vbuf[bi]], [Pp], Pp[:, 0:512],
                                  lhsT=wdd[:, jj * 256 + s * 128:jj * 256 + (s + 1) * 128],
                                  rhs=Vbuf[bi][:, jj, half * 512:(half + 1) * 512], start=(j == 0), stop=(j == 127))
                gen = run_steps(gen, PREP_STEPS)
            for s in range(2):
                kb.dma("sp", xr[:], x1s.ap((t0 + s * 128) * D, [[D, 128], [1, D]]), [x1s], [xr])
                resid_ln(xr, PO[s * 2], PO[s * 2 + 1], 1024, 2048, 3072, ybuf, ybuf, stf, mvf, sdf, rstdf)
                kb.dma("pool", y.ap((t0 + s * 128) * D, [[D, 128], [1, D]]), ybuf[:], [ybuf], [y])
            while gen is not None:
                gen = run_steps(gen, 1000)
        kb.barrier()
    kb.mark("end")
    kb.wait_all("sp")
    kb.wait_all("pool")
    return nc, kb


def prep_shared(inp):
    f = lambda a: np.ascontiguousarray(a, dtype=np.float32)
    sh = {}
    sh["w_ada"] = f(inp["w_ada"][0])
    sh["b_ada"] = f(inp["b_ada"][0][None, :])
    sh["rows"] = f(np.concatenate([inp["ln1_g"][0], inp["ln1_b"][0], inp["ln2_g"][0], inp["ln2_b"][0],
                                   inp["sgu_ln_g"][0], inp["sgu_ln_b"][0]])[None, :])
    w_in = inp["w_in"][0]
    sh["wz"] = f(w_in[:, 1304:2328])
    sh["w_merge"] = f(inp["w_merge"][0])
    sh["b_merge_col"] = f(inp["b_merge"][0].reshape(16, 128).T)
    sh["w_b1"] = f(inp["w_branch"][0, 1])
    sh["w_out"] = f(inp["w_out"][0])
    sh["wsT"] = f(inp["sgu_w"][0].transpose(2, 0, 1).reshape(128, 1024))
    sh["sgub_col"] = f(inp["sgu_b"][0].T)
    jj, ii = np.meshgrid(np.arange(128), np.arange(128), indexing="ij")
    sh["trilm"] = f((jj <= ii).astype(np.float32))
    sh["peer_wq"] = f(inp["peer_wq"][0])
    sh["keysT"] = f(inp["peer_keys"][0].reshape(16, 128, 128).transpose(2, 0, 1).reshape(128, 2048))
    pu = inp["peer_u"][0].reshape(128, 128, 8, 128)
    sh["UT"] = f(pu.transpose(1, 3, 2, 0).reshape(128, 128, 1024))
    pv = inp["peer_v"][0].reshape(128, 128, 1024)
    sh["VJ"] = f(pv.transpose(1, 0, 2))
    def swp(c):
        blocks = [np.concatenate([c[:, i * 64 + 32:(i + 1) * 64], c[:, i * 64:i * 64 + 32]], axis=1) for i in range(c.shape[1] // 64)]
        return np.concatenate(blocks, axis=1)
    qch = [np.concatenate([w_in[:, r * 64:(r + 1) * 64], w_in[:, (4 + r) * 64:(5 + r) * 64]], axis=1) for r in range(4)]
    kcs = [w_in[:, 512:640], w_in[:, 768:896], w_in[:, 1024:1152]]
    chunks = qch + [swp(c) for c in qch] + kcs + [swp(c) for c in kcs] + [w_in[:, 640:768]]
    sh["WAf"] = f(np.concatenate(chunks, axis=1))
    sh["WAt"] = f(np.concatenate([w_in[:, 896:1024], w_in[:, 1152:1280], w_in[:, 1280:1304]], axis=1))
    dup = lambda a: np.concatenate([a, a], axis=0)
    w1 = inp["cmp_w1"][0]
    sh["w1d"] = f(np.stack([dup(w1[kv].reshape(32, 64, 128).transpose(1, 0, 2).reshape(64, 4096)) for kv in range(2)]))
    pos = inp["cmp_pos"][0]
    sh["posTd"] = f(dup(np.concatenate([pos[0].T, pos[1].T], axis=1)))
    sh["b1col"] = f(inp["cmp_b1"][0].T)
    w2 = inp["cmp_w2"][0]
    sh["w2kd"] = f(np.concatenate([w2[0], w2[0]], axis=1))
    sh["w2v"] = f(w2[1])
    sh["b2kcol"] = f(dup(inp["cmp_b2"][0][0][:, None]))
    sh["b2vrow"] = f(inp["cmp_b2"][0][1][None, :])
    sh["w_b0"] = f(inp["w_branch"][0, 0])
    return sh


def prep_consts(S):
    f = lambda a: np.ascontiguousarray(a, dtype=np.float32)
    NEG = -30000.0
    cst = {}
    p = np.arange(128)
    d = p % 64
    inv_freq = (np.float32(10000.0) ** (-(np.arange(32, dtype=np.float32)) / np.float32(32))).astype(np.float32)
    ang = (np.arange(S, dtype=np.float32)[None, :] * inv_freq[d % 32][:, None]).astype(np.float32)
    cst["cosT"] = f(np.cos(ang))
    sgn = np.where(d < 32, -1.0, 1.0).astype(np.float32)[:, None]
    cst["sinS"] = f(np.sin(ang) * sgn)
    ncmp = (S - 32) // 16 + 1
    nsel = S // 64
    c0 = np.arange(ncmp)[:, None] * 16
    s0 = np.arange(nsel)[None, :] * 64
    ov = np.clip(np.minimum(c0 + 32, s0 + 64) - np.maximum(c0, s0), 0, None) / 32.0
    agg = np.zeros((512, 128), np.float32)
    agg[:ncmp, :nsel] = ov
    cst["aggd"] = f(agg.reshape(4, 128, 128).transpose(1, 0, 2).reshape(128, 512))
    nl = np.arange(128)[:, None, None]
    m = np.arange(16)[None, :, None]
    ql = np.arange(128)[None, None, :]
    cst["cmpb"] = f(np.where(16 * nl + 31 - ql <= 128 * m, 0.0, NEG).reshape(128, 2048))
    kk = np.arange(128)[:, None]
    qq = np.arange(128)[None, :]
    cst["caus"] = f(np.concatenate([np.where(kk <= qq, 0.0, NEG), np.where(kk > qq, 0.0, NEG)], axis=1))
    q = np.arange(128)[:, None]
    c = np.arange(254)[None, :]
    dd = c - 126 - (q >= 64)
    base = np.where(dd > 0, -1e30, np.where(dd == 0, 2e9, np.where(dd == -1, 1e9, 0.0)))
    j0 = np.zeros((128, 128))
    j0[:, 0] = 3e9
    cst["based"] = f(np.concatenate([base, j0], axis=1))
    key = np.arange(S)[None, :]
    cst["ebig"] = f((key // 64 == np.arange(128)[:, None]).astype(np.float32))
    return cst


def kernel(**inputs):
    B, S = inputs["x"].shape[0], inputs["x"].shape[1]
    nc, kb = build(S)
    sh = prep_shared(inputs)
    sh.update(prep_consts(S))
    in_maps = []
    for b in range(B):
        m = dict(sh)
        m["x"] = np.ascontiguousarray(inputs["x"][b], dtype=np.float32)
        m["c_col"] = np.ascontiguousarray(inputs["c"][b].reshape(8, 128).T, dtype=np.float32)
        in_maps.append(m)
    res = run_bass_kernel_spmd(nc, in_maps, core_ids=list(range(B)))
    return np.stack([np.asarray(r["y"], dtype=np.float32) for r in res.results], axis=0)
```

```python
import numpy as np
import concourse.bass as bass
import concourse.mybir as mybir
from concourse.bass_utils import run_bass_kernel_spmd
from contextlib import ExitStack

F32 = mybir.dt.float32
BF16 = mybir.dt.bfloat16
U32 = mybir.dt.uint32
AF = mybir.ActivationFunctionType
ALU = mybir.AluOpType
AX = mybir.AxisListType

import os
PREP_STEPS = int(os.environ.get("PREP_STEPS", "4"))
D = 1024
ALPHA = 2.0 ** 0.25
EPS = 1e-5


class Sem:
    def __init__(self, h, name):
        self.h = h
        self.name = name
        self.count = 0


class T:
    def __init__(self, t, name, shape, dt, space):
        self.t = t
        self.name = name
        self.shape = list(shape)
        self.dt = dt
        self.space = space
        self.w = {}
        self.r = {}
        self.dsem = None
        self.fsize = int(np.prod(shape[1:]))

    def __getitem__(self, idx):
        return self.t[idx]

    def ap(self, off, dims, parts=None, pstart=0):
        if self.space == "dram":
            return bass.AP(self.t, off, [list(d) for d in dims])
        if parts is None:
            parts = self.shape[0]
        return bass.AP(self.t, pstart * self.fsize + off, [[self.fsize, parts]] + [list(d) for d in dims])


class KB:
    def __init__(self, nc):
        self.nc = nc
        self.es = ExitStack()
        self.engs = {"pe": nc.tensor, "act": nc.scalar, "dve": nc.vector, "pool": nc.gpsimd, "sp": nc.sync}
        self.esem = {}
        self.waited = {k: {} for k in self.engs}
        self.all_sems = []
        for k in self.engs:
            self.esem[k] = self.new_sem("e_" + k)
        self.n_instr = 0
        self.n_wait = 0
        self.marks = []
        self.pe_count = 0

    def new_sem(self, name):
        h = self.es.enter_context(self.nc.semaphore(name))
        s = Sem(h, name)
        self.all_sems.append(s)
        return s

    def sb(self, name, shape, dt, es=None):
        t = (es or self.es).enter_context(self.nc.sbuf_tensor(name, list(shape), dt))
        return T(t, name, shape, dt, "sb")

    def ps(self, name, shape, dt, es=None):
        t = (es or self.es).enter_context(self.nc.psum_tensor(name, list(shape), dt))
        return T(t, name, shape, dt, "ps")

    def dram(self, name, shape, dt, kind=None):
        if kind is None:
            t = self.nc.dram_tensor(name, list(shape), dt)
        else:
            t = self.nc.dram_tensor(name, list(shape), dt, kind=kind)
        return T(t, name, shape, dt, "dram")

    def _wait(self, e, sem, val):
        if val <= 0:
            return
        w = self.waited[e]
        if w.get(sem, 0) >= val:
            return
        self.engs[e].wait_ge(sem.h, val)
        w[sem] = val
        self.n_wait += 1

    def _deps(self, e, reads, writes):
        mysem = self.esem[e]
        for b in reads:
            for s, v in b.w.items():
                if s is mysem and e == "pe":
                    continue
                self._wait(e, s, v)
        for b in writes:
            for s, v in b.w.items():
                if s is mysem and e == "pe":
                    continue
                self._wait(e, s, v)
            for s, v in b.r.items():
                if s is mysem and e == "pe":
                    continue
                self._wait(e, s, v)

    def op(self, e, fn, reads, writes, *a, **kw):
        self._deps(e, reads, writes)
        ins = getattr(self.engs[e], fn)(*a, **kw)
        if e == "pe":
            self.pe_count += 1
        s = self.esem[e]
        s.count += 1
        ins.then_inc(s.h, 1)
        for b in reads:
            b.r[s] = s.count
        for b in writes:
            b.w[s] = s.count
        self.n_instr += 1
        return ins

    def dma(self, q, out_ap, in_ap, reads, writes, sem=None, **kw):
        if sem is None:
            tgt = writes[0]
            if tgt.dsem is None:
                tgt.dsem = self.new_sem("d_" + tgt.name)
            sem = tgt.dsem
        self._deps(q, reads, writes)
        ins = self.engs[q].dma_start(out=out_ap, in_=in_ap, **kw)
        sem.count += 16
        ins.then_inc(sem.h, 16)
        for b in reads:
            b.r[sem] = sem.count
        for b in writes:
            b.w[sem] = sem.count
        self.n_instr += 1
        return ins

    def mark(self, name):
        self.marks.append((name, self.n_instr, self.n_wait, self.pe_count))

    def barrier(self):
        for e in self.engs:
            for s in self.all_sems:
                self._wait(e, s, s.count)

    def wait_all(self, e):
        for s in self.all_sems:
            self._wait(e, s, s.count)


def build(S, with_peer=True, with_nsa=True, stop_after=None):
    nc = bass.Bass("TRN2", target_bir_lowering=False)
    kb = KB(nc)
    NS = S // 128

    def din(name, shape, dt=F32):
        return kb.dram(name, shape, dt, kind="ExternalInput")

    x = din("x", [S, D])
    c_col = din("c_col", [128, 8])
    w_ada = din("w_ada", [1024, 6144])
    b_ada = din("b_ada", [1, 6144])
    rows = din("rows", [1, 5120])
    wz = din("wz", [1024, 1024])
    w_merge = din("w_merge", [1024, 2048])
    b_merge_col = din("b_merge_col", [128, 16])
    w_b1 = din("w_b1", [512, 1024])
    w_out = din("w_out", [1024, 1024])
    wsT = din("wsT", [128, 1024])
    sgub_col = din("sgub_col", [128, 8])
    trilm = din("trilm", [128, 128])
    peer_wq = din("peer_wq", [1024, 2048])
    keysT = din("keysT", [128, 2048])
    UT = din("UT", [128, 128, 1024])
    VJ = din("VJ", [128, 128, 1024])
    y = kb.dram("y", [S, D], F32, kind="ExternalOutput")
    x1s = kb.dram("x1s", [S, D], F32)
    UTb = kb.dram("UTb", [128, 128, 1024], BF16)
    VJb = kb.dram("VJb", [128, 128, 1024], BF16)
    NEG = -30000.0
    if with_nsa:
        WAf = din("WAf", [1024, 1920])
        WAt = din("WAt", [1024, 280])
        cosT = din("cosT", [128, S])
        sinS = din("sinS", [128, S])
        w1d = din("w1d", [2, 128, 4096])
        posTd = din("posTd", [128, 64])
        b1col = din("b1col", [128, 2])
        w2kd = din("w2kd", [128, 128])
        w2v = din("w2v", [128, 64])
        b2kcol = din("b2kcol", [128, 1])
        b2vrow = din("b2vrow", [1, 64])
        aggd = din("aggd", [128, 512])
        cmpb = din("cmpb", [128, 2048])
        caus = din("caus", [128, 256])
        based = din("based", [128, 382])
        ebig = din("ebig", [128, S])
        w_b0 = din("w_b0", [512, 1024])
        QTd = kb.dram("QTd", [128, 4 * S], BF16)
        ONd = kb.dram("ONd", [128, 4 * S], BF16)

    ident_f = kb.sb("ident_f", [128, 128], F32)
    ident_b = kb.sb("ident_b", [128, 128], BF16)
    ones_row = kb.sb("ones_row", [1, 128], F32)
    eps_col = kb.sb("eps_col", [128, 1], F32)
    modcol = kb.sb("modcol", [128, 32], F32)
    gt_bc = kb.sb("gt_bc", [128, 2048], F32)
    ln_bc = kb.sb("ln_bc", [128, 5120], F32)
    iota128 = kb.sb("iota128", [128, 128], F32)
    thr15 = kb.sb("thr15", [128, 15], F32)

    kb.op("pool", "memset", [], [ident_f], ident_f[:], 0.0)
    kb.op("pool", "affine_select", [ident_f], [ident_f], out=ident_f[:], in_=ident_f[:], pattern=[[-1, 128]],
          compare_op=ALU.not_equal, fill=1.0, base=0, channel_multiplier=1)
    kb.op("dve", "tensor_copy", [ident_f], [ident_b], out=ident_b[:], in_=ident_f[:])
    kb.op("pool", "memset", [], [ones_row], ones_row[:], 1.0)
    kb.op("pool", "memset", [], [eps_col], eps_col[:], EPS)
    kb.op("pool", "iota", [], [iota128], iota128[:], [[1, 128]], base=0, channel_multiplier=0,
          allow_small_or_imprecise_dtypes=True)
    kb.op("pool", "iota", [], [thr15], thr15[:], [[16, 15]], base=16, channel_multiplier=0,
          allow_small_or_imprecise_dtypes=True)

    def wslab(src, k, n, c0, w):
        return src.ap(c0, [[n, 128], [128 * n, k], [1, w]])

    kb.mark("P")
    with ExitStack() as es:
        wa = [kb.sb("wa%d" % i, [128, 8, 512], F32, es) for i in range(2)]
        mod_row = kb.sb("mod_row", [1, 6144], F32, es)
        b_row = kb.sb("b_row", [1, 6144], F32, es)
        rows_sb = kb.sb("rows_sb", [1, 5120], F32, es)
        ccol = kb.sb("ccol", [128, 8], F32, es)
        scol = kb.sb("scol", [128, 8], F32, es)
        P0 = kb.ps("pP0", [128, 512], F32, es)
        P1 = kb.ps("pP1", [128, 512], F32, es)
        kb.dma("sp", ccol[:], c_col[:], [c_col], [ccol])
        kb.dma("sp", b_row[:], b_ada[:], [b_ada], [b_row])
        kb.dma("sp", rows_sb[:], rows[:], [rows], [rows_sb])
        kb.op("act", "activation", [ccol], [scol], out=scol[:], in_=ccol[:], func=AF.Silu)
        for blk in range(12):
            wt = wa[blk % 2]
            kb.dma("sp", wt[:], wslab(w_ada, 8, 6144, blk * 512, 512), [w_ada], [wt])
            Pb = P0 if blk % 2 == 0 else P1
            for k in range(8):
                kb.op("pe", "matmul", [scol, wt], [Pb], Pb[0:1, 0:512], lhsT=scol[:, k:k + 1], rhs=wt[:, k, :],
                      start=(k == 0), stop=(k == 7))
            kb.op("dve", "tensor_tensor", [b_row], [mod_row, Pb], out=mod_row[0:1, blk * 512:(blk + 1) * 512],
                  in0=Pb[0:1, 0:512], in1=b_row[0:1, blk * 512:(blk + 1) * 512], op=ALU.add)
        for i, c0 in enumerate([2048, 2560, 5120, 5632]):
            Pb = P0 if i % 2 == 0 else P1
            kb.op("pe", "matmul", [ones_row, mod_row], [Pb], Pb[:, 0:512], lhsT=ones_row[0:1, :],
                  rhs=mod_row[0:1, c0:c0 + 512], start=True, stop=True)
            kb.op("act", "copy", [], [gt_bc, Pb], out=gt_bc[:, i * 512:(i + 1) * 512], in_=Pb[:, 0:512])
        for i in range(10):
            Pb = P0 if i % 2 == 0 else P1
            kb.op("pe", "matmul", [ones_row, rows_sb], [Pb], Pb[:, 0:512], lhsT=ones_row[0:1, :],
                  rhs=rows_sb[0:1, i * 512:(i + 1) * 512], start=True, stop=True)
            kb.op("act", "copy", [], [ln_bc, Pb], out=ln_bc[:, i * 512:(i + 1) * 512], in_=Pb[:, 0:512])
        chunks = list(range(0, 8)) + list(range(8, 16)) + list(range(24, 32)) + list(range(32, 40))
        for j, cch in enumerate(chunks):
            kb.op("pe", "matmul", [ones_row, mod_row], [P0], P0[:, j:j + 1], lhsT=mod_row[0:1, cch * 128:(cch + 1) * 128],
                  rhs=ones_row[0:1, 0:1], start=True, stop=True)
        kb.op("dve", "tensor_copy", [], [modcol, P0], out=modcol[:], in_=P0[:, 0:32])
        kb.op("dve", "tensor_scalar", [modcol], [modcol], out=modcol[:, 8:16], in0=modcol[:, 8:16], scalar1=1.0,
              scalar2=None, op0=ALU.add)
        kb.op("dve", "tensor_scalar", [modcol], [modcol], out=modcol[:, 24:32], in0=modcol[:, 24:32], scalar1=1.0,
              scalar2=None, op0=ALU.add)
        kb.barrier()

    def layernorm_stats(src, st, mv, sd, rstd):
        kb.op("dve", "bn_stats", [src], [st], out=st[:, 0:6], in_=src[:, 0:512])
        kb.op("dve", "bn_stats", [src], [st], out=st[:, 6:12], in_=src[:, 512:1024])
        kb.op("dve", "bn_aggr", [st], [mv], out=mv[:, 0:2], in_=st[:, 0:12])
        kb.op("act", "activation", [mv, eps_col], [sd], out=sd[:, 0:1], in_=mv[:, 1:2], func=AF.Sqrt,
              bias=eps_col[:, 0:1], scale=1.0)
        kb.op("dve", "reciprocal", [sd], [rstd], out=rstd[:, 0:1], in_=sd[:, 0:1])

    def ln_transpose(src, xn_b, PT, dstT, col0, ncols, sc_off, sh_off, st, mv, sd, rstd):
        layernorm_stats(src, st, mv, sd, rstd)
        kb.op("dve", "tensor_scalar", [src, mv, rstd], [xn_b], out=xn_b[:], in0=src[:], scalar1=mv[:, 0:1],
              scalar2=rstd[:, 0:1], op0=ALU.subtract, op1=ALU.mult)
        for k in range(8):
            kb.op("pe", "transpose", [xn_b, ident_b], [PT], PT[:, k * 128:(k + 1) * 128], xn_b[:, k * 128:(k + 1) * 128],
                  ident_b[:])
        for k in range(8):
            kb.op("act", "activation", [modcol], [dstT, PT], out=dstT[:, k, col0:col0 + 128],
                  in_=PT[:, k * 128:(k + 1) * 128], func=AF.Identity, bias=modcol[:, sh_off + k:sh_off + k + 1],
                  scale=modcol[:, sc_off + k:sc_off + k + 1])

    def resid_ln(xsrc, Pa, Pb, gt_off, g_off, b_off, ybuf, obuf, st, mv, sd, rstd):
        for half, Pp in enumerate([Pa, Pb]):
            kb.op("dve", "tensor_tensor", [gt_bc], [ybuf, Pp], out=ybuf[:, half * 512:(half + 1) * 512], in0=Pp[:, 0:512],
                  in1=gt_bc[:, gt_off + half * 512:gt_off + (half + 1) * 512], op=ALU.mult)
        kb.op("dve", "scalar_tensor_tensor", [xsrc, ybuf], [ybuf], out=ybuf[:], in0=xsrc[:], scalar=ALPHA, in1=ybuf[:],
              op0=ALU.mult, op1=ALU.add)
        layernorm_stats(ybuf, st, mv, sd, rstd)
        kb.op("dve", "scalar_tensor_tensor", [ybuf, mv, ln_bc], [obuf], out=obuf[:], in0=ybuf[:], scalar=mv[:, 0:1],
              in1=ln_bc[:, g_off:g_off + 1024], op0=ALU.subtract, op1=ALU.mult)
        kb.op("dve", "scalar_tensor_tensor", [obuf, rstd, ln_bc], [obuf], out=obuf[:], in0=obuf[:], scalar=rstd[:, 0:1],
              in1=ln_bc[:, b_off:b_off + 1024], op0=ALU.mult, op1=ALU.add)


    kb.mark("A")
    if with_nsa:
        NQ = S // 128
        es1 = ExitStack()
        KsT = kb.sb("KsT", [128, S], BF16, es1)
        KwT = kb.sb("KwT", [128, S], BF16, es1)
        VS = kb.sb("VS", [128, NQ * 130], BF16, es1)
        VW = kb.sb("VW", [128, NQ * 130], BF16, es1)
        Gt = kb.sb("Gt", [128, NQ * 24], F32, es1)
        KCT = kb.sb("KCT", [128, 512], BF16, es1)
        VC = kb.sb("VC", [128, 4 * 130], BF16, es1)
        kb.op("pool", "memset", [], [VS], VS[:], 1.0)
        kb.op("pool", "memset", [], [VW], VW[:], 1.0)
        kb.op("pool", "memset", [], [VC], VC[:], 1.0)
        kb.op("pool", "memset", [], [KCT], KCT[:], 0.0)
        with ExitStack() as es:
            KcR = kb.sb("KcR", [128, S], BF16, es)
            VcR = kb.sb("VcR", [128, S], BF16, es)
            xs1 = kb.sb("xsA", [128, 1024], F32, es)
            xn_b = kb.sb("xn_bA", [128, 1024], BF16, es)
            hT = kb.sb("hTA", [128, 8, 512], BF16, es)
            WAt_b = kb.sb("WAt_b", [128, 8, 280], BF16, es)
            wch = [kb.sb("wch%d" % i, [128, 8, 128], BF16, es) for i in range(4)]
            cs = kb.sb("cs", [128, 512], F32, es)
            sn = kb.sb("sn", [128, 512], F32, es)
            t1 = kb.sb("t1", [128, 512], F32, es)
            t2 = kb.sb("t2", [128, 512], F32, es)
            qtmp = [kb.sb("qtmp%d" % i, [128, 512], BF16, es) for i in range(2)]
            st = kb.sb("stA", [128, 12], F32, es)
            mv = kb.sb("mvA", [128, 2], F32, es)
            sd = kb.sb("sdA", [128, 1], F32, es)
            rstd = kb.sb("rstdA", [128, 1], F32, es)
            w1_b = [kb.sb("w1_b%d" % i, [128, 4096], BF16, es) for i in range(2)]
            posT_b = kb.sb("posT_b", [128, 64], BF16, es)
            b1c = kb.sb("b1c", [128, 2], F32, es)
            w2k_b = kb.sb("w2k_b", [128, 128], BF16, es)
            w2v_b = kb.sb("w2v_b", [128, 64], BF16, es)
            b2kc = kb.sb("b2kc", [128, 1], F32, es)
            b2v_b = kb.sb("b2v_b", [1, 64], BF16, es)
            ones_b = kb.sb("ones_b", [1, 128], BF16, es)
            hidT = kb.sb("hidT", [128, 512], BF16, es)
            biasv = kb.sb("biasv", [128, 1], F32, es)
            PT = kb.ps("PTA", [128, 1024], BF16, es)
            Pq = kb.ps("Pq", [128, 512], F32, es)
            Pqs = kb.ps("Pqs", [128, 512], F32, es)
            PV = kb.ps("PV", [128, 512], F32, es)

            kb.dma("pool", WAt_b[:], wslab(WAt, 8, 280, 0, 280), [WAt], [WAt_b])
            nw = 0
            for tg in range(S // 512):
                t0 = tg * 512
                for s in range(4):
                    kb.dma("sp", xs1[:], x.ap((t0 + s * 128) * D, [[D, 128], [1, D]]), [x], [xs1])
                    ln_transpose(xs1, xn_b, PT, hT, s * 128, 128, 8, 0, st, mv, sd, rstd)
                kb.dma("sp", cs[:], cosT.ap(t0, [[S, 128], [1, 512]]), [cosT], [cs])
                kb.dma("sp", sn[:], sinS.ap(t0, [[S, 128], [1, 512]]), [sinS], [sn])
                for s in range(4):
                    tile = tg * 4 + s
                    for k in range(8):
                        kb.op("pe", "matmul", [hT, WAt_b], [PV], PV[:, 0:280], lhsT=hT[:, k, s * 128:(s + 1) * 128],
                              rhs=WAt_b[:, k, :], start=(k == 0), stop=(k == 7))
                    kb.op("act", "copy", [], [VS, PV], out=VS.ap(tile * 130, [[65, 2], [1, 64]]),
                          in_=PV.ap(0, [[64, 2], [1, 64]]))
                    kb.op("act", "copy", [], [VW, PV], out=VW.ap(tile * 130, [[65, 2], [1, 64]]),
                          in_=PV.ap(128, [[64, 2], [1, 64]]))
                    kb.op("act", "activation", [], [Gt, PV], out=Gt.ap(tile * 24, [[1, 8], [8, 3]]),
                          in_=PV.ap(256, [[3, 8], [1, 3]]), func=AF.Sigmoid)
                jobs = [(c, c + 4, "q", c) for c in range(4)] + [(8, 11, "kc", 0), (9, 12, "ks", 0), (10, 13, "kw", 0)]
                for ji, (ca, cb_, kind, r) in enumerate(jobs):
                    wa_ = wch[nw % 4]
                    wb_ = wch[(nw + 1) % 4]
                    nw += 2
                    kb.dma("pool", wa_[:], wslab(WAf, 8, 1920, ca * 128, 128), [WAf], [wa_])
                    kb.dma("pool", wb_[:], wslab(WAf, 8, 1920, cb_ * 128, 128), [WAf], [wb_])
                    for (wt_, Pp) in ((wa_, Pq), (wb_, Pqs)):
                        for k in range(8):
                            kb.op("pe", "matmul", [hT, wt_], [Pp], Pp[:, 0:512], lhsT=wt_[:, k, :], rhs=hT[:, k, :],
                                  start=(k == 0), stop=(k == 7))
                    kb.op("dve", "tensor_tensor", [cs], [t1, Pq], out=t1[:], in0=Pq[:, 0:512], in1=cs[:], op=ALU.mult)
                    kb.op("dve", "tensor_tensor", [sn], [t2, Pqs], out=t2[:], in0=Pqs[:, 0:512], in1=sn[:], op=ALU.mult)
                    if kind == "q":
                        qb = qtmp[ji % 2]
                        kb.op("dve", "tensor_tensor", [t1, t2], [qb], out=qb[:], in0=t1[:], in1=t2[:], op=ALU.add)
                        kb.dma("sp", QTd.ap(r * S + t0, [[4 * S, 128], [1, 512]]), qb[:], [qb], [QTd])
                    else:
                        dstT = {"kc": KcR, "ks": KsT, "kw": KwT}[kind]
                        kb.op("dve", "tensor_tensor", [t1, t2], [dstT], out=dstT[:, t0:t0 + 512], in0=t1[:], in1=t2[:],
                              op=ALU.add)
                wv_ = wch[nw % 4]
                nw += 1
                kb.dma("pool", wv_[:], wslab(WAf, 8, 1920, 14 * 128, 128), [WAf], [wv_])
                for k in range(8):
                    kb.op("pe", "matmul", [hT, wv_], [Pq], Pq[:, 0:512], lhsT=wv_[:, k, :], rhs=hT[:, k, :],
                          start=(k == 0), stop=(k == 7))
                kb.op("act", "copy", [], [VcR, Pq], out=VcR[:, t0:t0 + 512], in_=Pq[:, 0:512])

            kb.mark("B")
            if S >= 512:
                ncmp = (S - 32) // 16 + 1
                for kv in range(2):
                    kb.dma("pool", w1_b[kv][:], w1d.ap(kv * 128 * 4096, [[4096, 128], [1, 4096]]), [w1d], [w1_b[kv]])
                kb.dma("pool", posT_b[:], posTd[:], [posTd], [posT_b])
                kb.dma("sp", b1c[:], b1col[:], [b1col], [b1c])
                kb.dma("pool", w2k_b[:], w2kd[:], [w2kd], [w2k_b])
                kb.dma("pool", w2v_b[:], w2v[:], [w2v], [w2v_b])
                kb.dma("sp", b2kc[:], b2kcol[:], [b2kcol], [b2kc])
                kb.dma("pool", b2v_b[:], b2vrow[:], [b2vrow], [b2v_b])
                kb.op("pool", "memset", [], [ones_b], ones_b[:], 1.0)
                kb.op("pool", "memset", [], [hidT], hidT[:], 0.0)
                nc_pad = min(ncmp, 511)
                for kv in range(2):
                    raw = KcR if kv == 0 else VcR
                    for p in range(32):
                        kb.op("pe", "matmul", [w1_b[kv], posT_b], [PV], PV[:, 0:1],
                              lhsT=w1_b[kv].ap(p * 128, [[1, 128]], parts=64), rhs=posT_b.ap(kv * 32 + p, [[1, 1]], parts=64),
                              start=(p == 0), stop=(p == 31))
                    kb.op("dve", "tensor_tensor", [b1c], [biasv, PV], out=biasv[:], in0=PV[:, 0:1], in1=b1c[:, kv:kv + 1],
                          op=ALU.add)
                    for g in range(2):
                        for p in range(32):
                            kb.op("pe", "matmul", [w1_b[kv], raw], [Pq], Pq[:, 0:nc_pad],
                                  lhsT=w1_b[kv].ap(p * 128, [[1, 128]], parts=64, pstart=g * 64),
                                  rhs=raw.ap(p, [[16, nc_pad]], parts=64, pstart=g * 64), start=(p == 0), stop=(p == 31))
                        kb.op("act", "activation", [biasv], [hidT, Pq], out=hidT[:, 0:nc_pad], in_=Pq[:, 0:nc_pad], func=AF.Gelu,
                              bias=biasv[:, 0:1], scale=1.0)
                        if kv == 0:
                            kb.op("pe", "matmul", [w2k_b, hidT], [Pqs], Pqs[:, 0:nc_pad], lhsT=w2k_b[:], rhs=hidT[:, 0:nc_pad],
                                  start=True, stop=True)
                            kb.op("act", "activation", [b2kc], [KCT, Pqs], out=KCT.ap(0, [[1, nc_pad]], parts=64, pstart=g * 64),
                                  in_=Pqs.ap(0, [[1, nc_pad]], parts=64, pstart=g * 64), func=AF.Identity,
                                  bias=b2kc.ap(0, [[1, 1]], parts=64, pstart=g * 64), scale=1.0)
                        else:
                            for nt in range(4):
                                kb.op("pe", "matmul", [hidT, w2v_b], [PV], PV[:, 0:64], lhsT=hidT[:, nt * 128:(nt + 1) * 128],
                                      rhs=w2v_b[:], start=True, stop=False)
                                kb.op("pe", "matmul", [ones_b, b2v_b], [PV], PV[:, 0:64], lhsT=ones_b[0:1, :], rhs=b2v_b[0:1, :],
                                      start=False, stop=True)
                                kb.op("act", "copy", [], [VC, PV], out=VC.ap((nt * 2 + g) * 65, [[1, 64]]), in_=PV[:, 0:64])
            kb.barrier()

        kb.mark("C1")
        with ExitStack() as es:
            E_b = kb.sb("E_b", [128, S], BF16, es)
            CMPB = kb.sb("CMPB", [128, 2048], BF16, es)
            CAUS = kb.sb("CAUS", [128, 256], BF16, es)
            AGG = kb.sb("AGG", [128, 512], BF16, es)
            BASE = kb.sb("BASE", [128, 382], F32, es)
            Qt = kb.sb("Qt", [128, 512], BF16, es)
            Pt = [kb.sb("Pt%d" % i, [128, 512], BF16, es) for i in range(3)]
            Osb = kb.sb("Osb", [128, 3 * 512], F32, es)
            onsa = kb.sb("onsa", [128, 512], F32, es)
            onT = kb.sb("onT", [128, 512], BF16, es)
            imp = kb.sb("imp", [128, 128], F32, es)
            val = kb.sb("val", [128, 128], F32, es)
            val2 = kb.sb("val2", [128, 128], F32, es)
            negs = kb.sb("negs", [128, 128], F32, es)
            negT = kb.sb("negT", [128, 128], BF16, es)
            m8 = kb.sb("m8", [128, 16], F32, es)
            zc = kb.sb("zc", [128, 4], F32, es)
            rzc = kb.sb("rzc", [128, 4], F32, es)
            zt = kb.sb("zt", [128, 4], F32, es)
            coef = kb.sb("coef", [128, 4], F32, es)
            PS = [kb.ps("PS%d" % i, [128, 512], F32, es) for i in range(3)]
            PO3 = [kb.ps("PO3_%d" % i, [128, 512], F32, es) for i in range(3)]
            PU = kb.ps("PU", [128, 512], F32, es)
            PTr = kb.ps("PTr", [128, 512], F32, es)

            kb.dma("pool", E_b[:], ebig[:], [ebig], [E_b])
            kb.dma("pool", CMPB[:], cmpb[:], [cmpb], [CMPB])
            kb.dma("pool", CAUS[:], caus[:], [caus], [CAUS])
            kb.dma("pool", AGG[:], aggd[:], [aggd], [AGG])
            kb.dma("sp", BASE[:], based[:], [based], [BASE])
            npt = [0]

            pend = []

            def flush(keep=0):
                while len(pend) > keep:
                    pend.pop(0)()

            def attn_tile(Pout, first, last, kT, kcol, Vt, voff, g, biases, extra=None):
                Ps = PS[npt[0] % 3]
                Pb = Pt[npt[0] % 3]
                npt[0] += 1
                nb = len(biases)
                for bi, (lt, lap, rt, rap) in enumerate(biases):
                    kb.op("pe", "matmul", [lt, rt], [Ps], Ps[:, 0:512], lhsT=lap, rhs=rap, start=(bi == 0), stop=False)
                kb.op("pe", "matmul", [kT, Qt], [Ps], Ps[:, 0:512], lhsT=kT.ap(kcol, [[1, 128]], parts=64, pstart=g * 64),
                      rhs=Qt.ap(0, [[128, 4], [1, 128]], parts=64, pstart=g * 64), start=(nb == 0), stop=True)
                kb.op("act", "activation", [], [Pb, Ps], out=Pb[:], in_=Ps[:, 0:512], func=AF.Exp, scale=0.125)
                flush(keep=1)

                def pv():
                    kb.op("pe", "matmul", [Vt, Pb], [Pout], Pout.ap(0, [[1, 512]], parts=65), lhsT=Vt.ap(voff, [[1, 65]]),
                          rhs=Pb[:], start=first, stop=last)
                    if extra is not None:
                        extra(Pb)
                pend.append(pv)

            for qt in range(NQ):
                kb.dma("sp", Qt.ap(0, [[128, 4], [1, 128]]), QTd.ap(qt * 128, [[4 * S, 128], [S, 4], [1, 128]]), [QTd], [Qt])
                for g in range(2):
                    ntmax = min(3, (8 * qt + 6) // 128)
                    for nt in range(ntmax + 1):
                        m = qt - 16 * nt
                        biases = []
                        if m <= 15:
                            biases.append((ident_b, ident_b[:], CMPB, CMPB.ap(m * 128, [[0, 4], [1, 128]])))
                        def imp_mm(Pb, nt=nt, ntmax=ntmax):
                            for r in range(4):
                                kb.op("pe", "matmul", [Pb, AGG], [PU], PU[:, r * 128:(r + 1) * 128], lhsT=Pb[:, r * 128:(r + 1) * 128],
                                      rhs=AGG[:, nt * 128:(nt + 1) * 128], start=(nt == 0 and r == 0), stop=(nt == ntmax and r == 3),
                                      skip_group_check=True)
                        attn_tile(PO3[0], nt == 0, nt == ntmax, KCT, nt * 128, VC, (nt * 2 + g) * 65, g, biases, extra=imp_mm)
                    flush()
                    kb.op("dve", "tensor_reduce", [], [zc, PU], out=zc[:], in_=PU.ap(0, [[128, 4], [1, 128]]), axis=AX.X, op=ALU.add)
                    kb.op("dve", "tensor_scalar", [zc], [zc], out=zc[:], in0=zc[:], scalar1=1e-30, scalar2=None, op0=ALU.max)
                    kb.op("dve", "reciprocal", [zc], [rzc], out=rzc[:], in_=zc[:])
                    kb.op("dve", "tensor_scalar", [rzc], [imp, PU], out=imp[:], in0=PU[:, 0:128], scalar1=rzc[:, 0:1], scalar2=None,
                          op0=ALU.mult)
                    for r in range(1, 4):
                        kb.op("dve", "scalar_tensor_tensor", [rzc, imp], [imp, PU], out=imp[:], in0=PU[:, r * 128:(r + 1) * 128],
                              scalar=rzc[:, r:r + 1], in1=imp[:], op0=ALU.mult, op1=ALU.add)
                    kb.op("dve", "tensor_tensor", [imp, BASE], [val], out=val[:], in0=imp[:], in1=BASE[:, 126 - 2 * qt:254 - 2 * qt],
                          op=ALU.add)
                    kb.op("dve", "tensor_tensor", [val, BASE], [val], out=val[:], in0=val[:], in1=BASE[:, 254:382], op=ALU.add)
                    kb.op("dve", "max", [val], [m8], out=m8[:, 0:8], in_=val[:])
                    kb.op("dve", "match_replace", [m8, val], [val2], out=val2[:], in_to_replace=m8[:, 0:8], in_values=val[:],
                          imm_value=-3e38)
                    kb.op("dve", "max", [val2], [m8], out=m8[:, 8:16], in_=val2[:])
                    kb.op("dve", "tensor_scalar", [val, m8], [negs], out=negs[:], in0=val[:], scalar1=m8[:, 15:16], scalar2=NEG,
                          op0=ALU.is_lt, op1=ALU.mult)
                    kb.op("pe", "transpose", [negs, ident_f], [PTr], PTr[:, 0:128], negs[:], ident_f[:])
                    kb.op("act", "copy", [], [negT, PTr], out=negT[:], in_=PTr[:, 0:128])
                    def finalize_branch(br, init):
                        kb.op("act", "copy", [], [Osb, PO3[br]], out=Osb.ap(br * 512, [[1, 512]], parts=65),
                              in_=PO3[br].ap(0, [[1, 512]], parts=65))
                        for r in range(4):
                            kb.op("pe", "transpose", [Osb, ident_f], [PTr], PTr[:, r * 65:(r + 1) * 65],
                                  Osb.ap(br * 512 + r * 128, [[1, 128]], parts=65), ident_f.ap(0, [[1, 65]], parts=65))
                        kb.op("dve", "tensor_scalar", [], [zt, PTr], out=zt[:], in0=PTr.ap(64, [[65, 4]]), scalar1=1e-30, scalar2=None,
                              op0=ALU.max)
                        kb.op("dve", "reciprocal", [zt], [zt], out=zt[:], in_=zt[:])
                        kb.op("dve", "tensor_tensor", [zt, Gt], [coef], out=coef[:], in0=zt[:],
                              in1=Gt[:, qt * 24 + br * 8 + g * 4:qt * 24 + br * 8 + g * 4 + 4], op=ALU.mult)
                        for r in range(4):
                            h = g * 4 + r
                            if init:
                                kb.op("dve", "tensor_scalar", [coef], [onsa, PTr], out=onsa[:, h * 64:(h + 1) * 64],
                                      in0=PTr[:, r * 65:r * 65 + 64], scalar1=coef[:, r:r + 1], scalar2=None, op0=ALU.mult)
                            else:
                                kb.op("dve", "scalar_tensor_tensor", [coef, onsa], [onsa, PTr], out=onsa[:, h * 64:(h + 1) * 64],
                                      in0=PTr[:, r * 65:r * 65 + 64], scalar=coef[:, r:r + 1], in1=onsa[:, h * 64:(h + 1) * 64],
                                      op0=ALU.mult, op1=ALU.add)

                    finalize_branch(0, True)
                    k0 = max(0, qt - 4)
                    for kt in range(k0, qt + 1):
                        biases = []
                        if kt == qt:
                            biases.append((ident_b, ident_b[:], CAUS, CAUS.ap(0, [[0, 4], [1, 128]])))
                        elif kt == qt - 4:
                            biases.append((ident_b, ident_b[:], CAUS, CAUS.ap(128, [[0, 4], [1, 128]])))
                        attn_tile(PO3[2], kt == k0, kt == qt, KwT, kt * 128, VW, (kt * 2 + g) * 65, g, biases)
                    flush()
                    finalize_branch(2, False)
                    for kt in range(qt + 1):
                        biases = [(E_b, E_b[:, kt * 128:(kt + 1) * 128], negT, negT.ap(0, [[0, 4], [1, 128]]))]
                        if kt == qt:
                            biases.append((ident_b, ident_b[:], CAUS, CAUS.ap(0, [[0, 4], [1, 128]])))
                        attn_tile(PO3[1], kt == 0, kt == qt, KsT, kt * 128, VS, (kt * 2 + g) * 65, g, biases)
                    flush()
                    finalize_branch(1, False)
                for kc in range(4):
                    kb.op("pe", "transpose", [onsa, ident_f], [PTr], PTr[:, kc * 128:(kc + 1) * 128], onsa[:, kc * 128:(kc + 1) * 128],
                          ident_f[:])
                kb.op("act", "copy", [], [onT, PTr], out=onT[:], in_=PTr[:, 0:512])
                kb.dma("sp", ONd.ap(qt * 128, [[4 * S, 128], [S, 4], [1, 128]]), onT.ap(0, [[128, 4], [1, 128]]), [onT], [ONd])
            kb.barrier()
        es1.close()

    kb.mark("W")
    if with_peer:
        with ExitStack() as es:
            cb = [kb.sb("cb%d" % i, [128, 2048], BF16, es) for i in range(4)]
            n = 0
            for src, dst in ((UT, UTb), (VJ, VJb)):
                for j2 in range(64):
                    b = cb[n % 4]
                    n += 1
                    kb.dma("pool", b.ap(0, [[1024, 2], [1, 1024]]), src.ap(j2 * 2 * 131072, [[1024, 128], [131072, 2], [1, 1024]]),
                           [src], [b])
                    kb.dma("sp", dst.ap(j2 * 2 * 131072, [[1024, 128], [131072, 2], [1, 1024]]), b.ap(0, [[1024, 2], [1, 1024]]),
                           [b], [dst])
            kb.barrier()

    kb.mark("C2")
    with ExitStack() as es:
        Wz_b = kb.sb("Wz_b", [128, 8, 1024], BF16, es)
        Wm_b = kb.sb("Wm_b", [128, 8, 2048], BF16, es)
        Wb1_b = kb.sb("Wb1_b", [128, 4, 1024], BF16, es)
        Wo_b = kb.sb("Wo_b", [128, 8, 1024], BF16, es)
        WsT_f = kb.sb("WsT_f", [128, 1024], F32, es)
        WsT_b = kb.sb("WsT_b", [128, 8, 128], BF16, es)
        tril_sb = kb.sb("tril_sb", [128, 128], F32, es)
        sgub = kb.sb("sgub", [128, 8], F32, es)
        bmcol = kb.sb("bmcol", [128, 16], F32, es)
        xs = [kb.sb("xs%d" % i, [128, 1024], F32, es) for i in range(4)]
        xn_b = kb.sb("xn_b", [128, 1024], BF16, es)
        hT = kb.sb("hT", [128, 8, 512], BF16, es)
        gab = kb.sb("gab", [128, 2, 512], BF16, es)
        u_b = kb.sb("u_b", [128, 512], BF16, es)
        v_f = kb.sb("v_f", [128, 512], F32, es)
        vb = kb.sb("vb", [128, 512], BF16, es)
        tmp = kb.sb("tmp", [128, 512], F32, es)
        osgu = kb.sb("osgu", [128, 512], BF16, es)
        osT = kb.sb("osT", [128, 4, 512], BF16, es)
        mT = kb.sb("mT", [128, 8, 512], BF16, es)
        ybuf = kb.sb("ybuf", [128, 1024], F32, es)
        x1 = kb.sb("x1", [128, 1024], F32, es)
        st = kb.sb("st", [128, 12], F32, es)
        mv = kb.sb("mv", [128, 2], F32, es)
        sd = kb.sb("sd", [128, 1], F32, es)
        rstd = kb.sb("rstd", [128, 1], F32, es)
        PT = kb.ps("PT", [128, 1024], BF16, es)
        PGA = kb.ps("PGA", [128, 512], F32, es)
        PGB = kb.ps("PGB", [128, 512], F32, es)
        PB_ = kb.ps("PB_", [128, 512], F32, es)
        if with_nsa:
            PA_ = kb.ps("PA_", [128, 512], F32, es)
            Wb0_b = kb.sb("Wb0_b", [128, 4, 1024], BF16, es)
            onsaT = kb.sb("onsaT", [128, 4, 512], BF16, es)
            tmpa = kb.sb("tmpa", [128, 512], F32, es)
            kb.dma("pool", Wb0_b[:], wslab(w_b0, 4, 1024, 0, 1024), [w_b0], [Wb0_b])
        PZ0 = kb.ps("PZ0", [128, 512], F32, es)
        PZ1 = kb.ps("PZ1", [128, 512], F32, es)
        PM = kb.ps("PM", [128, 512], F32, es)

        kb.dma("pool", Wz_b[:], wslab(wz, 8, 1024, 0, 1024), [wz], [Wz_b])
        kb.dma("pool", Wm_b[:], wslab(w_merge, 8, 2048, 0, 2048), [w_merge], [Wm_b])
        kb.dma("pool", Wb1_b[:], wslab(w_b1, 4, 1024, 0, 1024), [w_b1], [Wb1_b])
        kb.dma("pool", Wo_b[:], wslab(w_out, 8, 1024, 0, 1024), [w_out], [Wo_b])
        kb.dma("sp", WsT_f[:], wsT[:], [wsT], [WsT_f])
        kb.dma("sp", tril_sb[:], trilm[:], [trilm], [tril_sb])
        kb.dma("sp", sgub[:], sgub_col[:], [sgub_col], [sgub])
        kb.dma("sp", bmcol[:], b_merge_col[:], [b_merge_col], [bmcol])
        kb.op("dve", "tensor_tensor", [WsT_f, tril_sb], [WsT_b], out=WsT_b.ap(0, [[128, 8], [1, 128]]),
              in0=WsT_f.ap(0, [[128, 8], [1, 128]]), in1=tril_sb.ap(0, [[0, 8], [1, 128]]), op=ALU.mult)

        for tg in range(S // 512):
            t0 = tg * 512
            for s in range(4):
                kb.dma("sp", xs[s][:], x.ap((t0 + s * 128) * D, [[D, 128], [1, D]]), [x], [xs[s]])
            for s in range(4):
                ln_transpose(xs[s], xn_b, PT, hT, s * 128, 128, 8, 0, st, mv, sd, rstd)
            if with_nsa:
                kb.dma("sp", onsaT.ap(0, [[512, 4], [1, 512]]), ONd.ap(t0, [[4 * S, 128], [S, 4], [1, 512]]), [ONd], [onsaT])
            for s in range(4):
                for half, Pz in enumerate([PZ0, PZ1]):
                    for k in range(8):
                        kb.op("pe", "matmul", [hT, Wz_b], [Pz], Pz[:, 0:512], lhsT=hT[:, k, s * 128:(s + 1) * 128],
                              rhs=Wz_b[:, k, half * 512:(half + 1) * 512], start=(k == 0), stop=(k == 7))
                kb.op("act", "activation", [], [u_b, PZ0], out=u_b[:], in_=PZ0[:, 0:512], func=AF.Gelu)
                kb.op("act", "activation", [], [v_f, PZ1], out=v_f[:], in_=PZ1[:, 0:512], func=AF.Gelu)
                kb.op("dve", "bn_stats", [v_f], [st], out=st[:, 0:6], in_=v_f[:])
                kb.op("dve", "bn_aggr", [st], [mv], out=mv[:, 0:2], in_=st[:, 0:6])
                kb.op("act", "activation", [mv, eps_col], [sd], out=sd[:, 0:1], in_=mv[:, 1:2], func=AF.Sqrt,
                      bias=eps_col[:, 0:1], scale=1.0)
                kb.op("dve", "reciprocal", [sd], [rstd], out=rstd[:, 0:1], in_=sd[:, 0:1])
                kb.op("dve", "scalar_tensor_tensor", [v_f, mv, ln_bc], [tmp], out=tmp[:], in0=v_f[:], scalar=mv[:, 0:1],
                      in1=ln_bc[:, 4096:4608], op0=ALU.subtract, op1=ALU.mult)
                kb.op("dve", "scalar_tensor_tensor", [tmp, rstd, ln_bc], [vb], out=vb[:], in0=tmp[:], scalar=rstd[:, 0:1],
                      in1=ln_bc[:, 4608:5120], op0=ALU.mult, op1=ALU.add)
                for g in range(8):
                    kb.op("pe", "matmul", [WsT_b, vb], [PM], PM[:, g * 64:(g + 1) * 64], lhsT=WsT_b[:, g, :],
                          rhs=vb[:, g * 64:(g + 1) * 64], start=True, stop=True)
                kb.op("dve", "tensor_tensor", [sgub], [tmp, PM], out=tmp.ap(0, [[64, 8], [1, 64]]),
                      in0=PM.ap(0, [[64, 8], [1, 64]]), in1=sgub.ap(0, [[1, 8], [0, 64]]), op=ALU.add)
                kb.op("dve", "tensor_tensor", [tmp, u_b], [osgu], out=osgu[:], in0=tmp[:], in1=u_b[:], op=ALU.mult)
                for kc in range(4):
                    kb.op("pe", "transpose", [osgu, ident_b], [PT], PT[:, kc * 128:(kc + 1) * 128],
                          osgu[:, kc * 128:(kc + 1) * 128], ident_b[:])
                kb.op("act", "copy", [], [osT, PT], out=osT.ap(s * 128, [[512, 4], [1, 128]]),
                      in_=PT.ap(0, [[128, 4], [1, 128]]))
            for cch in range(8):
                for gi, (Pg, col) in enumerate([(PGA, cch), (PGB, 8 + cch)]):
                    if gi == 0 and not with_nsa:
                        continue
                    for k in range(8):
                        kb.op("pe", "matmul", [hT, Wm_b], [Pg], Pg[:, 0:512], lhsT=Wm_b[:, k, col * 128:(col + 1) * 128],
                              rhs=hT[:, k, :], start=(k == 0), stop=(k == 7))
                for kc in range(4):
                    kb.op("pe", "matmul", [osT, Wb1_b], [PB_], PB_[:, 0:512], lhsT=Wb1_b[:, kc, cch * 128:(cch + 1) * 128],
                          rhs=osT[:, kc, :], start=(kc == 0), stop=(kc == 3))
                kb.op("act", "activation", [bmcol], [gab, PGB], out=gab[:, 1, :], in_=PGB[:, 0:512], func=AF.Sigmoid,
                      bias=bmcol[:, 8 + cch:9 + cch], scale=1.0)
                if with_nsa:
                    for kc in range(4):
                        kb.op("pe", "matmul", [onsaT, Wb0_b], [PA_], PA_[:, 0:512], lhsT=Wb0_b[:, kc, cch * 128:(cch + 1) * 128],
                              rhs=onsaT[:, kc, :], start=(kc == 0), stop=(kc == 3))
                    kb.op("act", "activation", [bmcol], [gab, PGA], out=gab[:, 0, :], in_=PGA[:, 0:512], func=AF.Sigmoid,
                          bias=bmcol[:, cch:cch + 1], scale=1.0)
                    kb.op("dve", "tensor_tensor", [gab], [tmpa, PA_], out=tmpa[:], in0=PA_[:, 0:512], in1=gab[:, 0, :], op=ALU.mult)
                    kb.op("dve", "tensor_tensor", [gab], [tmp, PB_], out=tmp[:], in0=PB_[:, 0:512], in1=gab[:, 1, :], op=ALU.mult)
                    kb.op("dve", "tensor_tensor", [tmp, tmpa], [mT], out=mT[:, cch, :], in0=tmp[:], in1=tmpa[:], op=ALU.add)
                else:
                    kb.op("dve", "tensor_tensor", [gab], [mT, PB_], out=mT[:, cch, :], in0=PB_[:, 0:512], in1=gab[:, 1, :],
                          op=ALU.mult)
            for s in range(4):
                for half, Pz in enumerate([PZ0, PZ1]):
                    for k in range(8):
                        kb.op("pe", "matmul", [mT, Wo_b], [Pz], Pz[:, 0:512], lhsT=mT[:, k, s * 128:(s + 1) * 128],
                              rhs=Wo_b[:, k, half * 512:(half + 1) * 512], start=(k == 0), stop=(k == 7))
                resid_ln(xs[s], PZ0, PZ1, 0, 0, 1024, ybuf, x1, st, mv, sd, rstd)
                dst = x1s if with_peer else y
                kb.dma("pool", dst.ap((t0 + s * 128) * D, [[D, 128], [1, D]]), x1[:], [x1], [dst])
        kb.barrier()

    if not with_peer:
        kb.wait_all("sp")
        return nc, kb

    kb.mark("D")
    NG = S // 256
    GTd2 = [kb.dram("GTd%d" % i, [128, 128 * 256], BF16) for i in range(2)]
    with ExitStack() as es:
        Wq_b = kb.sb("Wq_b", [128, 8, 2048], BF16, es)
        keys_b = kb.sb("keys_b", [128, 16, 128], BF16, es)
        GT = kb.sb("GT", [128, 128 * 128], BF16, es)
        NBUF = 3
        Ubuf = [kb.sb("Ubuf%d" % i, [128, 2, 1024], BF16, es) for i in range(NBUF)]
        Vbuf = [kb.sb("Vbuf%d" % i, [128, 2, 1024], BF16, es) for i in range(NBUF)]
        Gs = [kb.sb("Gs%d" % i, [128, 2, 256], BF16, es) for i in range(NBUF)]
        x1t = [kb.sb("x1t%d" % i, [128, 1024], F32, es) for i in range(2)]
        xr = kb.sb("xr", [128, 1024], F32, es)
        h2T2 = [kb.sb("h2T%d" % i, [128, 8, 256], BF16, es) for i in range(2)]
        qT = kb.sb("qT", [128, 16, 256], BF16, es)
        sc = kb.sb("sc", [128, 4, 128], F32, es)
        scrA = kb.sb("scrA", [128, 2048], F32, es)
        scrB = kb.sb("scrB", [128, 2048], F32, es)
        t16 = kb.sb("t16", [128, 256], F32, es)
        i16 = kb.sb("i16", [128, 256], U32, es)
        i16f = kb.sb("i16f", [128, 256], F32, es)
        tv = kb.sb("tv", [128, 128], F32, es)
        pv = kb.sb("pv", [128, 128], U32, es)
        pvf = kb.sb("pvf", [128, 128], F32, es)
        ee = kb.sb("ee", [128, 128], F32, es)
        zz = kb.sb("zz", [128, 8], F32, es)
        rz = kb.sb("rz", [128, 8], F32, es)
        ak = kb.sb("ak", [128, 128], F32, es)
        bk = kb.sb("bk", [128, 128], F32, es)
        III = kb.sb("III", [128, 384], F32, es)
        ITs = kb.sb("ITs", [128, 384], F32, es)
        iota16 = kb.sb("iota16", [128, 16], F32, es)
        CH = 16
        Lb = kb.sb("Lb", [128, CH * 128], BF16, es)
        Rb = kb.sb("Rb", [128, CH * 128], BF16, es)
        actg = [kb.sb("actg%d" % i, [128, 512], BF16, es) for i in range(2)]
        wd = [kb.sb("wd%d" % i, [128, 512], BF16, es) for i in range(2)]
        ybuf = kb.sb("ybuf2", [128, 1024], F32, es)
        st = kb.sb("st2", [128, 12], F32, es)
        mv = kb.sb("mv2", [128, 2], F32, es)
        sd = kb.sb("sd2", [128, 1], F32, es)
        rstd = kb.sb("rstd2", [128, 1], F32, es)
        stf = kb.sb("st3", [128, 12], F32, es)
        mvf = kb.sb("mv3", [128, 2], F32, es)
        sdf = kb.sb("sd3", [128, 1], F32, es)
        rstdf = kb.sb("rstd3", [128, 1], F32, es)
        PO = [kb.ps("PO%d" % i, [128, 512], F32, es) for i in range(4)]
        PA = [kb.ps("PA%d" % i, [128, 512], F32, es) for i in range(2)]
        PG = [kb.ps("PG%d" % i, [128, 512], F32, es) for i in range(2)]

        kb.dma("pool", Wq_b[:], wslab(peer_wq, 8, 2048, 0, 2048), [peer_wq], [Wq_b])
        kb.dma("pool", keys_b.ap(0, [[1, 2048]]), keysT[:], [keysT], [keys_b])
        kb.op("pool", "iota", [], [iota16], iota16[:], [[1, 16]], base=0, channel_multiplier=0,
              allow_small_or_imprecise_dtypes=True)

        def ln_transpose_f(src, dstT, col0, sc_off, sh_off):
            layernorm_stats(src, st, mv, sd, rstd)
            kb.op("dve", "tensor_scalar", [src, mv, rstd], [scrA], out=scrA[:, 0:1024], in0=src[:], scalar1=mv[:, 0:1],
                  scalar2=rstd[:, 0:1], op0=ALU.subtract, op1=ALU.mult)
            for k in range(8):
                Pp = PG[k // 4]
                kb.op("pe", "transpose", [scrA, ident_f], [Pp], Pp[:, (k % 4) * 128:(k % 4 + 1) * 128],
                      scrA[:, k * 128:(k + 1) * 128], ident_f[:])
            for k in range(8):
                Pp = PG[k // 4]
                kb.op("act", "activation", [modcol], [dstT, Pp], out=dstT[:, k, col0:col0 + 128],
                      in_=Pp[:, (k % 4) * 128:(k % 4 + 1) * 128], func=AF.Identity,
                      bias=modcol[:, sh_off + k:sh_off + k + 1], scale=modcol[:, sc_off + k:sc_off + k + 1])

        def prep(tg):
            t0 = tg * 256
            h2T = h2T2[tg % 2]
            GTd = GTd2[tg % 2]
            for s in range(2):
                kb.dma("sp", x1t[s][:], x1s.ap((t0 + s * 128) * D, [[D, 128], [1, D]]), [x1s], [x1t[s]])
                ln_transpose_f(x1t[s], h2T, s * 128, 24, 16)
                yield
            for cch in range(16):
                Pq = PG[cch % 2]
                for k in range(8):
                    kb.op("pe", "matmul", [h2T, Wq_b], [Pq], Pq[:, 0:256], lhsT=Wq_b[:, k, cch * 128:(cch + 1) * 128],
                          rhs=h2T[:, k, :], start=(k == 0), stop=(k == 7))
                kb.op("act", "copy", [], [qT, Pq], out=qT[:, cch, :], in_=Pq[:, 0:256])
                yield
            for s in range(2):
                ts = slice(s * 128, (s + 1) * 128)
                for r4 in range(4):
                    Ps = PG[r4 % 2]
                    for rr in range(4):
                        r = r4 * 4 + rr
                        kb.op("pe", "matmul", [qT, keys_b], [Ps], Ps[:, rr * 128:(rr + 1) * 128], lhsT=qT[:, r, ts],
                              rhs=keys_b[:, r, :], start=True, stop=True)
                    kb.op("act", "copy", [], [sc, Ps], out=sc.ap(0, [[1, 512]]), in_=Ps[:, 0:512])
                    yield
                    for rr in range(4):
                        r = r4 * 4 + rr
                        kb.op("dve", "max", [sc], [t16], out=t16[:, r * 16:r * 16 + 8], in_=sc[:, rr, :])
                        kb.op("dve", "max_index", [t16, sc], [i16], out=i16[:, r * 16:r * 16 + 8],
                              in_max=t16[:, r * 16:r * 16 + 8], in_values=sc[:, rr, :])
                        kb.op("dve", "match_replace", [t16, sc], [scrA], out=scrA[:, rr * 128:(rr + 1) * 128],
                              in_to_replace=t16[:, r * 16:r * 16 + 8], in_values=sc[:, rr, :], imm_value=-1e30)
                        kb.op("dve", "max", [scrA], [t16], out=t16[:, r * 16 + 8:r * 16 + 16],
                              in_=scrA[:, rr * 128:(rr + 1) * 128])
                        kb.op("dve", "max_index", [t16, scrA], [i16], out=i16[:, r * 16 + 8:r * 16 + 16],
                              in_max=t16[:, r * 16 + 8:r * 16 + 16], in_values=scrA[:, rr * 128:(rr + 1) * 128])
                        yield
                kb.op("dve", "tensor_copy", [i16], [i16f], out=i16f[:], in_=i16[:])
                kb.op("dve", "tensor_tensor", [t16], [scrB], out=scrB.ap(0, [[256, 8], [16, 16], [1, 16]]),
                      in0=t16.ap(0, [[32, 8], [1, 16], [0, 16]]), in1=t16.ap(16, [[32, 8], [0, 16], [1, 16]]), op=ALU.add)
                yield
                for h in range(8):
                    cs_ = slice(h * 256, (h + 1) * 256)
                    kb.op("dve", "max", [scrB], [tv], out=tv[:, h * 16:h * 16 + 8], in_=scrB[:, cs_])
                    kb.op("dve", "max_index", [tv, scrB], [pv], out=pv[:, h * 16:h * 16 + 8], in_max=tv[:, h * 16:h * 16 + 8],
                          in_values=scrB[:, cs_])
                    kb.op("dve", "match_replace", [tv, scrB], [scrA], out=scrA[:, cs_], in_to_replace=tv[:, h * 16:h * 16 + 8],
                          in_values=scrB[:, cs_], imm_value=-1e30)
                    kb.op("dve", "max", [scrA], [tv], out=tv[:, h * 16 + 8:h * 16 + 16], in_=scrA[:, cs_])
                    kb.op("dve", "max_index", [tv, scrA], [pv], out=pv[:, h * 16 + 8:h * 16 + 16],
                          in_max=tv[:, h * 16 + 8:h * 16 + 16], in_values=scrA[:, cs_])
                    yield
                kb.op("dve", "tensor_tensor", [tv], [ee], out=ee.ap(0, [[16, 8], [1, 16]]), in0=tv.ap(0, [[16, 8], [1, 16]]),
                      in1=tv.ap(0, [[16, 8], [0, 16]]), op=ALU.subtract)
                kb.op("act", "activation", [ee], [ee], out=ee[:], in_=ee[:], func=AF.Exp)
                kb.op("dve", "tensor_reduce", [ee], [zz], out=zz[:], in_=ee.ap(0, [[16, 8], [1, 16]]), axis=AX.X, op=ALU.add)
                kb.op("dve", "reciprocal", [zz], [rz], out=rz[:], in_=zz[:])
                kb.op("dve", "tensor_tensor", [ee, rz], [III], out=III.ap(256, [[16, 8], [1, 16]]),
                      in0=ee.ap(0, [[16, 8], [1, 16]]), in1=rz.ap(0, [[1, 8], [0, 16]]), op=ALU.mult)
                yield
                kb.op("dve", "tensor_copy", [pv], [pvf], out=pvf[:], in_=pv[:])
                kb.op("dve", "tensor_tensor", [pvf, thr15], [scrA], out=scrA.ap(0, [[15, 128], [1, 15]]),
                      in0=pvf.ap(0, [[1, 128], [0, 15]]), in1=thr15.ap(0, [[0, 128], [1, 15]]), op=ALU.is_ge)
                kb.op("dve", "tensor_reduce", [scrA], [ak], out=ak[:], in_=scrA.ap(0, [[15, 128], [1, 15]]), axis=AX.X, op=ALU.add)
                kb.op("dve", "scalar_tensor_tensor", [ak, pvf], [bk], out=bk[:], in0=ak[:], scalar=-16.0, in1=pvf[:],
                      op0=ALU.mult, op1=ALU.add)
                yield
                for which, (sel, off) in enumerate([(ak, 0), (bk, 16)]):
                    kb.op("dve", "tensor_tensor", [iota16, sel], [scrA], out=scrA.ap(0, [[256, 8], [16, 16], [1, 16]]),
                          in0=iota16.ap(0, [[0, 8], [0, 16], [1, 16]]), in1=sel.ap(0, [[16, 8], [1, 16], [0, 16]]),
                          op=ALU.is_equal)
                    kb.op("dve", "tensor_tensor", [scrA, i16f], [scrB], out=scrB.ap(0, [[256, 8], [16, 16], [1, 16]]),
                          in0=scrA.ap(0, [[256, 8], [16, 16], [1, 16]]), in1=i16f.ap(off, [[32, 8], [0, 16], [1, 16]]),
                          op=ALU.mult)
                    kb.op("dve", "tensor_reduce", [scrB], [III], out=III.ap(which * 128, [[16, 8], [1, 16]]),
                          in_=scrB.ap(0, [[256, 8], [16, 16], [1, 16]]), axis=AX.X, op=ALU.add)
                    yield
                for i3 in range(3):
                    kb.op("pe", "transpose", [III, ident_f], [PG[0]], PG[0][:, i3 * 128:(i3 + 1) * 128],
                          III[:, i3 * 128:(i3 + 1) * 128], ident_f[:])
                kb.op("act", "copy", [], [ITs, PG[0]], out=ITs[:], in_=PG[0][:, 0:384])
                yield
                for ch in range(128 // CH):
                    kb.op("dve", "tensor_tensor", [iota128, ITs], [Lb], out=Lb.ap(0, [[128, CH], [1, 128]]),
                          in0=iota128.ap(0, [[0, CH], [1, 128]]), in1=ITs.ap(ch * CH, [[1, CH], [0, 128]]), op=ALU.is_equal)
                    kb.op("dve", "tensor_tensor", [iota128, ITs], [Rb], out=Rb.ap(0, [[128, CH], [1, 128]]),
                          in0=iota128.ap(0, [[0, CH], [1, 128]]), in1=ITs.ap(128 + ch * CH, [[1, CH], [0, 128]]), op=ALU.is_equal)
                    kb.op("dve", "tensor_tensor", [Rb, ITs], [Rb], out=Rb.ap(0, [[128, CH], [1, 128]]),
                          in0=Rb.ap(0, [[128, CH], [1, 128]]), in1=ITs.ap(256 + ch * CH, [[1, CH], [0, 128]]), op=ALU.mult)
                    yield
                    for t4 in range(CH // 4):
                        Pg = PG[t4 % 2]
                        for tt in range(4):
                            tl = t4 * 4 + tt
                            kb.op("pe", "matmul", [Lb, Rb], [Pg], Pg[:, tt * 128:(tt + 1) * 128], lhsT=Lb[:, tl * 128:(tl + 1) * 128],
                                  rhs=Rb[:, tl * 128:(tl + 1) * 128], start=True, stop=True)
                        tokb = ch * CH + t4 * 4
                        kb.op("act", "copy", [], [GT, Pg], out=GT.ap(tokb, [[1, 4], [128, 128]]), in_=Pg.ap(0, [[128, 4], [1, 128]]))
                        yield
                for jb in range(8):
                    kb.dma("pool", GTd.ap(jb * 16 * 256 + s * 128, [[128 * 256, 128], [256, 16], [1, 128]]),
                           GT.ap(jb * 16 * 128, [[128, 16], [1, 128]]), [GT], [GTd])
                yield

        def run_steps(gen, n):
            if gen is None:
                return None
            for _ in range(n):
                try:
                    next(gen)
                except StopIteration:
                    return None
            return gen

        gen = prep(0)
        while gen is not None:
            gen = run_steps(gen, 1000)
        for tg in range(NG):
            kb.mark("Dg%d" % tg)
            t0 = tg * 256
            h2T = h2T2[tg % 2]
            GTd = GTd2[tg % 2]
            gen = prep(tg + 1) if tg + 1 < NG else None
            pend_out = [None]
            for jp in range(64):
                bi = jp % NBUF
                j0 = jp * 2
                kb.dma("sp", Ubuf[bi].ap(0, [[1024, 2], [1, 1024]]), UTb.ap(j0 * 131072, [[1024, 128], [131072, 2], [1, 1024]]),
                       [UTb], [Ubuf[bi]])
                kb.dma("act", Vbuf[bi].ap(0, [[1024, 2], [1, 1024]]), VJb.ap(j0 * 131072, [[1024, 128], [131072, 2], [1, 1024]]),
                       [VJb], [Vbuf[bi]])
                kb.dma("sp", Gs[bi].ap(0, [[256, 2], [1, 256]]), GTd.ap(j0 * 256, [[128 * 256, 128], [256, 2], [1, 256]]),
                       [GTd], [Gs[bi]])
                Pa = PA[jp % 2]
                for jj in range(2):
                    for k in range(8):
                        kb.op("pe", "matmul", [h2T, Ubuf[bi]], [Pa], Pa[:, jj * 256:(jj + 1) * 256],
                              lhsT=Ubuf[bi][:, jj, k * 128:(k + 1) * 128], rhs=h2T[:, k, :], start=(k == 0), stop=(k == 7))
                ag = actg[jp % 2]
                wdd = wd[jp % 2]
                kb.op("act", "activation", [], [ag, Pa], out=ag[:], in_=Pa[:, 0:512], func=AF.Gelu)
                kb.op("dve", "tensor_tensor", [ag, Gs[bi]], [wdd], out=wdd[:], in0=ag[:], in1=Gs[bi].ap(0, [[1, 512]]), op=ALU.mult)
                def out_mm(wdd=wdd, bi=bi, j0=j0):
                    for jj in range(2):
                        j = j0 + jj
                        for s in range(2):
                            for half in range(2):
                                Pp = PO[s * 2 + half]
                                kb.op("pe", "matmul", [wdd, Vbuf[bi]], [Pp], Pp[:, 0:512],
                                      lhsT=wdd[:, jj * 256 + s * 128:jj * 256 + (s + 1) * 128],
                                      rhs=Vbuf[bi][:, jj, half * 512:(half + 1) * 512], start=(j == 0), stop=(j == 127))
                if pend_out[0] is not None:
                    pend_out[0]()
                pend_out[0] = out_mm
                gen = run_steps(gen, PREP_STEPS)
            pend_out[0]()
            pend_out[0] = None
            for s in range(2):
                kb.dma("sp", xr[:], x1s.ap((t0 + s * 128) * D, [[D, 128], [1, D]]), [x1s], [xr])
                resid_ln(xr, PO[s * 2], PO[s * 2 + 1], 1024, 2048, 3072, ybuf, ybuf, stf, mvf, sdf, rstdf)
                kb.dma("pool", y.ap((t0 + s * 128) * D, [[D, 128], [1, D]]), ybuf[:], [ybuf], [y])
            while gen is not None:
                gen = run_steps(gen, 1000)
        kb.barrier()
    kb.mark("end")
    kb.wait_all("sp")
    kb.wait_all("pool")
    return nc, kb


def prep_shared(inp):
    f = lambda a: np.ascontiguousarray(a, dtype=np.float32)
    sh = {}
    sh["w_ada"] = f(inp["w_ada"][0])
    sh["b_ada"] = f(inp["b_ada"][0][None, :])
    sh["rows"] = f(np.concatenate([inp["ln1_g"][0], inp["ln1_b"][0], inp["ln2_g"][0], inp["ln2_b"][0],
                                   inp["sgu_ln_g"][0], inp["sgu_ln_b"][0]])[None, :])
    w_in = inp["w_in"][0]
    sh["wz"] = f(w_in[:, 1304:2328])
    sh["w_merge"] = f(inp["w_merge"][0])
    sh["b_merge_col"] = f(inp["b_merge"][0].reshape(16, 128).T)
    sh["w_b1"] = f(inp["w_branch"][0, 1])
    sh["w_out"] = f(inp["w_out"][0])
    sh["wsT"] = f(inp["sgu_w"][0].transpose(2, 0, 1).reshape(128, 1024))
    sh["sgub_col"] = f(inp["sgu_b"][0].T)
    jj, ii = np.meshgrid(np.arange(128), np.arange(128), indexing="ij")
    sh["trilm"] = f((jj <= ii).astype(np.float32))
    sh["peer_wq"] = f(inp["peer_wq"][0])
    sh["keysT"] = f(inp["peer_keys"][0].reshape(16, 128, 128).transpose(2, 0, 1).reshape(128, 2048))
    pu = inp["peer_u"][0].reshape(128, 128, 8, 128)
    sh["UT"] = f(pu.transpose(1, 3, 2, 0).reshape(128, 128, 1024))
    pv = inp["peer_v"][0].reshape(128, 128, 1024)
    sh["VJ"] = f(pv.transpose(1, 0, 2))
    def swp(c):
        blocks = [np.concatenate([c[:, i * 64 + 32:(i + 1) * 64], c[:, i * 64:i * 64 + 32]], axis=1) for i in range(c.shape[1] // 64)]
        return np.concatenate(blocks, axis=1)
    qch = [np.concatenate([w_in[:, r * 64:(r + 1) * 64], w_in[:, (4 + r) * 64:(5 + r) * 64]], axis=1) for r in range(4)]
    kcs = [w_in[:, 512:640], w_in[:, 768:896], w_in[:, 1024:1152]]
    chunks = qch + [swp(c) for c in qch] + kcs + [swp(c) for c in kcs] + [w_in[:, 640:768]]
    sh["WAf"] = f(np.concatenate(chunks, axis=1))
    sh["WAt"] = f(np.concatenate([w_in[:, 896:1024], w_in[:, 1152:1280], w_in[:, 1280:1304]], axis=1))
    dup = lambda a: np.concatenate([a, a], axis=0)
    w1 = inp["cmp_w1"][0]
    sh["w1d"] = f(np.stack([dup(w1[kv].reshape(32, 64, 128).transpose(1, 0, 2).reshape(64, 4096)) for kv in range(2)]))
    pos = inp["cmp_pos"][0]
    sh["posTd"] = f(dup(np.concatenate([pos[0].T, pos[1].T], axis=1)))
    sh["b1col"] = f(inp["cmp_b1"][0].T)
    w2 = inp["cmp_w2"][0]
    sh["w2kd"] = f(np.concatenate([w2[0], w2[0]], axis=1))
    sh["w2v"] = f(w2[1])
    sh["b2kcol"] = f(dup(inp["cmp_b2"][0][0][:, None]))
    sh["b2vrow"] = f(inp["cmp_b2"][0][1][None, :])
    sh["w_b0"] = f(inp["w_branch"][0, 0])
    return sh


def prep_consts(S):
    f = lambda a: np.ascontiguousarray(a, dtype=np.float32)
    NEG = -30000.0
    cst = {}
    p = np.arange(128)
    d = p % 64
    inv_freq = (np.float32(10000.0) ** (-(np.arange(32, dtype=np.float32)) / np.float32(32))).astype(np.float32)
    ang = (np.arange(S, dtype=np.float32)[None, :] * inv_freq[d % 32][:, None]).astype(np.float32)
    cst["cosT"] = f(np.cos(ang))
    sgn = np.where(d < 32, -1.0, 1.0).astype(np.float32)[:, None]
    cst["sinS"] = f(np.sin(ang) * sgn)
    ncmp = (S - 32) // 16 + 1
    nsel = S // 64
    c0 = np.arange(ncmp)[:, None] * 16
    s0 = np.arange(nsel)[None, :] * 64
    ov = np.clip(np.minimum(c0 + 32, s0 + 64) - np.maximum(c0, s0), 0, None) / 32.0
    agg = np.zeros((512, 128), np.float32)
    agg[:ncmp, :nsel] = ov
    cst["aggd"] = f(agg.reshape(4, 128, 128).transpose(1, 0, 2).reshape(128, 512))
    nl = np.arange(128)[:, None, None]
    m = np.arange(16)[None, :, None]
    ql = np.arange(128)[None, None, :]
    cst["cmpb"] = f(np.where(16 * nl + 31 - ql <= 128 * m, 0.0, NEG).reshape(128, 2048))
    kk = np.arange(128)[:, None]
    qq = np.arange(128)[None, :]
    cst["caus"] = f(np.concatenate([np.where(kk <= qq, 0.0, NEG), np.where(kk > qq, 0.0, NEG)], axis=1))
    q = np.arange(128)[:, None]
    c = np.arange(254)[None, :]
    dd = c - 126 - (q >= 64)
    base = np.where(dd > 0, -1e30, np.where(dd == 0, 2e9, np.where(dd == -1, 1e9, 0.0)))
    j0 = np.zeros((128, 128))
    j0[:, 0] = 3e9
    cst["based"] = f(np.concatenate([base, j0], axis=1))
    key = np.arange(S)[None, :]
    cst["ebig"] = f((key // 64 == np.arange(128)[:, None]).astype(np.float32))
    return cst


def kernel(**inputs):
    B, S = inputs["x"].shape[0], inputs["x"].shape[1]
    nc, kb = build(S)
    sh = prep_shared(inputs)
    sh.update(prep_consts(S))
    in_maps = []
    for b in range(B):
        m = dict(sh)
        m["x"] = np.ascontiguousarray(inputs["x"][b], dtype=np.float32)
        m["c_col"] = np.ascontiguousarray(inputs["c"][b].reshape(8, 128).T, dtype=np.float32)
        in_maps.append(m)
    res = run_bass_kernel_spmd(nc, in_maps, core_ids=list(range(B)))
    return np.stack([np.asarray(r["y"], dtype=np.float32) for r in res.results], axis=0)
```
